# Optimizing a Trainium2 kernel written in Bass

```python
import math
import jax, jax.numpy as jnp
from jax import lax
import numpy as np

D_MODEL = 1024
BATCH = 4
SEQ = 8192
DEPTH = 2

N_MIXERS = 2
N_EVEN = (DEPTH + 1) // 2
N_ODD = DEPTH // 2

MLA_HEADS = 16
QK_NOPE = 64
QK_ROPE = 32
V_DIM = 64
Q_LORA = 384
KV_LORA = 256
MLA_IN = Q_LORA + KV_LORA + QK_ROPE
ROPE_THETA = 10000.0
Q_BLOCK = 128
NEG_INF = -1e30

S5_GROUP = 16
S5_GROUPS = D_MODEL // S5_GROUP
S5_STATE = 64
DT_MIN = 0.001
DT_MAX = 0.1

D_FF = 2816
N_EXPERTS = 8
TOP_K = 2
D_FF_EXPERT = 3584

EPS = 1e-6

kernel_name = "hybrid_mla_s5_moe_trunk"


def rmsnorm(x, g):
    xf = x.astype(jnp.float32)
    y = xf * lax.rsqrt(jnp.mean(xf * xf, axis=-1, keepdims=True) + EPS)
    return (y * g.astype(jnp.float32)).astype(x.dtype)


def rope(x, cos, sin):
    x1, x2 = jnp.split(x, 2, axis=-1)
    return jnp.concatenate([x1 * cos - x2 * sin, x2 * cos + x1 * sin], axis=-1).astype(x.dtype)


def mla_mixer(h, positions, w_in, q_norm, w_uq, kv_norm, w_ukv, w_o):
    B, S, _ = h.shape
    f32 = jnp.float32
    proj = h @ w_in
    c_q = rmsnorm(proj[..., :Q_LORA], q_norm)
    c_kv = rmsnorm(proj[..., Q_LORA:Q_LORA + KV_LORA], kv_norm)
    k_pe = proj[..., Q_LORA + KV_LORA:]
    q = (c_q @ w_uq).reshape(B, S, MLA_HEADS, QK_NOPE + QK_ROPE)
    q_nope, q_pe = q[..., :QK_NOPE], q[..., QK_NOPE:]
    inv_freq = ROPE_THETA ** (-jnp.arange(0, QK_ROPE, 2, dtype=f32) / QK_ROPE)
    ang = positions.astype(f32)[..., None] * inv_freq
    cos, sin = jnp.cos(ang), jnp.sin(ang)
    q_pe = rope(q_pe, cos[:, :, None, :], sin[:, :, None, :])
    k_pe = rope(k_pe, cos, sin)
    kv = (c_kv @ w_ukv).reshape(B, S, MLA_HEADS, QK_NOPE + V_DIM)
    k_nope, v = kv[..., :QK_NOPE], kv[..., QK_NOPE:]
    scale = 1.0 / math.sqrt(QK_NOPE + QK_ROPE)
    n_blocks = S // Q_BLOCK

    def to_blocks(t):
        return t.reshape(B, n_blocks, Q_BLOCK, *t.shape[2:]).swapaxes(0, 1)

    k_idx = jnp.arange(S, dtype=jnp.int32)

    def attend_block(args):
        qn, qp, start = args
        s = (jnp.einsum('bqhd,bkhd->bhqk', qn, k_nope, preferred_element_type=f32)
             + jnp.einsum('bqhr,bkr->bhqk', qp, k_pe, preferred_element_type=f32))
        q_idx = start + jnp.arange(Q_BLOCK, dtype=jnp.int32)
        causal = k_idx[None, :] <= q_idx[:, None]
        s = jnp.where(causal, s * scale, NEG_INF)
        p = jax.nn.softmax(s, axis=-1).astype(v.dtype)
        return jnp.einsum('bhqk,bkhd->bqhd', p, v)

    starts = jnp.arange(n_blocks, dtype=jnp.int32) * Q_BLOCK
    o = lax.map(attend_block, (to_blocks(q_nope), to_blocks(q_pe), starts))
    o = o.swapaxes(0, 1).reshape(B, S, MLA_HEADS * V_DIM)
    return o @ w_o


def _complex_affine_combine(left, right):
    a1r, a1i, b1r, b1i = left
    a2r, a2i, b2r, b2i = right
    ar = a2r * a1r - a2i * a1i
    ai = a2r * a1i + a2i * a1r
    br = a2r * b1r - a2i * b1i + b2r
    bi = a2r * b1i + a2i * b1r + b2i
    return (ar, ai, br, bi)


def s5_mixer(h, w_in, lam_re, lam_im, log_dt, b_re, b_im, c_re, c_im, d_skip, w_glu):
    B, S, D = h.shape
    f32 = jnp.float32
    u = (h @ w_in).astype(f32).reshape(B, S, S5_GROUPS, S5_GROUP)
    lr, li = lam_re.astype(f32), lam_im.astype(f32)
    dt = jnp.exp(log_dt.astype(f32))[:, None]
    mag = jnp.exp(lr * dt)
    ar = mag * jnp.cos(li * dt)
    ai = mag * jnp.sin(li * dt)
    den = lr * lr + li * li
    gr = ((ar - 1.0) * lr + ai * li) / den
    gi = (ai * lr - (ar - 1.0) * li) / den
    br, bi = b_re.astype(f32), b_im.astype(f32)
    bbr = gr[..., None] * br - gi[..., None] * bi
    bbi = gr[..., None] * bi + gi[..., None] * br
    bu_r = jnp.einsum('bsgh,gph->sbgp', u, bbr)
    bu_i = jnp.einsum('bsgh,gph->sbgp', u, bbi)
    a_r = jnp.broadcast_to(ar[None, None], (S, 1, S5_GROUPS, S5_STATE))
    a_i = jnp.broadcast_to(ai[None, None], (S, 1, S5_GROUPS, S5_STATE))
    _, _, xr, xi = lax.associative_scan(_complex_affine_combine, (a_r, a_i, bu_r, bu_i), axis=0)
    y = (jnp.einsum('sbgp,ghp->bsgh', xr, c_re.astype(f32))
         - jnp.einsum('sbgp,ghp->bsgh', xi, c_im.astype(f32))
         + d_skip.astype(f32) * u)
    y = jax.nn.gelu(y.reshape(B, S, D)).astype(h.dtype)
    z = y @ w_glu
    return z[..., :D] * jax.nn.sigmoid(z[..., D:])


def swiglu(h, w_gate, w_up, w_down):
    return (jax.nn.silu(h @ w_gate) * (h @ w_up)) @ w_down


def moe_swiglu(h, w_router, w_gate, w_up, w_down):
    B, S, D = h.shape
    t = h.reshape(B * S, D)
    logits = (t @ w_router).astype(jnp.float32)
    top_vals, top_idx = lax.top_k(logits, TOP_K)
    gates = jax.nn.softmax(top_vals, axis=-1)
    combine = jnp.sum(jax.nn.one_hot(top_idx, N_EXPERTS, dtype=jnp.float32) * gates[..., None], axis=1)
    out = jnp.zeros_like(t)
    for e in range(N_EXPERTS):
        he = swiglu(t, w_gate[e], w_up[e], w_down[e])
        out = out + combine[:, e:e + 1].astype(t.dtype) * he
    return out.reshape(B, S, D)


def setup_inputs(seed: int = 0) -> dict:
    key = jax.random.key(seed)
    ks = iter(jax.random.split(key, 40))
    f32 = jnp.float32

    def nrm(shape, scale):
        return jax.random.normal(next(ks), shape, f32) * scale

    D = D_MODEL
    x = jax.random.normal(next(ks), (BATCH, SEQ, D), f32)
    offset = jax.random.randint(next(ks), (BATCH, 1), 0, 1024, dtype=jnp.int32)
    positions = offset + jnp.arange(SEQ, dtype=jnp.int32)[None, :]
    mix_norm = 1.0 + nrm((DEPTH, D), 0.05)
    ffn_norm = 1.0 + nrm((DEPTH, D), 0.05)
    final_norm = 1.0 + nrm((D,), 0.05)
    mla_w_in = nrm((N_EVEN, D, MLA_IN), D ** -0.5)
    mla_q_norm = 1.0 + nrm((N_EVEN, Q_LORA), 0.05)
    mla_w_uq = nrm((N_EVEN, Q_LORA, MLA_HEADS * (QK_NOPE + QK_ROPE)), Q_LORA ** -0.5)
    mla_kv_norm = 1.0 + nrm((N_EVEN, KV_LORA), 0.05)
    mla_w_ukv = nrm((N_EVEN, KV_LORA, MLA_HEADS * (QK_NOPE + V_DIM)), KV_LORA ** -0.5)
    mla_w_o = nrm((N_EVEN, MLA_HEADS * V_DIM, D), (MLA_HEADS * V_DIM) ** -0.5)
    ffn_w_gate = nrm((N_EVEN, D, D_FF), D ** -0.5)
    ffn_w_up = nrm((N_EVEN, D, D_FF), D ** -0.5)
    ffn_w_down = nrm((N_EVEN, D_FF, D), D_FF ** -0.5)
    s5_w_in = nrm((N_ODD, D, D), D ** -0.5)
    s5_lambda_re = -0.5 + nrm((N_ODD, S5_GROUPS, S5_STATE), 0.01)
    s5_lambda_im = math.pi * jnp.arange(S5_STATE, dtype=f32)[None, None, :] + nrm((N_ODD, S5_GROUPS, S5_STATE), 0.01)
    s5_log_dt = jax.random.uniform(next(ks), (N_ODD, S5_GROUPS), f32, math.log(DT_MIN), math.log(DT_MAX))
    s5_b_re = nrm((N_ODD, S5_GROUPS, S5_STATE, S5_GROUP), (2 * S5_GROUP) ** -0.5)
    s5_b_im = nrm((N_ODD, S5_GROUPS, S5_STATE, S5_GROUP), (2 * S5_GROUP) ** -0.5)
    s5_c_re = nrm((N_ODD, S5_GROUPS, S5_GROUP, S5_STATE), (2 * S5_STATE) ** -0.5)
    s5_c_im = nrm((N_ODD, S5_GROUPS, S5_GROUP, S5_STATE), (2 * S5_STATE) ** -0.5)
    s5_d = nrm((N_ODD, S5_GROUPS, S5_GROUP), 1.0)
    s5_w_glu = nrm((N_ODD, D, 2 * D), D ** -0.5)
    moe_w_router = nrm((N_ODD, D, N_EXPERTS), D ** -0.5)
    moe_w_gate = nrm((N_ODD, N_EXPERTS, D, D_FF_EXPERT), D ** -0.5)
    moe_w_up = nrm((N_ODD, N_EXPERTS, D, D_FF_EXPERT), D ** -0.5)
    moe_w_down = nrm((N_ODD, N_EXPERTS, D_FF_EXPERT, D), D_FF_EXPERT ** -0.5)
    return {
        "x": x, "positions": positions,
        "mix_norm": mix_norm, "ffn_norm": ffn_norm, "final_norm": final_norm,
        "mla_w_in": mla_w_in, "mla_q_norm": mla_q_norm, "mla_w_uq": mla_w_uq,
        "mla_kv_norm": mla_kv_norm, "mla_w_ukv": mla_w_ukv, "mla_w_o": mla_w_o,
        "ffn_w_gate": ffn_w_gate, "ffn_w_up": ffn_w_up, "ffn_w_down": ffn_w_down,
        "s5_w_in": s5_w_in, "s5_lambda_re": s5_lambda_re, "s5_lambda_im": s5_lambda_im,
        "s5_log_dt": s5_log_dt, "s5_b_re": s5_b_re, "s5_b_im": s5_b_im,
        "s5_c_re": s5_c_re, "s5_c_im": s5_c_im, "s5_d": s5_d, "s5_w_glu": s5_w_glu,
        "moe_w_router": moe_w_router, "moe_w_gate": moe_w_gate,
        "moe_w_up": moe_w_up, "moe_w_down": moe_w_down,
    }


def reference(x, positions, mix_norm, ffn_norm, final_norm,
              mla_w_in, mla_q_norm, mla_w_uq, mla_kv_norm, mla_w_ukv, mla_w_o,
              ffn_w_gate, ffn_w_up, ffn_w_down,
              s5_w_in, s5_lambda_re, s5_lambda_im, s5_log_dt, s5_b_re, s5_b_im,
              s5_c_re, s5_c_im, s5_d, s5_w_glu,
              moe_w_router, moe_w_gate, moe_w_up, moe_w_down):
    h = x
    for i in range(DEPTH):
        j = i // N_MIXERS
        hn = rmsnorm(h, mix_norm[i])
        if i % N_MIXERS == 0:
            h = h + mla_mixer(hn, positions, mla_w_in[j], mla_q_norm[j], mla_w_uq[j],
                              mla_kv_norm[j], mla_w_ukv[j], mla_w_o[j])
        else:
            h = h + s5_mixer(hn, s5_w_in[j], s5_lambda_re[j], s5_lambda_im[j], s5_log_dt[j],
                             s5_b_re[j], s5_b_im[j], s5_c_re[j], s5_c_im[j], s5_d[j], s5_w_glu[j])
        hn = rmsnorm(h, ffn_norm[i])
        if i % 2 == 0:
            h = h + swiglu(hn, ffn_w_gate[j], ffn_w_up[j], ffn_w_down[j])
        else:
            h = h + moe_swiglu(hn, moe_w_router[j], moe_w_gate[j], moe_w_up[j], moe_w_down[j])
    return rmsnorm(h, final_norm)
```

```python
import numpy as np
import concourse.bass as bass
import concourse.mybir as mybir
from contextlib import ExitStack, contextmanager

F32 = mybir.dt.float32
BF16 = mybir.dt.bfloat16
I32 = mybir.dt.int32
AF = mybir.ActivationFunctionType
ALU = mybir.AluOpType
AX = mybir.AxisListType

ENGS = ("pe", "dve", "act", "pool", "sp")
DMAQ = ("sp", "pool", "act")
SAME_ENGINE_FIFO = ("pe",)
NDMASEM = 12


def _key(k):
    if isinstance(k, (str, int)):
        return k
    if isinstance(k, tuple):
        return tuple(_key(x) for x in k)
    return k.name


class Prog:
    def __init__(self, nc):
        self.nc = nc
        self.ops = []
        self.emitted = 0
        self.last_w = {}
        self.readers = {}
        self.stack = ExitStack()
        self.semstack = ExitStack()
        self.sem_eng = {e: self.semstack.enter_context(nc.semaphore("s_" + e)) for e in ENGS}
        self.sem_dma = {e: [self.semstack.enter_context(nc.semaphore("d_%s_%d" % (e, i))) for i in range(NDMASEM)]
                        for e in DMAQ}
        self.cnt = {e: 0 for e in ENGS}
        self.dcnt = {e: 0 for e in ENGS}
        self.seen = {e: {} for e in ENGS}
        self.dma_last = {}

    def sb(self, name, shape, dt):
        self.uid = getattr(self, "uid", 0) + 1
        return self.stack.enter_context(self.nc.sbuf_tensor("sb%d_%s" % (self.uid, name), list(shape), dt))

    def ps(self, name, shape, dt=F32):
        self.uid = getattr(self, "uid", 0) + 1
        return self.stack.enter_context(self.nc.psum_tensor("ps%d_%s" % (self.uid, name), list(shape), dt))

    @contextmanager
    def phase(self):
        saved = self.stack
        st = ExitStack()
        self.stack = st
        try:
            yield
            self.flush()
        except BaseException:
            import traceback
            traceback.print_exc()
            raise
        finally:
            st.close()
            self.stack = saved

    def op(self, eng, fn, reads=(), writes=(), dma=False):
        oid = len(self.ops)
        reads = [_key(k) for k in reads]
        writes = [_key(k) for k in writes]
        deps = set()
        for k in reads:
            w = self.last_w.get(k)
            if w is not None:
                deps.add(w)
        for k in writes:
            w = self.last_w.get(k)
            if w is not None:
                deps.add(w)
            for r in self.readers.get(k, ()):
                deps.add(r)
        deps.discard(oid)
        rec = dict(id=oid, eng=eng, fn=fn, deps=deps, dma=dma, sig=False)
        self.ops.append(rec)
        for k in reads:
            self.readers.setdefault(k, []).append(oid)
        for k in writes:
            self.last_w[k] = oid
            self.readers[k] = []
        return oid

    def dma(self, eng, out, in_, reads=(), writes=(), **kw):
        def fn(e):
            return e.dma_start(out=out, in_=in_, **kw)
        return self.op(eng, fn, reads, writes, dma=True)

    def cc(self, kind, alu, groups, in_ap, out_ap, reads=(), writes=()):
        def fn(e):
            return e.collective_compute(kind, alu, replica_groups=groups, ins=[in_ap], outs=[out_ap])
        oid = self.op("pool", fn, reads, writes)
        self.ops[oid]["cc"] = True
        return oid

    def flush(self):
        nc = self.nc
        ops = self.ops
        batch = ops[self.emitted:]
        if not batch:
            return
        eng_ops = {e: [] for e in ENGS}
        for o in batch:
            eng_ops[o["eng"]].append(o)
            nd = set()
            for d in o["deps"]:
                po = ops[d]
                if po["eng"] == o["eng"] and po["eng"] in SAME_ENGINE_FIFO and not po["dma"] and not o["dma"] \
                        and not po.get("cc") and not o.get("cc"):
                    continue
                nd.add(d)
            o["deps"] = nd
            for d in nd:
                ops[d]["sig"] = True
        for e in ENGS:
            for o in reversed(eng_ops[e]):
                if not o["dma"] and not o.get("cc"):
                    o["sig"] = True
                    break
        for o in batch:
            e = o["eng"]
            if o["dma"]:
                k = self.dcnt[e]
                self.dcnt[e] += 1
                o["sem"] = self.sem_dma[e][k % NDMASEM]
                o["semkey"] = (e, k % NDMASEM)
                o["val"] = 16 * (k // NDMASEM + 1)
                o["prev_val"] = 16 * (k // NDMASEM)
                self.dma_last[o["semkey"]] = (o["sem"], o["val"])
            elif o.get("cc"):
                self.ncc = getattr(self, "ncc", 0) + 1
                o["sem"] = self.semstack.enter_context(nc.semaphore("cc_%d" % self.ncc))
                o["semkey"] = ("cc", self.ncc)
                o["val"] = 1
                self.dma_last[o["semkey"]] = (o["sem"], 1)
            elif o["sig"]:
                self.cnt[e] += 1
                o["sem"] = self.sem_eng[e]
                o["semkey"] = (e, -1)
                o["val"] = self.cnt[e]
        final_eng = {e: self.cnt[e] for e in ENGS}
        final_dma = dict(self.dma_last)

        def run_engine(ename, eobj):
            seen = self.seen[ename]
            for o in eng_ops[ename]:
                waits = {}
                for d in o["deps"]:
                    po = ops[d]
                    key = po["semkey"]
                    if seen.get(key, 0) >= po["val"]:
                        continue
                    if key not in waits or waits[key][1] < po["val"]:
                        waits[key] = (po["sem"], po["val"])
                if o["dma"] and o["prev_val"] > 0:
                    key = o["semkey"]
                    if seen.get(key, 0) < o["prev_val"]:
                        if key not in waits or waits[key][1] < o["prev_val"]:
                            waits[key] = (o["sem"], o["prev_val"])
                for key, (sem, val) in waits.items():
                    eobj.wait_ge(sem, val)
                    seen[key] = val
                ins = o["fn"](eobj)
                if o["dma"]:
                    ins.then_inc(o["sem"], 16)
                elif o.get("cc"):
                    ins.then_inc(o["sem"])
                elif o["sig"]:
                    ins.then_inc(o["sem"], 1)
            for e2 in ENGS:
                key = (e2, -1)
                if final_eng[e2] > seen.get(key, 0):
                    eobj.wait_ge(self.sem_eng[e2], final_eng[e2])
                    seen[key] = final_eng[e2]
            for key, (sem, val) in final_dma.items():
                if seen.get(key, 0) < val:
                    eobj.wait_ge(sem, val)
                    seen[key] = val

        with nc.Block() as block:
            @block.tensor
            def _(e):
                run_engine("pe", e)

            @block.vector
            def _(e):
                run_engine("dve", e)

            @block.scalar
            def _(e):
                run_engine("act", e)

            @block.gpsimd
            def _(e):
                run_engine("pool", e)

            @block.sync
            def _(e):
                run_engine("sp", e)
        self.emitted = len(ops)
        self.last_w = {}
        self.readers = {}
        for o in batch:
            o["fn"] = None

    def finish(self):
        self.flush()
        self.stack.close()
        self.semstack.close()

import math
from concourse.bass_utils import run_bass_kernel_spmd
from concourse.ap import AP

D = 1024
S = 8192
T = 4096
TT = 512
H = 16
EPS = 1e-6
NEGBIG = 30000.0
TWO_PI = 2.0 * math.pi


class Ring:
    def __init__(self, p, name, n, shape, dt, psum=False):
        self.tiles = [(p.ps if psum else p.sb)("%s%d" % (name, i), shape, dt) for i in range(n)]
        self.i = 0

    def next(self):
        t = self.tiles[self.i % len(self.tiles)]
        self.i += 1
        return t


class KB:
    def __init__(self, nc, dbg=None):
        self.nc = nc
        self.p = Prog(nc)
        self.dbg = dbg
        self.ins = {}
        self.outs = {}
        self.alt = 0
        self.wkeys = {}

    def din(self, name, shape, dt=F32):
        a = self.nc.dram_tensor(name, list(shape), dt, kind="ExternalInput").ap()
        self.ins[name] = a
        return a

    def dout(self, name, shape, dt=F32):
        a = self.nc.dram_tensor(name, list(shape), dt, kind="ExternalOutput").ap()
        self.outs[name] = a
        return a

    def dscr(self, name, shape, dt):
        return self.nc.dram_tensor(name, list(shape), dt).ap()

    def mm(self, out, lhsT, rhs, start, stop, r, w):
        self.p.op("pe", lambda e, a=out, b=lhsT, c=rhs, s=start, t=stop: e.matmul(a, b, c, start=s, stop=t),
                  reads=r, writes=w)

    def act(self, out, in_, func, r, w, bias=None, scale=None):
        kw = {}
        if bias is not None:
            kw["bias"] = bias
        if scale is not None:
            kw["scale"] = scale
        self.p.op("act", lambda e, a=out, b=in_, f=func, kw=kw: e.activation(a, b, f, **kw), reads=r, writes=w)

    def tt(self, eng, out, a, b, op, r, w):
        self.p.op(eng, lambda e, o=out, x=a, y=b, f=op: e.tensor_tensor(o, x, y, f), reads=r, writes=w)

    def ts(self, eng, out, a, s1, op0, r, w, s2=None, op1=None):
        if op1 is None:
            self.p.op(eng, lambda e, o=out, x=a, s=s1, f=op0: e.tensor_scalar(o, x, s, None, f), reads=r, writes=w)
        else:
            self.p.op(eng, lambda e, o=out, x=a, s=s1, f=op0, t=s2, g=op1: e.tensor_scalar(o, x, s, t, f, g),
                      reads=r, writes=w)

    def stt(self, eng, out, a, s, b, op0, op1, r, w):
        self.p.op(eng, lambda e, o=out, x=a, sc=s, y=b, f=op0, g=op1: e.scalar_tensor_tensor(o, x, sc, y, f, g),
                  reads=r, writes=w)

    def copy(self, eng, out, in_, r, w):
        if eng == "act":
            self.act(out, in_, AF.Copy, r, w)
        else:
            self.p.op(eng, lambda e, o=out, x=in_: e.tensor_copy(o, x), reads=r, writes=w)

    def memset(self, eng, ap, val, w):
        self.p.op(eng, lambda e, a=ap, v=val: e.memset(a, v), reads=(), writes=w)

    def recip(self, out, in_, r, w):
        self.p.op("dve", lambda e, o=out, x=in_: e.reciprocal(o, x), reads=r, writes=w)

    def dma(self, eng, out, in_, r=(), w=(), **kw):
        self.p.dma(eng, out, in_, reads=r, writes=w, **kw)

    def veng(self):
        self.alt += 1
        return "dve" if self.alt % 2 else "pool"

    def load_w(self, tile, dram, K, M, m0=0, mw=None, eng="pool"):
        kc = K // 128
        mw = M if mw is None else mw
        src = dram.rearrange("(k p) m -> p k m", p=128)
        step = 1024
        keys = []
        for a in range(0, mw, step):
            b = min(mw, a + step)
            self.dma(eng, tile[:, 0:kc, a:b], src[:, :, m0 + a:m0 + b], w=[(tile, "w", a)])
            keys.append((tile, "w", a))
        self.wkeys[tile.name] = keys
        return keys

    def wk(self, tile):
        return self.wkeys[tile.name]

    def rmsnorm(self, src_chunks, src_keys, nck, Dn, gains, gc, out_tile, out_key, W=TT):
        c = self.c
        sq = c["sq_ring"].next()
        for k in range(nck):
            self.act(sq[:, k, :W], src_chunks[k], AF.Square, r=[src_keys[k]], w=[(sq, k)])
        ps = c["ps_ring"].next()
        for k in range(nck):
            self.mm(ps[:, :W], c["ones_bf"][:], sq[:, k, :W], k == 0, k == nck - 1, r=[(sq, k), c["ones_bf"]], w=[ps])
        sd = c["sd_ring"].next()
        self.act(sd[:, :W], ps[:, :W], AF.Sqrt, r=[ps], w=[sd], bias=c["cst"][:, 2:3], scale=1.0 / Dn)
        rs = c["rs_ring"].next()
        self.recip(rs[:, :W], sd[:, :W], r=[sd], w=[rs])
        for k in range(nck):
            self.stt("dve", out_tile[:, k, :W], src_chunks[k], gains[:, gc + k:gc + k + 1], rs[:, :W],
                     ALU.mult, ALU.mult, r=[src_keys[k], rs], w=[(out_key, k)])
        return rs


G_MIX0, G_FFN0, G_MIX1, G_FFN1, G_FIN, G_QN, G_KVN = 0, 8, 16, 24, 32, 40, 43
RB = 2048


def norm_rings(kb, p, nps=None):
    c = kb.c
    c["sq_ring"] = Ring(p, "sq", 2, [128, 8, TT], BF16)
    c["sd_ring"] = Ring(p, "sd", 2, [128, TT], F32)
    c["rs_ring"] = Ring(p, "rs", 2, [128, TT], F32)


def phase_kvq(kb, I):
    p, c = kb.p, kb.c
    gains, cst = c["gains"], c["cst"]
    kT, qT, vS = I["kT"], I["qT"], I["vS"]
    with p.phase():
        ropeC = kb.dscr("ropeC", [128, S], F32)
        ropeS = kb.dscr("ropeS", [128, S], F32)
        with ExitStack() as tmp:
            p.stack, sv2 = tmp, p.stack
            posi = p.sb("posi", [128, RB], I32)
            ang = p.sb("ang", [128, RB], F32)
            t1 = p.sb("rt1", [128, RB], F32)
            t2 = p.sb("rt2", [128, RB], F32)
            ki = p.sb("rki", [128, RB], I32)
            tabt = p.sb("tabt", [128, RB], F32)
            for rb in range(S // RB):
                rc_ = slice(rb * RB, (rb + 1) * RB)
                kb.dma("sp", posi[:], I["pos128"][:, rc_], w=[posi])
                kb.copy("dve", ang[:], posi[:], r=[posi], w=[ang])
                kb.ts("dve", ang[:], ang[:], cst[:, 0:1], ALU.mult, r=[ang, cst], w=[ang])
                for (tab, shift, sgn) in ((ropeS, 0.0, True), (ropeC, math.pi / 2, False)):
                    kb.ts("dve", t1[:], ang[:], shift, ALU.add, r=[ang], w=[t1])
                    kb.ts("dve", t2[:], t1[:], 1.0 / TWO_PI, ALU.mult, r=[t1], w=[t2])
                    kb.copy("dve", ki[:], t2[:], r=[t2], w=[ki])
                    kb.copy("dve", t2[:], ki[:], r=[ki], w=[t2])
                    kb.stt("dve", t1[:], t2[:], -TWO_PI, t1[:], ALU.mult, ALU.add, r=[t1, t2], w=[t1])
                    kb.ts("dve", t2[:], t1[:], math.pi, ALU.is_gt, r=[t1], w=[t2])
                    kb.stt("dve", t1[:], t2[:], -TWO_PI, t1[:], ALU.mult, ALU.add, r=[t1, t2], w=[t1])
                    kb.ts("dve", t2[:], t1[:], -math.pi, ALU.is_lt, r=[t1], w=[t2])
                    kb.stt("dve", t1[:], t2[:], TWO_PI, t1[:], ALU.mult, ALU.add, r=[t1, t2], w=[t1])
                    kb.ts("dve", t1[:], t1[:], math.pi, ALU.min, r=[t1], w=[t1], s2=-math.pi, op1=ALU.max)
                    kb.act(tabt[:], t1[:], AF.Sin, r=[t1], w=[tabt])
                    if sgn:
                        kb.ts("dve", tabt[:], tabt[:], cst[:, 1:2], ALU.mult, r=[tabt, cst], w=[tabt])
                    kb.dma("sp", tab[:, rc_], tabt[:], r=[tabt], w=[("rope", rb)])
            kbf = p.sb("kbf", [128, S // 128], F32)
            kbb = p.sb("kbb", [128, S // 128], BF16)
            kb.dma("sp", kbf[:], I["kbias"], w=[kbf])
            kb.copy("dve", kbb[:], kbf[:], r=[kbf], w=[kbb])
            for h in range(H):
                kb.dma("sp", kT[h, 96:97, :].rearrange("o (p j) -> (o p) j", p=128), kbb[:], r=[kbb], w=[("kT", h, "b")])
            qm1 = p.sb("qm1", [128, T // 128], BF16)
            kb.memset("dve", qm1[:], -1.0, w=[qm1])
            for h in range(H):
                kb.dma("sp", qT[h, 96:97, :].rearrange("o (p j) -> (o p) j", p=128), qm1[:], r=[qm1], w=[("qT", h, "b")])
            p.flush()
            p.stack = sv2
        psr = c["ps_ring"] = Ring(p, "ps", 6, [128, 512], F32, psum=True)
        norm_rings(kb, p)
        W_in_q = p.sb("W_in_q", [128, 8, 384], BF16)
        W_in_kv = p.sb("W_in_kv", [128, 8, 256], BF16)
        W_in_pe = p.sb("W_in_pe", [128, 8, 32], BF16)
        W_in_pesw = p.sb("W_in_pesw", [128, 8, 32], BF16)
        W_uq_nope = p.sb("W_uq_nope", [128, 3, 1024], BF16)
        W_uq_pe = p.sb("W_uq_pe", [128, 3, 512], BF16)
        W_uq_pesw = p.sb("W_uq_pesw", [128, 3, 512], BF16)
        W_uk = p.sb("W_uk", [128, 2, 1024], BF16)
        W_uv = p.sb("W_uv", [128, 2, 1024], BF16)
        kb.load_w(W_in_kv, I["w_in_kv"], D, 256)
        kb.load_w(W_in_pe, I["w_in_pe"], D, 32)
        kb.load_w(W_in_pesw, I["w_in_pesw"], D, 32)
        kb.load_w(W_uk, I["w_uk"], 256, 1024)
        kb.load_w(W_uv, I["w_uv"], 256, 1024)
        kb.load_w(W_in_q, I["w_in_q"], D, 384)
        kb.load_w(W_uq_nope, I["w_uq_nope"], 384, 1024)
        kb.load_w(W_uq_pe, I["w_uq_pe"], 384, 512)
        kb.load_w(W_uq_pesw, I["w_uq_pesw"], 384, 512)
        ct_ring = Ring(p, "ct", 3, [128, TT], F32)
        st_ring = Ring(p, "st", 3, [128, TT], F32)
        x_ring = Ring(p, "xt", 2, [128, 8, TT], F32)
        hn_ring = Ring(p, "hn", 3, [128, 8, TT], BF16)
        lat_ring = Ring(p, "lat", 3, [128, 3, TT], F32)
        latn_ring = Ring(p, "latn", 3, [128, 3, TT], BF16)
        ev_ring = Ring(p, "ev", 4, [128, TT], BF16)
        rp_ring = Ring(p, "rp", 4, [128, TT], F32)
        vt_ring = Ring(p, "vt", 2, [128, 1024], BF16)
        xTv = I["xT"].rearrange("(k p) t -> p k t", p=128)
        n_tiles = S // TT
        st1 = {}

        def stage1(tt):
            cols = slice(tt * TT, (tt + 1) * TT)
            xt = x_ring.next()
            kb.dma("sp", xt[:], xTv[:, :, cols], w=[xt])
            Ct = ct_ring.next()
            St = st_ring.next()
            kb.dma("sp", Ct[:], ropeC[:, cols], w=[Ct])
            kb.dma("sp", St[:], ropeS[:, cols], w=[St])
            hn = hn_ring.next()
            kb.rmsnorm([xt[:, k, :] for k in range(8)], [xt] * 8, 8, D, gains, G_MIX0, hn, hn)
            st1[tt] = (hn, Ct, St)

        stage1(0)
        for tt in range(n_tiles):
            cols = slice(tt * TT, (tt + 1) * TT)
            own = tt >= n_tiles // 2
            ocols = slice((tt - n_tiles // 2) * TT, (tt - n_tiles // 2 + 1) * TT)
            if tt + 1 < n_tiles:
                stage1(tt + 1)
            hn, Ct, St = st1.pop(tt)
            hnk = [(hn, k) for k in range(8)]
            lat = lat_ring.next()
            for m in range(2):
                ps = psr.next()
                for k in range(8):
                    kb.mm(ps[:], W_in_kv[:, k, m * 128:(m + 1) * 128], hn[:, k, :], k == 0, k == 7,
                          r=[hnk[k]] + kb.wk(W_in_kv), w=[ps])
                kb.copy("act", lat[:, m, :], ps[:], r=[ps], w=[(lat, m)])
            if own:
                latq = lat_ring.next()
                for m in range(3):
                    ps = psr.next()
                    for k in range(8):
                        kb.mm(ps[:], W_in_q[:, k, m * 128:(m + 1) * 128], hn[:, k, :], k == 0, k == 7,
                              r=[hnk[k]] + kb.wk(W_in_q), w=[ps])
                    kb.copy("act", latq[:, m, :], ps[:], r=[ps], w=[(latq, m)])
            latn = latn_ring.next()
            kb.rmsnorm([lat[:, m, :] for m in range(2)], [(lat, m) for m in range(2)], 2, 256, gains, G_KVN, latn, latn)
            if own:
                latnq = latn_ring.next()
                kb.rmsnorm([latq[:, m, :] for m in range(3)], [(latq, m) for m in range(3)], 3, 384, gains, G_QN, latnq, latnq)
            psa = psr.next()
            psb = psr.next()
            for k in range(8):
                kb.mm(psa[0:32, :], W_in_pe[:, k, :], hn[:, k, :], k == 0, k == 7, r=[hnk[k]] + kb.wk(W_in_pe), w=[psa])
            for k in range(8):
                kb.mm(psb[0:32, :], W_in_pesw[:, k, :], hn[:, k, :], k == 0, k == 7, r=[hnk[k]] + kb.wk(W_in_pesw), w=[psb])
            r1 = rp_ring.next()
            r2 = rp_ring.next()
            kb.tt("dve", r1[0:32, :], psa[0:32, :], Ct[0:32, :], ALU.mult, r=[psa, Ct], w=[r1])
            kb.tt("dve", r2[0:32, :], psb[0:32, :], St[0:32, :], ALU.mult, r=[psb, St], w=[r2])
            kpe = ev_ring.next()
            kb.tt("pool", kpe[0:32, :], r1[0:32, :], r2[0:32, :], ALU.add, r=[r1, r2], w=[kpe])
            for h in range(H):
                kb.dma("sp", kT[h, 64:96, cols], kpe[0:32, :], r=[kpe], w=[("kT", h, tt, "pe")])
            for j in range(8):
                ps = psr.next()
                for kc in range(2):
                    kb.mm(ps[:], W_uk[:, kc, j * 128:(j + 1) * 128], latn[:, kc, :], kc == 0, kc == 1,
                          r=[(latn, kc)] + kb.wk(W_uk), w=[ps])
                kn = ev_ring.next()
                kb.copy("act", kn[:], ps[:], r=[ps], w=[kn])
                kb.dma("sp", kT[2 * j, 0:64, cols], kn[0:64, :], r=[kn], w=[("kT", 2 * j, tt, "n")])
                kb.dma("sp", kT[2 * j + 1, 0:64, cols], kn[64:128, :], r=[kn], w=[("kT", 2 * j + 1, tt, "n")])
            for tb in range(4):
                vt = vt_ring.next()
                for nb in range(2):
                    ps = psr.next()
                    for kc in range(2):
                        kb.mm(ps[:], latn[:, kc, tb * 128:(tb + 1) * 128], W_uv[:, kc, nb * 512:(nb + 1) * 512],
                              kc == 0, kc == 1, r=[(latn, kc)] + kb.wk(W_uv), w=[ps])
                    kb.copy("dve", vt[:, nb * 512:(nb + 1) * 512], ps[:], r=[ps], w=[(vt, nb)])
                r0 = tt * TT + tb * 128
                kb.dma("sp", vS[r0:r0 + 128, :], vt[:], r=[(vt, 0), (vt, 1)], w=[("vS", tt, tb)])
            if own:
                latn = latnq
                for j in range(8):
                    ps = psr.next()
                    for kc in range(3):
                        kb.mm(ps[:], W_uq_nope[:, kc, j * 128:(j + 1) * 128], latn[:, kc, :], kc == 0, kc == 2,
                              r=[(latn, kc)] + kb.wk(W_uq_nope), w=[ps])
                    qn = ev_ring.next()
                    kb.copy("act", qn[:], ps[:], r=[ps], w=[qn])
                    kb.dma("sp", qT[2 * j, 0:64, ocols], qn[0:64, :], r=[qn], w=[("qT", 2 * j, tt, "n")])
                    kb.dma("sp", qT[2 * j + 1, 0:64, ocols], qn[64:128, :], r=[qn], w=[("qT", 2 * j + 1, tt, "n")])
                for j in range(4):
                    psa = psr.next()
                    psb = psr.next()
                    for kc in range(3):
                        kb.mm(psa[:], W_uq_pe[:, kc, j * 128:(j + 1) * 128], latn[:, kc, :], kc == 0, kc == 2,
                              r=[(latn, kc)] + kb.wk(W_uq_pe), w=[psa])
                    for kc in range(3):
                        kb.mm(psb[:], W_uq_pesw[:, kc, j * 128:(j + 1) * 128], latn[:, kc, :], kc == 0, kc == 2,
                              r=[(latn, kc)] + kb.wk(W_uq_pesw), w=[psb])
                    r1 = rp_ring.next()
                    r2 = rp_ring.next()
                    kb.tt("dve", r1[:], psa[:], Ct[:], ALU.mult, r=[psa, Ct], w=[r1])
                    kb.tt("dve", r2[:], psb[:], St[:], ALU.mult, r=[psb, St], w=[r2])
                    qpe = ev_ring.next()
                    kb.tt("pool", qpe[:], r1[:], r2[:], ALU.add, r=[r1, r2], w=[qpe])
                    for hh in range(4):
                        kb.dma("sp", qT[4 * j + hh, 64:96, ocols], qpe[32 * hh:32 * hh + 32, :], r=[qpe],
                               w=[("qT", 4 * j + hh, tt, "pe")])


def phase_attn(kb, I):
    p, c = kb.p, kb.c
    gains = c["gains"]
    kT, qT, vS = I["kT"], I["qT"], I["vS"]
    scale = 1.0 / math.sqrt(96.0)
    n_tiles = S // TT
    with p.phase():
        oT = p.sb("oT", [128, 8, T], BF16)
        pss_ring = Ring(p, "pss", 4, [128, TT], F32, psum=True)
        with ExitStack() as tmp:
            p.stack, sv2 = tmp, p.stack
            masks = p.sb("masks", [128, 4, TT], BF16)
            kb.memset("pool", masks[:], 1.0, w=[masks])
            for d in range(4):
                p.op("pool", lambda e, d=d: e.affine_select(masks[:, d, :], masks[:, d, :], [[1, TT]], ALU.is_ge, 0.0,
                                                             base=-128 * d, channel_multiplier=-1),
                     reads=[masks], writes=[masks])
            K_ring = Ring(p, "Kh", 2, [97, S], BF16)
            Q_ring = Ring(p, "Qh", 2, [97, T], BF16)
            V_ring = Ring(p, "Vh", 2, [128, S // 128, 128], BF16)
            for vt in V_ring.tiles:
                kb.memset("pool", vt[:, :, 64:128], 1.0, w=[(vt, "ones")])
            pt_ring = Ring(p, "pt", 8, [128, TT], BF16)
            pso_ring = Ring(p, "pso", 2, [128, TT], F32, psum=True)
            rc_ring = Ring(p, "rc", 2, [64, TT], F32)
            LA = 3
            heads = {}

            def load_head(h):
                Kh = K_ring.next()
                Qh = Q_ring.next()
                Vh = V_ring.next()
                kb.dma("sp", Kh[:], kT[h], w=[Kh])
                kb.dma("sp", Qh[:], qT[h], w=[Qh])
                vsrc = vS[:, h * 64:(h + 1) * 64].rearrange("(kt p) d -> p kt d", p=128)
                for a in range(0, S // 128, 16):
                    kb.dma("sp", Vh[:, a:a + 16, 0:64], vsrc[:, a:a + 16, :], w=[(Vh, "v", a)])
                vkeys = [(Vh, "v", a) for a in range(0, S // 128, 16)] + [(Vh, "ones")]
                heads[h] = (Kh, Qh, Vh, vkeys)

            tiles = []
            for h in range(kb.nheads):
                for qt in range(T // TT):
                    nk = (T + TT * (qt + 1)) // 128
                    for kt in range(nk):
                        tiles.append((h, qt, kt, nk))
            load_head(0)
            pts = {}
            psos = {}
            for i in range(len(tiles) + LA):
                if i < len(tiles):
                    h, qt, kt, nk = tiles[i]
                    Kh, Qh, Vh, vkeys = heads[h]
                    pss = pss_ring.next()
                    kb.mm(pss[:], Kh[:, kt * 128:(kt + 1) * 128], Qh[:, qt * TT:(qt + 1) * TT], True, True,
                          r=[Kh, Qh], w=[pss])
                    pt = pt_ring.next()
                    kb.act(pt[:], pss[:], AF.Exp, r=[pss], w=[pt], scale=scale)
                    dd = kt - (nk - 4)
                    if dd >= 0:
                        kb.tt("pool", pt[:], pt[:], masks[:, dd, :], ALU.mult, r=[pt, masks], w=[pt])
                    pts[i] = pt
                if i >= LA:
                    j = i - LA
                    h, qt, kt, nk = tiles[j]
                    Kh, Qh, Vh, vkeys = heads[h]
                    if kt == 0:
                        psos[(h, qt)] = pso_ring.next()
                    pso = psos[(h, qt)]
                    pt = pts.pop(j)
                    kb.mm(pso[:], Vh[:, kt, :], pt[:], kt == 0, kt == nk - 1, r=vkeys + [pt], w=[pso])
                    if qt == 0 and kt == 0 and h + 1 < kb.nheads:
                        load_head(h + 1)
                    if kt == nk - 1:
                        rc = rc_ring.next()
                        kb.recip(rc[:], pso[64:128, :], r=[pso], w=[rc])
                        po = (h % 2) * 64
                        kb.tt("dve", oT[po:po + 64, h // 2, qt * TT:(qt + 1) * TT], pso[0:64, :], rc[:], ALU.mult,
                              r=[pso, rc], w=[("oT", h // 2, qt, h % 2)])
            p.flush()
            p.stack = sv2
        W_o = p.sb("W_o", [128, 8, D], BF16)
        kb.load_w(W_o, I["w_o"], D, D)
        c["ps_ring"] = pss_ring
        norm_rings(kb, p)
        x_ring = Ring(p, "xt", 2, [128, 8, TT], F32)
        h1_ring = Ring(p, "h1", 2, [128, 8, TT], F32)
        hn_ring = Ring(p, "hn", 2, [128, 8, TT], BF16)
        xTv = I["xT"].rearrange("(k p) t -> p k t", p=128)
        h1Tv = I["h1T"].rearrange("(k p) t -> p k t", p=128)
        hn2Tv = I["hn2T"].rearrange("(k p) t -> p k t", p=128)
        for qt in range(T // TT):
            cols = slice(qt * TT, (qt + 1) * TT)
            xt = x_ring.next()
            kb.dma("sp", xt[:], xTv[:, :, T + qt * TT:T + (qt + 1) * TT], w=[xt])
            h1 = h1_ring.next()
            for m in range(8):
                ps = pss_ring.next()
                for k in range(8):
                    kb.mm(ps[:], W_o[:, k, m * 128:(m + 1) * 128], oT[:, k, cols], k == 0, k == 7,
                          r=[("oT", k, qt, 0), ("oT", k, qt, 1)] + kb.wk(W_o), w=[ps])
                kb.tt("dve", h1[:, m, :], ps[:], xt[:, m, :], ALU.add, r=[ps, xt], w=[(h1, m)])
            kb.dma("sp", h1Tv[:, :, cols], h1[:], r=[(h1, m) for m in range(8)], w=[("h1T", qt)])
            hn = hn_ring.next()
            kb.rmsnorm([h1[:, k, :] for k in range(8)], [(h1, k) for k in range(8)], 8, D, gains, G_FFN0, hn, hn)
            kb.dma("sp", hn2Tv[:, :, cols], hn[:], r=[(hn, k) for k in range(8)], w=[("hn2T", qt)])


def ffn_phase(kb, experts, FF, hnT, accT, outT, comb=None, final_norm=None):
    p, c = kb.p, kb.c
    nff = FF // 128
    BLK = 4
    blocks = [(b, min(BLK, nff - b)) for b in range(0, nff, BLK)]
    TS = 2048
    nq = TS // TT
    with p.phase():
        hn = p.sb("f_hn", [128, 8, TS], BF16)
        acc = p.sb("f_acc", [128, 8, TS], F32)
        psg_ring = Ring(p, "psg", 2, [128, TT], F32, psum=True)
        psu_ring = Ring(p, "psu", 2, [128, TT], F32, psum=True)
        psd_ring = Ring(p, "psd", 3, [128, TT], F32, psum=True)
        sg_ring = Ring(p, "sg", 3, [128, TT], F32)
        a_ring = Ring(p, "a", 2, [128, BLK, TT], BF16)
        wg_ring = Ring(p, "wg", 3, [128, 8, BLK * 128], BF16)
        wu_ring = Ring(p, "wu", 3, [128, 8, BLK * 128], BF16)
        wd_ring = Ring(p, "wd", 3, [128, BLK, D], BF16)
        hnv = hnT.rearrange("(k p) t -> p k t", p=128)
        accv = accT.rearrange("(k p) t -> p k t", p=128)
        outv = outT.rearrange("(k p) t -> p k t", p=128)
        def emit_down(a, Wd, nb, qt, l):
            for m in range(8):
                psd = psd_ring.next()
                for f in range(nb):
                    kb.mm(psd[:], Wd[:, f, m * 128:(m + 1) * 128], a[:, f, :], f == 0, f == nb - 1,
                          r=[(a, f)] + kb.wk(Wd), w=[psd])
                kb.tt("dve", acc[:, m, l], psd[:], acc[:, m, l], ALU.add, r=[psd, (acc, qt, m)],
                      w=[(acc, qt, m)])

        pend = None
        for sup in range(T // TS):
            for qt in range(nq):
                g = slice(sup * TS + qt * TT, sup * TS + (qt + 1) * TT)
                l = slice(qt * TT, (qt + 1) * TT)
                kb.dma("sp", hn[:, :, l], hnv[:, :, g], w=[(hn, qt)])
                kb.dma("sp", acc[:, :, l], accv[:, :, g], w=[(acc, qt, m) for m in range(8)])
            for ei, (wg_d, wu_d, wd_d) in enumerate(experts):
                for (b0, nb) in blocks:
                    Wg = wg_ring.next()
                    Wu = wu_ring.next()
                    Wd = wd_ring.next()
                    kb.load_w(Wg, wg_d, D, FF, m0=b0 * 128, mw=nb * 128)
                    kb.load_w(Wu, wu_d, D, FF, m0=b0 * 128, mw=nb * 128)
                    kb.load_w(Wd, wd_d[b0 * 128:(b0 + nb) * 128, :], nb * 128, D)
                    for qt in range(nq):
                        l = slice(qt * TT, (qt + 1) * TT)
                        a = a_ring.next()
                        for f in range(nb):
                            psg = psg_ring.next()
                            psu = psu_ring.next()
                            for k in range(8):
                                kb.mm(psg[:], Wg[:, k, f * 128:(f + 1) * 128], hn[:, k, l], k == 0, k == 7,
                                      r=[(hn, qt)] + kb.wk(Wg), w=[psg])
                            for k in range(8):
                                kb.mm(psu[:], Wu[:, k, f * 128:(f + 1) * 128], hn[:, k, l], k == 0, k == 7,
                                      r=[(hn, qt)] + kb.wk(Wu), w=[psu])
                            sg = sg_ring.next()
                            kb.act(sg[:], psg[:], AF.Silu, r=[psg], w=[sg])
                            if comb is None:
                                kb.tt("dve", a[:, f, :], psu[:], sg[:], ALU.mult, r=[psu, sg], w=[(a, f)])
                            else:
                                bc = comb(sup, qt, ei)
                                kb.tt("pool", sg[:], sg[:], bc[0], ALU.mult, r=[sg, bc[1]], w=[sg])
                                kb.tt("dve", a[:, f, :], psu[:], sg[:], ALU.mult, r=[psu, sg], w=[(a, f)])
                        emit_down(a, Wd, nb, qt, l)
            if pend is not None:
                emit_down(*pend)
                pend = None
            for qt in range(nq):
                g = slice(sup * TS + qt * TT, sup * TS + (qt + 1) * TT)
                l = slice(qt * TT, (qt + 1) * TT)
                if final_norm is None:
                    kb.dma("sp", outv[:, :, g], acc[:, :, l], r=[(acc, qt, m) for m in range(8)], w=[("fout", sup, qt)])
                else:
                    final_norm(sup, qt, acc, l, outv, g)


def make_inputs_l0(kb):
    I = {}
    I["xT"] = kb.din("xT", [D, S])
    I["pos128"] = kb.din("pos128", [128, S], I32)
    I["kbias"] = kb.din("kbias", [128, S // 128])
    I["gains_d"] = kb.din("gains", [128, 48])
    I["cst_d"] = kb.din("cst", [128, 8])
    for name, shp in (("w_in_q", [D, 384]), ("w_in_kv", [D, 256]), ("w_in_pe", [D, 32]), ("w_in_pesw", [D, 32]),
                      ("w_uq_nope", [384, 1024]), ("w_uq_pe", [384, 512]), ("w_uq_pesw", [384, 512]),
                      ("w_uk", [256, 1024]), ("w_uv", [256, 1024]), ("w_o", [D, D]),
                      ("ffn_wg", [D, 2816]), ("ffn_wu", [D, 2816]), ("ffn_wd", [2816, D])):
        I[name] = kb.din(name, shp)
    return I


def setup_consts(kb, I):
    p, c = kb.p, kb.c
    c["gains"] = p.sb("gains", [128, 48], F32)
    c["cst"] = p.sb("cst", [128, 8], F32)
    c["ones_bf"] = p.sb("ones_bf", [128, 128], BF16)
    kb.dma("sp", c["gains"][:], I["gains_d"], w=[c["gains"]])
    kb.dma("sp", c["cst"][:], I["cst_d"], w=[c["cst"]])
    kb.memset("pool", c["ones_bf"][:], 1.0, w=[c["ones_bf"]])


def build_l0(stop_after="all", nheads=H):
    nc = bass.Bass("TRN2", target_bir_lowering=False)
    kb = KB(nc)
    kb.c = {}
    kb.nheads = nheads
    p = kb.p
    I = make_inputs_l0(kb)
    dbg = stop_after != "all"
    mk = (lambda n, s, d: kb.dout(n, s, d)) if stop_after == "kv" else (lambda n, s, d: kb.dscr(n, s, d))
    I["kT"] = mk("kT", [H, 97, S], BF16)
    I["qT"] = mk("qT", [H, 97, T], BF16)
    I["vS"] = mk("vS", [S, 1024], BF16)
    mk = (lambda n, s, d: kb.dout(n, s, d)) if stop_after == "attn" else (lambda n, s, d: kb.dscr(n, s, d))
    I["h1T"] = mk("h1T", [D, T], F32)
    I["hn2T"] = mk("hn2T", [D, T], BF16)
    I["h2T"] = kb.dout("h2T", [D, T], F32)
    setup_consts(kb, I)
    phase_kvq(kb, I)
    if stop_after != "kv":
        phase_attn(kb, I)
        if stop_after != "attn":
            ffn_phase(kb, [(I["ffn_wg"], I["ffn_wu"], I["ffn_wd"])], 2816, I["hn2T"], I["h1T"], I["h2T"])
    p.finish()
    return nc, kb


def host_inputs_l0(inp, core):
    b, hf = core // 2, core % 2
    x = inp["x"][b]
    pos = inp["positions"][b]
    xT = np.zeros((D, S), np.float32)
    posl = np.zeros((S,), np.int32)
    kbias = np.zeros((S,), np.float32)
    if hf == 1:
        xT[:] = x.T
        posl[:] = pos
    else:
        xT[:, T:] = x[:T].T
        posl[T:] = pos[:T]
        kbias[:T] = NEGBIG
    m = {}
    m["xT"] = xT
    m["pos128"] = np.ascontiguousarray(np.broadcast_to(posl[None, :], (128, S)))
    m["kbias"] = np.ascontiguousarray(kbias.reshape(128, S // 128))
    return m


def host_weights_l0(inp):
    m = {}
    g = np.zeros((128, 48), np.float32)

    def col(v):
        return v.reshape(-1, 128).T
    g[:, 0:8] = col(inp["mix_norm"][0])
    g[:, 8:16] = col(inp["ffn_norm"][0])
    g[:, 16:24] = col(inp["mix_norm"][1])
    g[:, 24:32] = col(inp["ffn_norm"][1])
    g[:, 32:40] = col(inp["final_norm"])
    g[:, 40:43] = col(inp["mla_q_norm"][0])
    g[:, 43:45] = col(inp["mla_kv_norm"][0])
    m["gains"] = g
    cst = np.zeros((128, 8), np.float32)
    r = np.arange(128)
    cst[:, 0] = (10000.0 ** (-(np.arange(0, 32, 2, dtype=np.float32)) / 32.0)).astype(np.float32)[r % 16]
    cst[:, 1] = np.where((r % 32) < 16, -1.0, 1.0)
    cst[:, 2] = EPS
    m["cst"] = cst
    w_in = inp["mla_w_in"][0]
    m["w_in_q"] = np.ascontiguousarray(w_in[:, :384])
    m["w_in_kv"] = np.ascontiguousarray(w_in[:, 384:640])
    pe = w_in[:, 640:672]
    m["w_in_pe"] = np.ascontiguousarray(pe)
    m["w_in_pesw"] = np.ascontiguousarray(np.concatenate([pe[:, 16:], pe[:, :16]], 1))
    wq = inp["mla_w_uq"][0].reshape(384, 16, 96)
    m["w_uq_nope"] = np.ascontiguousarray(wq[:, :, :64].reshape(384, 1024))
    qpe = wq[:, :, 64:]
    m["w_uq_pe"] = np.ascontiguousarray(qpe.reshape(384, 512))
    m["w_uq_pesw"] = np.ascontiguousarray(np.concatenate([qpe[:, :, 16:], qpe[:, :, :16]], 2).reshape(384, 512))
    wkv = inp["mla_w_ukv"][0].reshape(256, 16, 128)
    m["w_uk"] = np.ascontiguousarray(wkv[:, :, :64].reshape(256, 1024))
    m["w_uv"] = np.ascontiguousarray(wkv[:, :, 64:].reshape(256, 1024))
    m["w_o"] = np.ascontiguousarray(inp["mla_w_o"][0])
    m["ffn_wg"] = np.ascontiguousarray(inp["ffn_w_gate"][0])
    m["ffn_wu"] = np.ascontiguousarray(inp["ffn_w_up"][0])
    m["ffn_wd"] = np.ascontiguousarray(inp["ffn_w_down"][0])
    return m

NCH = T // 8
XW = NCH + 2


def sincos(kb, p, ang, out_sin, out_cos, W, tag):
    t1 = p.sb(tag + "t1", [128, W], F32)
    t2 = p.sb(tag + "t2", [128, W], F32)
    ki = p.sb(tag + "ki", [128, W], I32)
    for (out, shift) in ((out_sin, 0.0), (out_cos, math.pi / 2)):
        kb.ts("dve", t1[:], ang[:], shift, ALU.add, r=[ang], w=[t1])
        kb.ts("dve", t2[:], t1[:], 1.0 / TWO_PI, ALU.mult, r=[t1], w=[t2])
        kb.copy("dve", ki[:], t2[:], r=[t2], w=[ki])
        kb.copy("dve", t2[:], ki[:], r=[ki], w=[t2])
        kb.stt("dve", t1[:], t2[:], -TWO_PI, t1[:], ALU.mult, ALU.add, r=[t1, t2], w=[t1])
        kb.ts("dve", t2[:], t1[:], math.pi, ALU.is_gt, r=[t1], w=[t2])
        kb.stt("dve", t1[:], t2[:], -TWO_PI, t1[:], ALU.mult, ALU.add, r=[t1, t2], w=[t1])
        kb.ts("dve", t2[:], t1[:], -math.pi, ALU.is_lt, r=[t1], w=[t2])
        kb.stt("dve", t1[:], t2[:], TWO_PI, t1[:], ALU.mult, ALU.add, r=[t1, t2], w=[t1])
        kb.ts("dve", t1[:], t1[:], math.pi, ALU.min, r=[t1], w=[t1], s2=-math.pi, op1=ALU.max)
        kb.act(out[:], t1[:], AF.Sin, r=[t1], w=[out])


def cmul(kb, p, outr, outi, ar, ai, br, bi, tmp, keys_r, keys_w):
    kb.tt("dve", outr, ar, br, ALU.mult, r=keys_r, w=keys_w[0:1])
    kb.tt("dve", tmp, ai, bi, ALU.mult, r=keys_r, w=[("tmp", _key(keys_w[0]))])
    kb.tt("dve", outr, outr, tmp, ALU.subtract, r=keys_w[0:1] + [("tmp", _key(keys_w[0]))], w=keys_w[0:1])
    kb.tt("dve", outi, ar, bi, ALU.mult, r=keys_r, w=keys_w[1:2])
    kb.tt("dve", tmp, ai, br, ALU.mult, r=keys_r + keys_w[0:1], w=[("tmp", _key(keys_w[0]))])
    kb.tt("dve", outi, outi, tmp, ALU.add, r=keys_w[1:2] + [("tmp", _key(keys_w[0]))], w=keys_w[1:2])


def s5_coefs(kb, p, lr_d, li_d, ldt_d, W, tag):
    lr = p.sb(tag + "lr", [128, W], F32)
    li = p.sb(tag + "li", [128, W], F32)
    dt = p.sb(tag + "dt", [128, W], F32)
    kb.dma("sp", lr[:], lr_d, w=[lr])
    kb.dma("sp", li[:], li_d, w=[li])
    kb.dma("sp", dt[:], ldt_d, w=[dt])
    kb.act(dt[:], dt[:], AF.Exp, r=[dt], w=[dt])
    mag = p.sb(tag + "mag", [128, W], F32)
    kb.tt("dve", mag[:], lr[:], dt[:], ALU.mult, r=[lr, dt], w=[mag])
    kb.act(mag[:], mag[:], AF.Exp, r=[mag], w=[mag])
    th = p.sb(tag + "th", [128, W], F32)
    kb.tt("dve", th[:], li[:], dt[:], ALU.mult, r=[li, dt], w=[th])
    sn = p.sb(tag + "sn", [128, W], F32)
    cs = p.sb(tag + "cs", [128, W], F32)
    sincos(kb, p, th, sn, cs, W, tag)
    apr = p.sb(tag + "apr", [128, 9, W], F32)
    api = p.sb(tag + "api", [128, 9, W], F32)
    kb.memset("dve", apr[:, 0, :], 1.0, w=[(apr, 0)])
    kb.memset("dve", api[:, 0, :], 0.0, w=[(api, 0)])
    kb.tt("dve", apr[:, 1, :], mag[:], cs[:], ALU.mult, r=[mag, cs], w=[(apr, 1)])
    kb.tt("dve", api[:, 1, :], mag[:], sn[:], ALU.mult, r=[mag, sn], w=[(api, 1)])
    tmp = p.sb(tag + "tmp", [128, W], F32)
    for n in range(2, 9):
        cmul(kb, p, apr[:, n, :], api[:, n, :], apr[:, n - 1, :], api[:, n - 1, :], apr[:, 1, :], api[:, 1, :], tmp[:],
             [(apr, n - 1), (api, n - 1), (apr, 1), (api, 1)], [(apr, n), (api, n)])
    den = p.sb(tag + "den", [128, W], F32)
    t2 = p.sb(tag + "t2", [128, W], F32)
    kb.tt("dve", den[:], lr[:], lr[:], ALU.mult, r=[lr], w=[den])
    kb.tt("dve", t2[:], li[:], li[:], ALU.mult, r=[li], w=[t2])
    kb.tt("dve", den[:], den[:], t2[:], ALU.add, r=[den, t2], w=[den])
    kb.recip(den[:], den[:], r=[den], w=[den])
    am1 = p.sb(tag + "am1", [128, W], F32)
    kb.ts("dve", am1[:], apr[:, 1, :], -1.0, ALU.add, r=[(apr, 1)], w=[am1])
    gr = p.sb(tag + "gr", [128, W], F32)
    gi = p.sb(tag + "gi", [128, W], F32)
    kb.tt("dve", gr[:], am1[:], lr[:], ALU.mult, r=[am1, lr], w=[gr])
    kb.tt("dve", t2[:], api[:, 1, :], li[:], ALU.mult, r=[(api, 1), li], w=[t2])
    kb.tt("dve", gr[:], gr[:], t2[:], ALU.add, r=[gr, t2], w=[gr])
    kb.tt("dve", gr[:], gr[:], den[:], ALU.mult, r=[gr, den], w=[gr])
    kb.tt("dve", gi[:], api[:, 1, :], lr[:], ALU.mult, r=[(api, 1), lr], w=[gi])
    kb.tt("dve", t2[:], am1[:], li[:], ALU.mult, r=[am1, li], w=[t2])
    kb.tt("dve", gi[:], gi[:], t2[:], ALU.subtract, r=[gi, t2], w=[gi])
    kb.tt("dve", gi[:], gi[:], den[:], ALU.mult, r=[gi, den], w=[gi])
    wgr = p.sb(tag + "wgr", [128, 8, W], F32)
    wgi = p.sb(tag + "wgi", [128, 8, W], F32)
    for n in range(8):
        cmul(kb, p, wgr[:, n, :], wgi[:, n, :], apr[:, n, :], api[:, n, :], gr[:], gi[:], tmp[:],
             [(apr, n), (api, n), gr, gi], [(wgr, n), (wgi, n)])
    return apr, api, wgr, wgi


def bc_last(ap2, n):
    return AP(ap2.tensor, ap2.offset, [list(ap2.ap[0]), list(ap2.ap[1]), [0, n]])


def bc_mid(ap2, n):
    return AP(ap2.tensor, ap2.offset, [list(ap2.ap[0]), [0, n], list(ap2.ap[1])])


def make_inputs_l1(kb, first, skip=(), moe=True):
    I = {}
    if "gains" not in skip:
        I["gains_d"] = kb.din("gains", [128, 48])
        I["cst_d"] = kb.din("cst", [128, 8])
    for name, shp in (("s5_w_in", [D, D]), ("s5_w_glu", [D, 2 * D]),
                      ("lam_re_pm", [128, 32]), ("lam_im_pm", [128, 32]), ("ldt_pm", [128, 32]),
                      ("lam_re_fm", [128, 1024]), ("lam_im_fm", [128, 1024]), ("ldt_fm", [128, 1024]),
                      ("Bp_re", [128, 1024]), ("Bp_im", [128, 1024]), ("Ct_re", [128, 1024]), ("Ct_im", [128, 1024]),
                      ("Bt_re", [128, 1024]), ("Bt_im", [128, 1024]), ("d_col", [128, 8]),
                      ("x0", [128, 64]), ("sel", [8, 1024]), ("w_router", [128, 64])):
        if name not in skip:
            I[name] = kb.din(name, shp)
    for e in range(8 if moe else 0):
        I["moe_wg%d" % e] = kb.din("moe_wg%d" % e, [D, 3584])
        I["moe_wu%d" % e] = kb.din("moe_wu%d" % e, [D, 3584])
        I["moe_wd%d" % e] = kb.din("moe_wd%d" % e, [3584, D])
    return I


def phase_s5(kb, I, passes):
    p, c = kb.p, kb.c
    gains = c["gains"]
    uT, ygT = I["uT"], I["ygT"]
    with p.phase():
        psr = c["ps_ring"] = Ring(p, "ps", 4, [128, TT], F32, psum=True)
        norm_rings(kb, p)
        W_s = p.sb("W_s5in", [128, 8, D], BF16)
        kb.load_w(W_s, I["s5_w_in"], D, D)
        x_ring = Ring(p, "xt", 2, [128, 8, TT], F32)
        hn_ring = Ring(p, "hn", 2, [128, 8, TT], BF16)
        u_ring = Ring(p, "ut", 2, [128, 8, TT], BF16)
        h2v = I["h2T"].rearrange("(k p) t -> p k t", p=128)
        uTv = uT.rearrange("(k p) t -> p k t", p=128)
        for qt in range(T // TT):
            cols = slice(qt * TT, (qt + 1) * TT)
            xt = x_ring.next()
            kb.dma("sp", xt[:], h2v[:, :, cols], w=[xt])
            hn = hn_ring.next()
            kb.rmsnorm([xt[:, k, :] for k in range(8)], [xt] * 8, 8, D, gains, G_MIX1, hn, hn)
            ut = u_ring.next()
            for m in range(8):
                ps = psr.next()
                for k in range(8):
                    kb.mm(ps[:], W_s[:, k, m * 128:(m + 1) * 128], hn[:, k, :], k == 0, k == 7,
                          r=[(hn, k)] + kb.wk(W_s), w=[ps])
                kb.copy("act", ut[:, m, :], ps[:], r=[ps], w=[(ut, m)])
            kb.dma("sp", uTv[:, :, cols], ut[:], r=[(ut, m) for m in range(8)], w=[("uT", qt)])
    with p.phase():
        Win = p.sb("Win", [128, 8, 8, 2, 128], BF16)
        Kt = p.sb("Kt", [128, 8, 8, 128], BF16)
        Cf = p.sb("Cf", [128, 8, 32, 2, 32], BF16)
        PQ = p.sb("PQ", [128, 2, 2, 32], F32)
        ident = p.sb("ident", [128, 128], F32)
        kb.memset("pool", ident[:], 0.0, w=[ident])
        p.op("pool", lambda e: e.affine_select(ident[:], ident[:], [[1, 128]], ALU.not_equal, 1.0,
                                               base=0, channel_multiplier=-1), reads=[ident], writes=[ident])
        for blk in range(4):
          with ExitStack() as tmp:
            p.stack, sv2 = tmp, p.stack
            cs_ = slice(blk * 256, (blk + 1) * 256)
            apr, api, wgr, wgi = s5_coefs(kb, p, I["lam_re_fm"][:, cs_], I["lam_im_fm"][:, cs_], I["ldt_fm"][:, cs_], 256, "fm%d" % blk)
            Btr = p.sb("Btr", [128, 256], F32)
            Bti = p.sb("Bti", [128, 256], F32)
            kb.dma("sp", Btr[:], I["Bt_re"][:, cs_], w=[Btr])
            kb.dma("sp", Bti[:], I["Bt_im"][:, cs_], w=[Bti])
            w1 = p.sb("w1", [128, 256], F32)
            w2 = p.sb("w2", [128, 256], F32)
            for j in range(8):
                n = 7 - j
                kb.tt("dve", w1[:], wgr[:, n, :], Btr[:], ALU.mult, r=[(wgr, n), Btr], w=[w1])
                kb.tt("dve", w2[:], wgi[:, n, :], Bti[:], ALU.mult, r=[(wgi, n), Bti], w=[w2])
                kb.tt("dve", Win[:, j, 2 * blk:2 * blk + 2, 0, :], w1[:].rearrange("p (a b) -> p a b", a=2),
                      w2[:].rearrange("p (a b) -> p a b", a=2), ALU.subtract, r=[w1, w2], w=[(Win, j, 0)])
                kb.tt("dve", w1[:], wgr[:, n, :], Bti[:], ALU.mult, r=[(wgr, n), Bti], w=[w1])
                kb.tt("dve", w2[:], wgi[:, n, :], Btr[:], ALU.mult, r=[(wgi, n), Btr], w=[w2])
                kb.tt("dve", Win[:, j, 2 * blk:2 * blk + 2, 1, :], w1[:].rearrange("p (a b) -> p a b", a=2),
                      w2[:].rearrange("p (a b) -> p a b", a=2), ALU.add, r=[w1, w2], w=[(Win, j, 1)])
            p.flush()
            p.stack = sv2
        with ExitStack() as tmp:
            p.stack, sv2 = tmp, p.stack
            apr, api, wgr, wgi = s5_coefs(kb, p, I["lam_re_pm"], I["lam_im_pm"], I["ldt_pm"], 32, "pm")
            Bpr = p.sb("Bpr", [128, 32, 32], F32)
            Bpi = p.sb("Bpi", [128, 32, 32], F32)
            Ctr = p.sb("Ctr", [128, 32, 32], F32)
            Cti = p.sb("Cti", [128, 32, 32], F32)
            for (t_, n_) in ((Bpr, "Bp_re"), (Bpi, "Bp_im"), (Ctr, "Ct_re"), (Cti, "Ct_im")):
                kb.dma("sp", t_[:], I[n_].rearrange("p (a b) -> p a b", a=32), w=[t_])
            dcol = p.sb("dcol", [128, 8], F32)
            kb.dma("sp", dcol[:], I["d_col"], w=[dcol])
            kb.copy("dve", PQ[:, 0, 0, :], apr[:, 8, :], r=[(apr, 8)], w=[(PQ, 0)])
            kb.copy("dve", PQ[:, 0, 1, :], apr[:, 8, :], r=[(apr, 8)], w=[(PQ, 1)])
            kb.ts("dve", PQ[:, 1, 0, :], api[:, 8, :], -1.0, ALU.mult, r=[(api, 8)], w=[(PQ, 2)])
            kb.copy("dve", PQ[:, 1, 1, :], api[:, 8, :], r=[(api, 8)], w=[(PQ, 3)])
            c1 = p.sb("c1", [128, 32, 32], F32)
            c2 = p.sb("c2", [128, 32, 32], F32)
            for s in range(8):
                arb = bc_last(apr[:, s + 1, :], 32)
                aib = bc_last(api[:, s + 1, :], 32)
                kb.tt("dve", c1[:], Ctr[:], arb, ALU.mult, r=[Ctr, (apr, s + 1)], w=[c1])
                kb.tt("dve", c2[:], Cti[:], aib, ALU.mult, r=[Cti, (api, s + 1)], w=[c2])
                kb.tt("dve", Cf[:, s, :, 0, :], c1[:], c2[:], ALU.subtract, r=[c1, c2], w=[(Cf, s, 0)])
                kb.tt("dve", c1[:], Ctr[:], aib, ALU.mult, r=[Ctr, (api, s + 1)], w=[c1])
                kb.tt("dve", c2[:], Cti[:], arb, ALU.mult, r=[Cti, (apr, s + 1)], w=[c2])
                kb.stt("dve", Cf[:, s, :, 1, :], c1[:], -1.0, c2[:], ALU.mult, ALU.subtract, r=[c1, c2], w=[(Cf, s, 1)])
            WBr = p.sb("WBr", [128, 32, 32], BF16)
            WBi = p.sb("WBi", [128, 32, 32], BF16)
            Ctrb = p.sb("Ctrb", [128, 32, 32], BF16)
            Ctib = p.sb("Ctib", [128, 32, 32], BF16)
            kb.copy("dve", Ctrb[:], Ctr[:], r=[Ctr], w=[Ctrb])
            kb.copy("dve", Ctib[:], Cti[:], r=[Cti], w=[Ctib])
            kst = p.sb("kst", [128, 128], F32)
            psk_ring = Ring(p, "psk", 2, [128, 32], F32, psum=True)
            for tau in range(8):
                wrb = bc_last(wgr[:, tau, :], 32)
                wib = bc_last(wgi[:, tau, :], 32)
                kb.tt("dve", c1[:], Bpr[:], wrb, ALU.mult, r=[Bpr, (wgr, tau)], w=[c1])
                kb.tt("dve", c2[:], Bpi[:], wib, ALU.mult, r=[Bpi, (wgi, tau)], w=[c2])
                kb.tt("dve", WBr[:], c1[:], c2[:], ALU.subtract, r=[c1, c2], w=[WBr])
                kb.tt("dve", c1[:], Bpi[:], wrb, ALU.mult, r=[Bpi, (wgr, tau)], w=[c1])
                kb.tt("dve", c2[:], Bpr[:], wib, ALU.mult, r=[Bpr, (wgi, tau)], w=[c2])
                kb.stt("dve", WBi[:], c1[:], -1.0, c2[:], ALU.mult, ALU.subtract, r=[c1, c2], w=[WBi])
                for kk in range(8):
                    psk = psk_ring.next()
                    for q in range(4):
                        k = 4 * kk + q
                        p.op("pe", lambda e, o=psk[32 * q:32 * q + 32, :], l=WBr[:, k, :], rr=Ctrb[:, k, :], q=q:
                             e.matmul(o, l, rr, start=True, stop=False, tile_position=(0, 32 * q)),
                             reads=[WBr, Ctrb], writes=[psk])
                        p.op("pe", lambda e, o=psk[32 * q:32 * q + 32, :], l=WBi[:, k, :], rr=Ctib[:, k, :], q=q:
                             e.matmul(o, l, rr, start=False, stop=True, tile_position=(0, 32 * q)),
                             reads=[WBi, Ctib], writes=[psk])
                    if tau == 0:
                        kb.ts("dve", kst[:], ident[:], dcol[:, kk:kk + 1], ALU.mult, r=[ident, dcol], w=[kst])
                    else:
                        kb.memset("dve", kst[:], 0.0, w=[kst])
                    for q in range(4):
                        sl = slice(32 * q, 32 * q + 32)
                        kb.tt("dve", kst[sl, sl], kst[sl, sl], psk[sl, :], ALU.add, r=[kst, psk], w=[kst])
                    kb.copy("act", Kt[:, tau, kk, :], kst[:], r=[kst], w=[(Kt, tau, kk)])
            p.flush()
            p.stack = sv2
        X = p.sb("X", [128, 2, 32, XW], BF16)
        pse_ring = Ring(p, "pse", 3, [128, NCH], F32, psum=True)
        u_ring = Ring(p, "uc", 2, [128, T], BF16)
        uTv = uT.rearrange("(k p) t -> p k t", p=128)
        for kk in range(8):
            uc = u_ring.next()
            kb.dma("sp", uc[:], uTv[:, kk, :], w=[uc])
            ucv = uc[:].rearrange("p (c j) -> p j c", j=8)
            for q in range(4):
                for ri in range(2):
                    pse = pse_ring.next()
                    for j in range(8):
                        p.op("pe", lambda e, o=pse[:], l=Win[32 * q:32 * q + 32, j, kk, ri, :], rr=ucv[32 * q:32 * q + 32, j, :],
                             j=j, q=q: e.matmul(o, l, rr, start=(j == 0), stop=(j == 7), tile_position=(32 * q, 0)),
                             reads=[uc, (Win, j, ri)], writes=[pse])
                    kb.copy("act" if ri == 0 else "dve", X[:, ri, 4 * kk + q, 1:1 + NCH], pse[:], r=[pse], w=[("X", ri, 4 * kk + q)])
        p.flush()
        s_ring = Ring(p, "st", 3, [128, 2, 32], F32)
        t_ring = Ring(p, "tt", 3, [128, 2, 32], F32)
        t2_ring = Ring(p, "t2", 3, [128, 2, 32], F32)
        x0 = p.sb("x0", [128, 2, 32], F32)
        for pi, mode in enumerate(passes):
            Sc = s_ring.next()
            if mode == "zero":
                kb.memset("dve", Sc[:], 0.0, w=[Sc])
            elif mode == "cc":
                kb.dma("sp", x0[:], I["xg"][0:128, :].rearrange("p (a b) -> p a b", a=2), r=["xg"], w=[x0])
                kb.ts("dve", Sc[:], x0[:], c["cst"][:, 3:4], ALU.mult, r=[x0, c["cst"]], w=[Sc])
            else:
                kb.dma("sp", x0[:], I["x0"].rearrange("p (a b) -> p a b", a=2), w=[x0])
                kb.copy("dve", Sc[:], x0[:], r=[x0], w=[Sc])
            final = (pi == len(passes) - 1)
            if final:
                kb.copy("act", X[:, :, :, 0], Sc[:], r=[Sc], w=[("Xc", 0)])
            for cc in range(NCH):
                Sa = Sc[:]
                Ssw = AP(Sa.tensor, Sa.offset + 32, [list(Sa.ap[0]), [-32, 2], [1, 32]])
                t1 = t_ring.next()
                t2 = t2_ring.next()
                Sn = s_ring.next()
                kb.tt("dve", t1[:], PQ[:, 0, :, :], Sc[:], ALU.mult, r=[Sc], w=[t1])
                kb.tt("pool", t2[:], PQ[:, 1, :, :], Ssw, ALU.mult, r=[Sc], w=[t2])
                kb.tt("dve", t1[:], t1[:], X[:, :, :, 1 + cc], ALU.add, r=[t1, ("Xc", 1 + cc)], w=[t1])
                kb.tt("dve", Sn[:], t1[:], t2[:], ALU.add, r=[t1, t2], w=[Sn])
                if final:
                    kb.copy("act", X[:, :, :, 1 + cc], Sn[:], r=[Sn], w=[("Xc", 1 + cc)])
                Sc = Sn
            if I.get("xfin") is not None and len(passes) == 1:
                kb.dma("sp", I["xfin"].rearrange("p (a b) -> p a b", a=2), Sc[:], r=[Sc], w=["xfin"])
            if not final and passes[pi + 1] == "cc":
                kb.dma("sp", I["xi"].rearrange("p (a b) -> p a b", a=2), Sc[:], r=[Sc], w=["xi"])
                p.cc("AllGather", ALU.bypass, [[0, 1], [2, 3], [4, 5], [6, 7]], I["xi"], I["xg"], reads=["xi"], writes=["xg"])
        p.flush()
        if passes == ["zero", "fix"][:1] and I.get("s5_fix"):
            kb.dma("sp", I["xi"].rearrange("p (a b) -> p a b", a=2), Sc[:], r=[], w=["xi"])
            p.cc("AllGather", ALU.bypass, [[0, 1], [2, 3], [4, 5], [6, 7]], I["xi"], I["xg"], reads=["xi"], writes=["xg"])
            kb.dma("sp", x0[:], I["xg"][0:128, :].rearrange("p (a b) -> p a b", a=2), r=["xg"], w=[x0])
            kb.ts("dve", x0[:], x0[:], c["cst"][:, 3:4], ALU.mult, r=[x0, c["cst"]], w=[x0])
            with ExitStack() as tmp:
                p.stack, sv2 = tmp, p.stack
                NL = 10
                PW = p.sb("PW", [128, NL, 2, 2, 32], F32)
                kb.copy("dve", PW[:, 0].rearrange("p a b c -> p (a b c)"), PQ[:].rearrange("p a b c -> p (a b c)"), r=[], w=[("PW", 0)])
                ta = p.sb("pwa", [128, 32], F32)
                tb_ = p.sb("pwb", [128, 32], F32)
                for l in range(NL - 1):
                    ar = PW[:, l, 0, 0, :]
                    ai = PW[:, l, 1, 1, :]
                    kb.tt("dve", ta[:], ar, ar, ALU.mult, r=[("PW", l)], w=[ta])
                    kb.tt("dve", tb_[:], ai, ai, ALU.mult, r=[("PW", l)], w=[tb_])
                    kb.tt("dve", PW[:, l + 1, 0, 0, :], ta[:], tb_[:], ALU.subtract, r=[ta, tb_], w=[("PW", l + 1)])
                    kb.copy("dve", PW[:, l + 1, 0, 1, :], PW[:, l + 1, 0, 0, :], r=[("PW", l + 1)], w=[("PW", l + 1)])
                    kb.tt("dve", ta[:], ar, ai, ALU.mult, r=[("PW", l)], w=[ta])
                    kb.ts("dve", PW[:, l + 1, 1, 1, :], ta[:], 2.0, ALU.mult, r=[ta], w=[("PW", l + 1)])
                    kb.ts("dve", PW[:, l + 1, 1, 0, :], ta[:], -2.0, ALU.mult, r=[ta], w=[("PW", l + 1)])
                ZW = 514
                KQ = 4
                Z = p.sb("Zq", [128, 2, KQ, ZW], F32)
                z1 = p.sb("zt1", [128, 2, KQ, 256], F32)
                z2 = p.sb("zt2", [128, 2, KQ, 256], F32)
                Za = Z[:]
                pstr = list(Za.ap[0])
                for kq in range(32 // KQ):
                    ks = slice(kq * KQ, (kq + 1) * KQ)
                    kb.copy("dve", Z[:, :, :, 0], x0[:, :, ks], r=[x0], w=[Z])
                    n = 1
                    for l in range(NL):
                        m = min(n, 513 - n)
                        Pa = PW[:, l, 0, :, ks]
                        Qa = PW[:, l, 1, :, ks]
                        Pb = AP(Pa.tensor, Pa.offset, [list(Pa.ap[0]), list(Pa.ap[1]), list(Pa.ap[2]), [0, m]])
                        Qb = AP(Qa.tensor, Qa.offset, [list(Qa.ap[0]), list(Qa.ap[1]), list(Qa.ap[2]), [0, m]])
                        Zsw = AP(Za.tensor, Za.offset + KQ * ZW, [pstr, [-KQ * ZW, 2], [ZW, KQ], [1, m]])
                        kb.tt("dve", z1[:, :, :, 0:m], Z[:, :, :, 0:m], Pb, ALU.mult, r=[Z, ("PW", l)], w=[z1])
                        kb.tt("dve", z2[:, :, :, 0:m], Qb, Zsw, ALU.mult, r=[Z, ("PW", l)], w=[z2])
                        kb.tt("dve", Z[:, :, :, n:n + m], z1[:, :, :, 0:m], z2[:, :, :, 0:m], ALU.add, r=[z1, z2], w=[Z])
                        n *= 2
                    kb.tt("dve", X[:, :, ks, 0:513], X[:, :, ks, 0:513], Z[:, :, :, 0:513], ALU.add, r=[Z], w=[("Xq", kq)])
                p.flush()
                p.stack = sv2
        if I.get("ygT") is None:
            return
        psy_ring = Ring(p, "psy", 3, [128, NCH], F32, psum=True)
        yg_ring = Ring(p, "yg", 2, [128, T], BF16)
        ygv = ygT.rearrange("(k p) t -> p k t", p=128)
        for kk in range(8):
            uc = u_ring.next()
            kb.dma("sp", uc[:], uTv[:, kk, :], w=[uc])
            ucv = uc[:].rearrange("p (c j) -> p j c", j=8)
            yg = yg_ring.next()
            ygs = yg[:].rearrange("p (c j) -> p j c", j=8)
            for s in range(8):
                psy = psy_ring.next()
                for j in range(s + 1):
                    kb.mm(psy[:], Kt[:, s - j, kk, :], ucv[:, j, :], j == 0, False, r=[uc], w=[psy])
                for q in range(4):
                    k = 4 * kk + q
                    for ri in range(2):
                        p.op("pe", lambda e, o=psy[32 * q:32 * q + 32, :], l=Cf[:, s, k, ri, :], rr=X[:, ri, k, 0:NCH],
                             q=q, last=(q == 3 and ri == 1):
                             e.matmul(o, l, rr, start=False, stop=last, tile_position=(0, 32 * q), skip_group_check=True),
                             reads=[], writes=[psy])
                kb.act(ygs[:, s, :], psy[:], AF.Gelu_apprx_tanh, r=[psy], w=[(yg, s)])
            kb.dma("sp", ygv[:, kk, :], yg[:], r=[(yg, s) for s in range(8)], w=[("ygT", kk)])


def phase_glu_router(kb, I):
    p, c = kb.p, kb.c
    gains = c["gains"]
    with p.phase():
        psr = c["ps_ring"] = Ring(p, "ps", 6, [128, TT], F32, psum=True)
        norm_rings(kb, p)
        Wglu = p.sb("Wglu", [128, 8, 2 * D], BF16)
        kb.load_w(Wglu, I["s5_w_glu"], D, 2 * D)
        Wr = p.sb("Wr", [128, 8, 8], F32)
        kb.dma("sp", Wr[:], I["w_router"].rearrange("p (a b) -> p a b", a=8), w=[Wr])
        ident = p.sb("ident", [128, 128], F32)
        kb.memset("pool", ident[:], 0.0, w=[ident])
        p.op("pool", lambda e: e.affine_select(ident[:], ident[:], [[1, 128]], ALU.not_equal, 1.0,
                                               base=0, channel_multiplier=-1), reads=[ident], writes=[ident])
        yg_ring = Ring(p, "ygt", 2, [128, 8, TT], BF16)
        x_ring = Ring(p, "xt", 2, [128, 8, TT], F32)
        h3_ring = Ring(p, "h3", 2, [128, 8, TT], F32)
        hn_ring = Ring(p, "hn", 2, [128, 8, TT], BF16)
        hf_ring = Ring(p, "hf", 1, [128, 8, TT], F32)
        sg_ring = Ring(p, "sg", 3, [128, TT], F32)
        lg_ring = Ring(p, "lg", 2, [128, 8], F32)
        sm_ring = Ring(p, "sm", 2, [128, 16], F32)
        cb_ring = Ring(p, "cb", 2, [128, 8], F32)
        ct_ring = Ring(p, "cbt", 2, [8, TT], F32)
        ygv = I["ygT"].rearrange("(k p) t -> p k t", p=128)
        h2v = I["h2T"].rearrange("(k p) t -> p k t", p=128)
        h3v = I["h3T"].rearrange("(k p) t -> p k t", p=128)
        hn4v = I["hn4T"].rearrange("(k p) t -> p k t", p=128)
        for qt in range(T // TT):
            cols = slice(qt * TT, (qt + 1) * TT)
            yg = yg_ring.next()
            kb.dma("sp", yg[:], ygv[:, :, cols], w=[yg])
            xt = x_ring.next()
            kb.dma("sp", xt[:], h2v[:, :, cols], w=[xt])
            h3 = h3_ring.next()
            for m in range(8):
                psa = psr.next()
                psb = psr.next()
                for k in range(8):
                    kb.mm(psa[:], Wglu[:, k, m * 128:(m + 1) * 128], yg[:, k, :], k == 0, k == 7, r=[yg] + kb.wk(Wglu), w=[psa])
                for k in range(8):
                    kb.mm(psb[:], Wglu[:, k, D + m * 128:D + (m + 1) * 128], yg[:, k, :], k == 0, k == 7, r=[yg] + kb.wk(Wglu), w=[psb])
                sg = sg_ring.next()
                kb.act(sg[:], psb[:], AF.Sigmoid, r=[psb], w=[sg])
                kb.tt("dve", sg[:], psa[:], sg[:], ALU.mult, r=[psa, sg], w=[sg])
                kb.tt("pool", h3[:, m, :], sg[:], xt[:, m, :], ALU.add, r=[sg, xt], w=[(h3, m)])
            kb.dma("sp", h3v[:, :, cols], h3[:], r=[(h3, m) for m in range(8)], w=[("h3T", qt)])
            hn = hn_ring.next()
            rs = kb.rmsnorm([h3[:, k, :] for k in range(8)], [(h3, k) for k in range(8)], 8, D, gains, G_FFN1, hn, hn)
            kb.dma("sp", hn4v[:, :, cols], hn[:], r=[(hn, k) for k in range(8)], w=[("hn4T", qt)])
            hf = hf_ring.next()
            for k in range(8):
                kb.stt("dve", hf[:, k, :], h3[:, k, :], gains[:, G_FFN1 + k:G_FFN1 + k + 1], rs[:], ALU.mult, ALU.mult,
                       r=[(h3, k), rs], w=[(hf, k)])
            cbt = ct_ring.next()
            for tb in range(4):
                psl = psr.next()
                for k in range(8):
                    kb.mm(psl[:, 0:8], hf[:, k, tb * 128:(tb + 1) * 128], Wr[:, k, :], k == 0, k == 7, r=[(hf, k), Wr], w=[psl])
                lg = lg_ring.next()
                kb.copy("dve", lg[:], psl[:, 0:8], r=[psl], w=[lg])
                sm = sm_ring.next()
                p.op("dve", lambda e, o=sm[:, 0:8], i=lg[:]: e.max(o, i), reads=[lg], writes=[sm])
                kb.tt("dve", sm[:, 8:9], sm[:, 1:2], sm[:, 0:1], ALU.subtract, r=[sm], w=[sm])
                kb.act(sm[:, 9:10], sm[:, 8:9], AF.Exp, r=[sm], w=[sm])
                kb.ts("dve", sm[:, 10:11], sm[:, 9:10], 1.0, ALU.add, r=[sm], w=[sm])
                kb.recip(sm[:, 10:11], sm[:, 10:11], r=[sm], w=[sm])
                kb.tt("dve", sm[:, 11:12], sm[:, 9:10], sm[:, 10:11], ALU.mult, r=[sm], w=[sm])
                cb = cb_ring.next()
                cb2 = cb_ring.next()
                kb.ts("dve", cb[:], lg[:], sm[:, 0:1], ALU.is_equal, r=[lg, sm], w=[cb], s2=sm[:, 10:11], op1=ALU.mult)
                kb.ts("dve", cb2[:], lg[:], sm[:, 1:2], ALU.is_equal, r=[lg, sm], w=[cb2], s2=sm[:, 11:12], op1=ALU.mult)
                kb.tt("dve", cb[:], cb[:], cb2[:], ALU.add, r=[cb, cb2], w=[cb])
                pst = psr.next()
                p.op("pe", lambda e, o=pst[0:8, 0:128], i=cb[:]: e.transpose(o, i, ident[:]), reads=[cb, ident], writes=[pst])
                kb.copy("dve", cbt[:, tb * 128:(tb + 1) * 128], pst[0:8, 0:128], r=[pst], w=[(cbt, tb)])
            kb.dma("sp", I["combT"][:, cols], cbt[:], r=[(cbt, tb) for tb in range(4)], w=[("combT", qt)])


def phase_moe(kb, I):
    p, c = kb.p, kb.c
    gains = c["gains"]
    experts = [(I["moe_wg%d" % e], I["moe_wu%d" % e], I["moe_wd%d" % e]) for e in range(8)]
    st = {}

    def comb(sup, qt, ei):
        key = (sup, ei)
        if st.get("cur") != key:
            st["cur"] = key
            if "sel" not in st:
                st["sel"] = p.sb("sel", [8, 8, 128], F32)
                kb.dma("sp", st["sel"][:], I["sel"].rearrange("p (a b) -> p a b", a=8), w=[st["sel"]])
                st["cT"] = p.sb("cT", [8, 2048], F32)
                st["bc_ring"] = Ring(p, "bc", 2, [128, 4, TT], F32)
                st["psb_ring"] = Ring(p, "psb", 1, [128, TT], F32, psum=True)
            if st.get("cTsup") != sup:
                st["cTsup"] = sup
                kb.dma("sp", st["cT"][:], I["combT"][:, sup * 2048:(sup + 1) * 2048], w=[st["cT"]])
            bc = st["bc"] = st["bc_ring"].next()
            for q2 in range(4):
                psb = st["psb_ring"].next()
                g0 = q2 * TT
                kb.mm(psb[:], st["sel"][:, ei, :], st["cT"][:, g0:g0 + TT], True, True, r=[st["sel"], st["cT"]], w=[psb])
                kb.copy("act", bc[:, q2, :], psb[:], r=[psb], w=[(bc, q2)])
        bc = st["bc"]
        return (bc[:, qt, :], (bc, qt))

    ffn_phase(kb, experts, 3584, I["hn4T"], I["h3T"], I["h4T"], comb=comb)
    with p.phase():
        psr = c["ps_ring"] = Ring(p, "ps", 2, [128, TT], F32, psum=True)
        norm_rings(kb, p)
        x_ring = Ring(p, "xt", 2, [128, 8, TT], F32)
        o_ring = Ring(p, "fo", 2, [128, 8, TT], F32)
        h4v = I["h4T"].rearrange("(k p) t -> p k t", p=128)
        outv = I["outT"].rearrange("(k p) t -> p k t", p=128)
        for qt in range(T // TT):
            cols = slice(qt * TT, (qt + 1) * TT)
            xt = x_ring.next()
            kb.dma("sp", xt[:], h4v[:, :, cols], w=[xt])
            o = o_ring.next()
            kb.rmsnorm([xt[:, k, :] for k in range(8)], [xt] * 8, 8, D, gains, G_FIN, o, o)
            kb.dma("sp", outv[:, :, cols], o[:], r=[(o, k) for k in range(8)], w=[("fout", qt)])


def host_weights_l1(inp, moe=True):
    m = {}
    lam_re, lam_im, ldt = inp["s5_lambda_re"][0], inp["s5_lambda_im"][0], inp["s5_log_dt"][0]
    b_re, b_im, c_re, c_im, d = inp["s5_b_re"][0], inp["s5_b_im"][0], inp["s5_c_re"][0], inp["s5_c_im"][0], inp["s5_d"][0]
    r = np.arange(128)
    glp, pp = r // 64, r % 64
    k = np.arange(32)
    g_pm = 2 * k[None, :] + glp[:, None]
    m["lam_re_pm"] = np.ascontiguousarray(lam_re[g_pm, pp[:, None]])
    m["lam_im_pm"] = np.ascontiguousarray(lam_im[g_pm, pp[:, None]])
    m["ldt_pm"] = np.ascontiguousarray(ldt[g_pm])
    for nm, arr, tr in (("Bp_re", b_re, False), ("Bp_im", b_im, False), ("Ct_re", c_re, True), ("Ct_im", c_im, True)):
        out = np.zeros((128, 32, 2, 16), np.float32)
        for gl in range(2):
            rows = np.where(glp == gl)[0]
            gg = g_pm[rows]
            if tr:
                vals = arr[gg, :, pp[rows][:, None]]
            else:
                vals = arr[gg, pp[rows][:, None], :]
            out[rows, :, gl, :] = vals
        m[nm] = np.ascontiguousarray(out.reshape(128, 1024))
    q_, gl_, h_ = r // 32, (r % 32) // 16, r % 16
    col = np.arange(1024)
    kk_, glc, pc = col // 128, (col % 128) // 64, col % 64
    g_fm = 2 * (4 * kk_[None, :] + q_[:, None]) + glc[None, :]
    m["lam_re_fm"] = np.ascontiguousarray(lam_re[g_fm, pc[None, :]])
    m["lam_im_fm"] = np.ascontiguousarray(lam_im[g_fm, pc[None, :]])
    m["ldt_fm"] = np.ascontiguousarray(ldt[g_fm])
    mask = (glc[None, :] == gl_[:, None])
    for nm, arr in (("Bt_re", b_re), ("Bt_im", b_im)):
        vals = arr[g_fm, pc[None, :], h_[:, None]]
        m[nm] = np.ascontiguousarray(np.where(mask, vals, np.float32(0)).astype(np.float32))
    m["d_col"] = np.ascontiguousarray(d.reshape(8, 128).T)
    sel = np.zeros((8, 8, 128), np.float32)
    for e in range(8):
        sel[e, e, :] = 1.0
    m["sel"] = sel.reshape(8, 1024)
    m["w_router"] = np.ascontiguousarray(inp["moe_w_router"][0].reshape(8, 128, 8).transpose(1, 0, 2).reshape(128, 64))
    m["s5_w_in"] = np.ascontiguousarray(inp["s5_w_in"][0])
    m["s5_w_glu"] = np.ascontiguousarray(inp["s5_w_glu"][0])
    for e in range(8 if moe else 0):
        m["moe_wg%d" % e] = inp["moe_w_gate"][0, e]
        m["moe_wu%d" % e] = inp["moe_w_up"][0, e]
        m["moe_wd%d" % e] = inp["moe_w_down"][0, e]
    return m


def build_A():
    nc = bass.Bass("TRN2", target_bir_lowering=False)
    kb = KB(nc)
    kb.c = {}
    kb.nheads = H
    p = kb.p
    I = make_inputs_l0(kb)
    for name, shp in (("s5_w_in", [D, D]), ("lam_re_pm", [128, 32]), ("lam_im_pm", [128, 32]), ("ldt_pm", [128, 32]),
                      ("lam_re_fm", [128, 1024]), ("lam_im_fm", [128, 1024]), ("ldt_fm", [128, 1024]),
                      ("Bp_re", [128, 1024]), ("Bp_im", [128, 1024]), ("Ct_re", [128, 1024]), ("Ct_im", [128, 1024]),
                      ("Bt_re", [128, 1024]), ("Bt_im", [128, 1024]), ("d_col", [128, 8])):
        I[name] = kb.din(name, shp)
    I["kT"] = kb.dscr("kT", [H, 97, S], BF16)
    I["qT"] = kb.dscr("qT", [H, 97, T], BF16)
    I["vS"] = kb.dscr("vS", [S, 1024], BF16)
    I["h1T"] = kb.dscr("h1T", [D, T], F32)
    I["hn2T"] = kb.dscr("hn2T", [D, T], BF16)
    I["h2T"] = kb.dout("h2T", [D, T], F32)
    I["uT"] = kb.dscr("uT", [D, T], BF16)
    I["ygT"] = None
    I["xfin"] = kb.dout("xfin", [128, 64], F32)
    setup_consts(kb, I)
    phase_kvq(kb, I)
    phase_attn(kb, I)
    ffn_phase(kb, [(I["ffn_wg"], I["ffn_wu"], I["ffn_wd"])], 2816, I["hn2T"], I["h1T"], I["h2T"])
    phase_s5(kb, I, ["zero"])
    p.finish()
    return nc


def build_B():
    nc = bass.Bass("TRN2", target_bir_lowering=False)
    kb = KB(nc)
    kb.c = {}
    p = kb.p
    I = make_inputs_l1(kb, False)
    I["h2T"] = kb.din("h2T", [D, T])
    I["uT"] = kb.dscr("uT", [D, T], BF16)
    I["ygT"] = kb.dscr("ygT", [D, T], BF16)
    I["h3T"] = kb.dscr("h3T", [D, T], F32)
    I["hn4T"] = kb.dscr("hn4T", [D, T], BF16)
    I["h4T"] = kb.dscr("h4T", [D, T], F32)
    I["combT"] = kb.dscr("combT", [8, T], F32)
    I["outT"] = kb.dout("outT", [D, T], F32)
    I["xfin"] = None
    setup_consts(kb, I)
    phase_s5(kb, I, ["x0"])
    phase_glu_router(kb, I)
    phase_moe(kb, I)
    p.finish()
    return nc


def build_full():
    nc = bass.Bass("TRN2", target_bir_lowering=False)
    kb = KB(nc)
    kb.c = {}
    kb.nheads = H
    p = kb.p
    I = make_inputs_l0(kb)
    I1 = make_inputs_l1(kb, False, skip=("gains", "cst", "x0"))
    I.update(I1)
    I["kT"] = kb.dscr("kT", [H, 97, S], BF16)
    I["qT"] = kb.dscr("qT", [H, 97, T], BF16)
    I["vS"] = kb.dscr("vS", [S, 1024], BF16)
    I["h1T"] = kb.dscr("h1T", [D, T], F32)
    I["hn2T"] = kb.dscr("hn2T", [D, T], BF16)
    I["h2T"] = kb.dscr("h2T", [D, T], F32)
    I["uT"] = kb.dscr("uT", [D, T], BF16)
    I["ygT"] = kb.dscr("ygT", [D, T], BF16)
    I["h3T"] = kb.dscr("h3T", [D, T], F32)
    I["hn4T"] = kb.dscr("hn4T", [D, T], BF16)
    I["h4T"] = kb.dscr("h4T", [D, T], F32)
    I["combT"] = kb.dscr("combT", [8, T], F32)
    I["xi"] = kb.dscr("xi", [128, 64], F32)
    I["xg"] = kb.dscr("xg", [256, 64], F32)
    I["xfin"] = None
    I["outT"] = kb.dout("outT", [D, T], F32)
    setup_consts(kb, I)
    phase_kvq(kb, I)
    phase_attn(kb, I)
    ffn_phase(kb, [(I["ffn_wg"], I["ffn_wu"], I["ffn_wd"])], 2816, I["hn2T"], I["h1T"], I["h2T"])
    phase_s5(kb, I, ["zero", "cc"])
    phase_glu_router(kb, I)
    phase_moe(kb, I)
    p.finish()
    return nc


_CACHE = {}


def kernel(**inp):
    inp = {k: np.asarray(v) for k, v in inp.items()}
    n = 8
    W = host_weights_l0(inp)
    W.update(host_weights_l1(inp))
    if "F" not in _CACHE:
        _CACHE["F"] = build_full()
    maps = []
    for c_ in range(n):
        m = dict(W)
        m.update(host_inputs_l0(inp, c_))
        cst = W["cst"].copy()
        cst[:, 3] = float(c_ % 2)
        m["cst"] = cst
        maps.append(m)
    res = run_bass_kernel_spmd(_CACHE["F"], maps, core_ids=list(range(n)))
    out = np.zeros((4, 8192, D), np.float32)
    for c_ in range(n):
        b, hf = c_ // 2, c_ % 2
        out[b, hf * T:(hf + 1) * T, :] = np.asarray(res.results[c_]["outT"]).T
    return out

NIT = 24
NSLOT = NIT * 512
IU32 = mybir.dt.uint32


def phase_glu_router_tm(kb, I):
    p, c = kb.p, kb.c
    gains = c["gains"]
    R = c["route"]
    with p.phase():
        psr = c["ps_ring"] = Ring(p, "ps", 4, [128, TT], F32, psum=True)
        pstf_ring = Ring(p, "pstf", 2, [128, TT], F32, psum=True)
        pstb_ring = Ring(p, "pstb", 1, [128, 1024], BF16, psum=True)
        norm_rings(kb, p)
        Wglu = p.sb("Wglu", [128, 8, 2 * D], BF16)
        kb.load_w(Wglu, I["s5_w_glu"], D, 2 * D)
        Wr = p.sb("Wr", [128, 8, 8], F32)
        kb.dma("sp", Wr[:], I["w_router"].rearrange("p (a b) -> p a b", a=8), w=[Wr])
        ident = p.sb("ident", [128, 128], F32)
        kb.memset("pool", ident[:], 0.0, w=[ident])
        p.op("pool", lambda e: e.affine_select(ident[:], ident[:], [[1, 128]], ALU.not_equal, 1.0,
                                               base=0, channel_multiplier=-1), reads=[ident], writes=[ident])
        identb = p.sb("identb", [128, 128], BF16)
        kb.copy("dve", identb[:], ident[:], r=[ident], w=[identb])
        yg_ring = Ring(p, "ygt", 2, [128, 8, TT], BF16)
        x_ring = Ring(p, "xt", 2, [128, 8, TT], F32)
        h3_ring = Ring(p, "h3", 2, [128, 8, TT], F32)
        hn_ring = Ring(p, "hn", 2, [128, 8, TT], BF16)
        hf_ring = Ring(p, "hf", 1, [128, 8, TT], F32)
        sg_ring = Ring(p, "sg", 3, [128, TT], F32)
        tmf_ring = Ring(p, "tmf", 2, [128, 1024], F32)
        tmb_ring = Ring(p, "tmb", 2, [128, 1024], BF16)
        ygv = I["ygT"].rearrange("(k p) t -> p k t", p=128)
        h2v = I["h2T"].rearrange("(k p) t -> p k t", p=128)
        for qt in range(T // TT):
            cols = slice(qt * TT, (qt + 1) * TT)
            yg = yg_ring.next()
            kb.dma("sp", yg[:], ygv[:, :, cols], w=[yg])
            xt = x_ring.next()
            kb.dma("sp", xt[:], h2v[:, :, cols], w=[xt])
            h3 = h3_ring.next()
            for m in range(8):
                psa = psr.next()
                psb = psr.next()
                for k in range(8):
                    kb.mm(psa[:], Wglu[:, k, m * 128:(m + 1) * 128], yg[:, k, :], k == 0, k == 7, r=[yg] + kb.wk(Wglu), w=[psa])
                for k in range(8):
                    kb.mm(psb[:], Wglu[:, k, D + m * 128:D + (m + 1) * 128], yg[:, k, :], k == 0, k == 7, r=[yg] + kb.wk(Wglu), w=[psb])
                sg = sg_ring.next()
                kb.act(sg[:], psb[:], AF.Sigmoid, r=[psb], w=[sg])
                kb.tt("dve", sg[:], psa[:], sg[:], ALU.mult, r=[psa, sg], w=[sg])
                kb.tt("pool", h3[:, m, :], sg[:], xt[:, m, :], ALU.add, r=[sg, xt], w=[(h3, m)])
            hn = hn_ring.next()
            rs = kb.rmsnorm([h3[:, k, :] for k in range(8)], [(h3, k) for k in range(8)], 8, D, gains, G_FFN1, hn, hn)
            hf = hf_ring.next()
            for k in range(8):
                kb.stt("dve", hf[:, k, :], h3[:, k, :], gains[:, G_FFN1 + k:G_FFN1 + k + 1], rs[:], ALU.mult, ALU.mult,
                       r=[(h3, k), rs], w=[(hf, k)])
            for tb in range(4):
                blk = qt * 4 + tb
                tsl = slice(tb * 128, (tb + 1) * 128)
                tmf = tmf_ring.next()
                for hh in range(2):
                    pst = pstf_ring.next()
                    for kq in range(4):
                        k = hh * 4 + kq
                        p.op("pe", lambda e, o=pst[:, kq * 128:(kq + 1) * 128], i=h3[:, k, tsl]: e.transpose(o, i, ident[:]),
                             reads=[(h3, k), ident], writes=[pst])
                    kb.copy("act", tmf[:, hh * 512:(hh + 1) * 512], pst[:], r=[pst], w=[(tmf, hh)])
                kb.dma("sp", I["h3TM"][blk * 128:(blk + 1) * 128, :], tmf[:], r=[(tmf, 0), (tmf, 1)], w=[("h3TM", blk)])
                tmb = tmb_ring.next()
                pstb = pstb_ring.next()
                for k in range(8):
                    p.op("pe", lambda e, o=pstb[:, k * 128:(k + 1) * 128], i=hn[:, k, tsl]: e.transpose(o, i, identb[:]),
                         reads=[(hn, k), identb], writes=[pstb])
                kb.copy("act", tmb[:], pstb[:], r=[pstb], w=[tmb])
                kb.dma("sp", I["hn4TM"][blk * 128:(blk + 1) * 128, :], tmb[:], r=[tmb], w=[("hn4TM", blk)])
                psl = psr.next()
                for k in range(8):
                    kb.mm(psl[:, 0:8], hf[:, k, tsl], Wr[:, k, :], k == 0, k == 7, r=[(hf, k), Wr], w=[psl])
                kb.copy("dve", R["LG"][:, blk, :], psl[:, 0:8], r=[psl], w=[("LG", blk)])


def phase_route(kb, I):
    p, c = kb.p, kb.c
    R = c["route"]
    NB = 32
    with p.phase():
        LG = R["LG"]
        v1 = p.sb("rv1", [128, NB], F32)
        v2 = p.sb("rv2", [128, NB], F32)
        lg2 = p.sb("rlg2", [128, NB, 8], F32)
        p.op("dve", lambda e: e.tensor_reduce(v1[:], LG[:], AX.X, ALU.max), reads=[], writes=[v1])
        kb.tt("dve", R["M1"][:], LG[:], bc_last(v1[:], 8), ALU.is_equal, r=[v1], w=[R["M1"]])
        kb.stt("dve", lg2[:], R["M1"][:], -1.0e30, LG[:], ALU.mult, ALU.add, r=[R["M1"]], w=[lg2])
        p.op("dve", lambda e: e.tensor_reduce(v2[:], lg2[:], AX.X, ALU.max), reads=[lg2], writes=[v2])
        kb.tt("dve", R["M2"][:], lg2[:], bc_last(v2[:], 8), ALU.is_equal, r=[v2, lg2], w=[R["M2"]])
        dlt = p.sb("rdlt", [128, NB], F32)
        kb.tt("dve", dlt[:], v2[:], v1[:], ALU.subtract, r=[v1, v2], w=[dlt])
        kb.act(dlt[:], dlt[:], AF.Exp, r=[dlt], w=[dlt])
        den_ = p.sb("rden", [128, NB], F32)
        kb.ts("dve", den_[:], dlt[:], 1.0, ALU.add, r=[dlt], w=[den_])
        kb.recip(den_[:], den_[:], r=[den_], w=[den_])
        kb.copy("dve", R["G"][:, :, 0], den_[:], r=[den_], w=[R["G"]])
        kb.tt("dve", R["G"][:, :, 1], dlt[:], den_[:], ALU.mult, r=[dlt, den_, R["G"]], w=[R["G"]])
        M = p.sb("rM", [128, NB, 8], F32)
        Mb = p.sb("rMb", [128, NB, 8], BF16)
        kb.tt("dve", M[:], R["M1"][:], R["M2"][:], ALU.add, r=[R["M1"], R["M2"]], w=[M])
        kb.copy("dve", Mb[:], M[:], r=[M], w=[Mb])
        U = p.sb("rU", [128, 128], BF16)
        kb.memset("pool", U[:], 1.0, w=[U])
        p.op("pool", lambda e: e.affine_select(U[:], U[:], [[1, 128]], ALU.is_gt, 0.0, base=0, channel_multiplier=-1),
             reads=[U], writes=[U])
        pst = p.ps("rpst", [128, NB * 8], F32)
        psp = p.ps("rpsp", [128, NB * 8], F32)
        Mbf = Mb[:].rearrange("p a b -> p (a b)")
        kb.mm(pst[:], c["ones_bf"][:], Mbf, True, True, r=[Mb, c["ones_bf"]], w=[pst])
        kb.mm(psp[:], U[:], Mbf, True, True, r=[Mb, U], w=[psp])
        tot = p.sb("rtot", [128, NB, 8], F32)
        pre = p.sb("rpre", [128, NB, 8], F32)
        kb.copy("dve", tot[:].rearrange("p a b -> p (a b)"), pst[:], r=[pst], w=[tot])
        kb.copy("dve", pre[:].rearrange("p a b -> p (a b)"), psp[:], r=[psp], w=[pre])
        boff = p.sb("rboff", [128, NB + 1, 8], F32)
        kb.memset("dve", boff[:, 0, :], 0.0, w=[boff])
        for b in range(NB):
            kb.tt("dve", boff[:, b + 1, :], boff[:, b, :], tot[:, b, :], ALU.add, r=[boff, tot], w=[boff])
        q = p.sb("rq", [128, 8], F32)
        qi = p.sb("rqi", [128, 8], I32)
        qf = p.sb("rqf", [128, 8], F32)
        fx = p.sb("rfx", [128, 8], F32)
        kb.ts("dve", q[:], boff[:, NB, :], 511.0, ALU.add, r=[boff], w=[q], s2=1.0 / 512.0, op1=ALU.mult)
        kb.copy("dve", qi[:], q[:], r=[q], w=[qi])
        kb.copy("dve", qf[:], qi[:], r=[qi], w=[qf])
        kb.tt("dve", fx[:], qf[:], q[:], ALU.is_gt, r=[qf, q], w=[fx])
        kb.tt("dve", qf[:], qf[:], fx[:], ALU.subtract, r=[qf, fx], w=[qf])
        start = p.sb("rstart", [128, 9], F32)
        kb.memset("dve", start[:, 0:1], 0.0, w=[start])
        for e in range(8):
            kb.stt("dve", start[:, e + 1:e + 2], qf[:, e:e + 1], 512.0, start[:, e:e + 1], ALU.mult, ALU.add,
                   r=[qf, start], w=[start])
        pos = p.sb("rpos", [128, NB, 8], F32)
        kb.tt("dve", pos[:], pre[:], boff[:, 0:NB, :], ALU.add, r=[pre, boff], w=[pos])
        kb.tt("dve", pos[:], pos[:], bc_mid(start[:, 0:8], NB), ALU.add, r=[pos, start], w=[pos])
        tmp = p.sb("rtmp", [128, NB, 8], F32)
        pf = p.sb("rpf", [128, NB], F32)
        for (Mx, dst) in ((R["M1"], R["posA"]), (R["M2"], R["posB"])):
            kb.tt("dve", tmp[:], pos[:], Mx[:], ALU.mult, r=[pos], w=[tmp])
            p.op("dve", lambda e, o=pf[:], i=tmp[:]: e.tensor_reduce(o, i, AX.X, ALU.add), reads=[tmp], writes=[pf])
            kb.copy("dve", dst[:], pf[:], r=[pf], w=[dst])
        tid = p.sb("rtid", [128, NB, 16], I32)
        p.op("pool", lambda e: e.iota(tid[:], [[128, NB], [0, 16]], base=0, channel_multiplier=1), writes=[tid])
        z = p.sb("rz", [128, NSLOT // 128 * 16], I32)
        kb.memset("pool", z[:], 0, w=[z])
        kb.dma("sp", I["tokT"].rearrange("(p j) c -> p (j c)", p=128), z[:], r=[z], w=["tokT0"])
        for b in range(NB):
            for di, dst in enumerate((R["posA"], R["posB"])):
                p.op("pool", lambda e, dst=dst, b=b: e.indirect_dma_start(
                    out=I["tokT"], out_offset=bass.IndirectOffsetOnAxis(ap=dst[:, b:b + 1], axis=0),
                    in_=tid[:, b, :], in_offset=None), reads=[dst, tid, "tokT0"], writes=[("tokT", b, di)], dma=True)
        thr = p.sb("rthr", [128, NIT], F32)
        p.op("pool", lambda e: e.iota(thr[:], [[512, NIT]], base=0, channel_multiplier=0, allow_small_or_imprecise_dtypes=True),
             writes=[thr])
        ei = p.sb("rei", [128, NIT], F32)
        kb.memset("dve", ei[:], 0.0, w=[ei])
        for e in range(1, 8):
            kb.stt("dve", ei[:], thr[:], start[:, e:e + 1], ei[:], ALU.is_ge, ALU.add, r=[thr, start, ei], w=[ei])
        pj = p.sb("rpj", [128, NIT, 14], F32)
        p.op("pool", lambda e: e.iota(pj[:], [[0, NIT], [128, 14]], base=0, channel_multiplier=1,
                                      allow_small_or_imprecise_dtypes=True), writes=[pj])
        e1792 = p.sb("re1792", [128, NIT], F32)
        kb.ts("dve", e1792[:], ei[:], 1792.0, ALU.mult, r=[ei], w=[e1792])
        kb.tt("dve", pj[:], pj[:], bc_last(e1792[:], 14), ALU.add, r=[pj, e1792], w=[pj])
        kb.copy("dve", R["widx"][:], pj[:], r=[pj], w=[R["widx"]])


def phase_moe_routed(kb, I):
    p, c = kb.p, kb.c
    R = c["route"]
    with p.phase():
        identb = p.sb("identb", [128, 128], BF16)
        idf = p.sb("idf", [128, 128], F32)
        kb.memset("pool", idf[:], 0.0, w=[idf])
        p.op("pool", lambda e: e.affine_select(idf[:], idf[:], [[1, 128]], ALU.not_equal, 1.0,
                                               base=0, channel_multiplier=-1), reads=[idf], writes=[idf])
        kb.copy("dve", identb[:], idf[:], r=[idf], w=[identb])
        idx_ring = Ring(p, "ix", 2, [128, 4, 16], I32)
        xg_ring = Ring(p, "xg", 2, [128, 4, 1024], BF16)
        xT_ring = Ring(p, "xT", 2, [128, 8, TT], BF16)
        wg_ring = Ring(p, "wg", 3, [128, 8, 512], BF16)
        wu_ring = Ring(p, "wu", 3, [128, 8, 512], BF16)
        wd_ring = Ring(p, "wd", 3, [128, 4, 1024], BF16)
        a_ring = Ring(p, "a", 2, [128, 4, TT], BF16)
        sg_ring = Ring(p, "sg", 3, [128, TT], F32)
        ya_ring = Ring(p, "ya", 2, [128, 4, 1024], F32)
        psg_ring = Ring(p, "psg", 2, [128, TT], F32, psum=True)
        psu_ring = Ring(p, "psu", 2, [128, TT], F32, psum=True)
        psd_ring = Ring(p, "psd", 2, [128, TT], F32, psum=True)
        pst_ring = Ring(p, "pstx", 2, [128, TT], BF16, psum=True)
        Yv = I["Y"].rearrange("(i j p) f -> i p j f", j=4, p=128)
        tokv = I["tokT"].rearrange("(i j p) c -> i p j c", j=4, p=128)
        def emit_down(a, Wd, ya, first):
            for j in range(4):
                for hc in range(2):
                    psd = psd_ring.next()
                    for f in range(4):
                        kb.mm(psd[:], a[:, f, j * 128:(j + 1) * 128], Wd[:, f, hc * 512:(hc + 1) * 512], f == 0, f == 3,
                              r=[(a, f), (Wd, 0), (Wd, 1)], w=[psd])
                    ysl = ya[:, j, hc * 512:(hc + 1) * 512]
                    if first:
                        kb.copy("act", ysl, psd[:], r=[psd], w=[(ya, j, hc)])
                    else:
                        kb.tt("dve", ysl, psd[:], ysl, ALU.add, r=[psd, (ya, j, hc)], w=[(ya, j, hc)])

        pend = None
        xgs = {}

        def gather_tokens(it):
            ix = idx_ring.next()
            kb.dma("sp", ix[:], tokv[it], w=[ix])
            xg = xg_ring.next()
            for j in range(4):
                p.op("pool", lambda e, o=xg[:, j, :], ia=ix[:, j, 0:1]: e.indirect_dma_start(
                    out=o, out_offset=None, in_=I["hn4TM"], in_offset=bass.IndirectOffsetOnAxis(ap=ia, axis=0)),
                    reads=[ix], writes=[(xg, j)], dma=True)
            xgs[it] = xg

        gather_tokens(0)
        for it in range(NIT):
            xg = xgs.pop(it)
            xT = xT_ring.next()
            for k in range(8):
                pst = pst_ring.next()
                for j in range(4):
                    p.op("pe", lambda e, o=pst[:, j * 128:(j + 1) * 128], i=xg[:, j, k * 128:(k + 1) * 128]:
                         e.transpose(o, i, identb[:]), reads=[(xg, j), identb], writes=[pst])
                kb.copy("act" if k % 2 else "dve", xT[:, k, :], pst[:], r=[pst], w=[(xT, k)])
            ya = ya_ring.next()
            for blk in range(7):
                Wg = wg_ring.next()
                Wu = wu_ring.next()
                Wd = wd_ring.next()
                for (Wt, tab, nm) in ((Wg, I["WG"], "g"), (Wu, I["WU"], "u"), (Wd, I["WD"], "d")):
                    for half in range(2):
                        if nm == "d":
                            o = Wt[:, 2 * half:2 * half + 2, :].rearrange("p a b -> p (a b)")
                        else:
                            o = Wt[:, 4 * half:4 * half + 4, :].rearrange("p a b -> p (a b)")
                        p.op("pool", lambda e, o=o, tab=tab, ia=R["widx"][:, it, 2 * blk + half:2 * blk + half + 1]:
                             e.indirect_dma_start(out=o, out_offset=None, in_=tab,
                                                  in_offset=bass.IndirectOffsetOnAxis(ap=ia, axis=0)),
                             reads=[R["widx"]], writes=[(Wt, half)], dma=True)
                if blk == 1 and it + 1 < NIT:
                    gather_tokens(it + 1)
                a = a_ring.next()
                for f in range(4):
                    psg = psg_ring.next()
                    psu = psu_ring.next()
                    for k in range(8):
                        kb.mm(psg[:], Wg[:, k, f * 128:(f + 1) * 128], xT[:, k, :], k == 0, k == 7,
                              r=[(xT, k), (Wg, 0), (Wg, 1)], w=[psg])
                    for k in range(8):
                        kb.mm(psu[:], Wu[:, k, f * 128:(f + 1) * 128], xT[:, k, :], k == 0, k == 7,
                              r=[(xT, k), (Wu, 0), (Wu, 1)], w=[psu])
                    sg = sg_ring.next()
                    kb.act(sg[:], psg[:], AF.Silu, r=[psg], w=[sg])
                    kb.tt("dve", a[:, f, :], psu[:], sg[:], ALU.mult, r=[psu, sg], w=[(a, f)])
                if pend is not None:
                    emit_down(*pend)
                pend = (a, Wd, ya, blk == 0)
            emit_down(*pend)
            pend = None
            kb.dma("sp", Yv[it], ya[:], r=[(ya, j, hc) for j in range(4) for hc in range(2)], w=[("Y", it)])


def phase_combine(kb, I):
    p, c = kb.p, kb.c
    R = c["route"]
    with p.phase():
        gf = p.sb("gfin", [128, 1024], F32)
        kb.dma("sp", gf[:], I["gfin_bc"], w=[gf])
        yA_ring = Ring(p, "yA", 4, [128, 1024], F32)
        yB_ring = Ring(p, "yB", 4, [128, 1024], F32)
        h_ring = Ring(p, "h3r", 4, [128, 1024], F32)
        j_ring = Ring(p, "junk", 2, [128, 1024], F32)
        ss_ring = Ring(p, "ss", 2, [128, 2], F32)
        o_ring = Ring(p, "oo", 2, [128, 1024], F32)
        for b in range(32):
            yA = yA_ring.next()
            yB = yB_ring.next()
            h = h_ring.next()
            for (yy, pos) in ((yA, R["posA"]), (yB, R["posB"])):
                p.op("pool", lambda e, o=yy[:], ia=pos[:, b:b + 1]: e.indirect_dma_start(
                    out=o, out_offset=None, in_=I["Y"], in_offset=bass.IndirectOffsetOnAxis(ap=ia, axis=0)),
                    reads=[pos], writes=[yy], dma=True)
            kb.dma("sp", h[:], I["h3TM"][b * 128:(b + 1) * 128, :], w=[h])
            kb.stt("dve", h[:], yA[:], R["G"][:, b, 0:1], h[:], ALU.mult, ALU.add, r=[yA, h], w=[h])
            kb.stt("dve", h[:], yB[:], R["G"][:, b, 1:2], h[:], ALU.mult, ALU.add, r=[yB, h], w=[h])
            jk = j_ring.next()
            ss = ss_ring.next()
            p.op("act", lambda e, o=jk[:], i=h[:], a=ss[:, 0:1]: e.activation(o, i, AF.Square, accum_out=a),
                 reads=[h], writes=[jk, ss])
            kb.act(ss[:, 1:2], ss[:, 0:1], AF.Sqrt, r=[ss], w=[ss], bias=c["cst"][:, 2:3], scale=1.0 / D)
            kb.recip(ss[:, 1:2], ss[:, 1:2], r=[ss], w=[ss])
            o = o_ring.next()
            kb.stt("dve", o[:], h[:], ss[:, 1:2], gf[:], ALU.mult, ALU.mult, r=[h, ss, gf], w=[o])
            kb.dma("sp", I["out_tm"][b * 128:(b + 1) * 128, :], o[:], r=[o], w=[("out", b)])


def build_full_routed():
    nc = bass.Bass("TRN2", target_bir_lowering=False)
    kb = KB(nc)
    kb.c = {}
    kb.nheads = H
    p = kb.p
    I = make_inputs_l0(kb)
    I1 = make_inputs_l1(kb, False, skip=("gains", "cst", "x0", "sel"), moe=False)
    I.update(I1)
    I["WG"] = kb.din("WG", [8 * 7 * 2 * 128, 2048])
    I["WU"] = kb.din("WU", [8 * 7 * 2 * 128, 2048])
    I["WD"] = kb.din("WD", [8 * 7 * 2 * 128, 2048])
    I["gfin_bc"] = kb.din("gfin_bc", [128, 1024])
    for nm, shp, dt in (("kT", [H, 97, S], BF16), ("qT", [H, 97, T], BF16), ("vS", [S, 1024], BF16), ("h1T", [D, T], F32),
                        ("hn2T", [D, T], BF16), ("h2T", [D, T], F32), ("uT", [D, T], BF16), ("ygT", [D, T], BF16),
                        ("h3TM", [T, D], F32), ("hn4TM", [T, D], BF16), ("Y", [NSLOT, D], F32), ("tokT", [NSLOT, 16], I32),
                        ("xi", [128, 64], F32), ("xg", [256, 64], F32)):
        I[nm] = kb.dscr(nm, shp, dt)
    I["xfin"] = None
    I["out_tm"] = kb.dout("out_tm", [T, D], F32)
    setup_consts(kb, I)
    R = kb.c["route"] = {}
    R["M1"] = p.sb("rM1", [128, 32, 8], F32)
    R["M2"] = p.sb("rM2", [128, 32, 8], F32)
    R["G"] = p.sb("rG", [128, 32, 2], F32)
    R["LG"] = p.sb("rLG", [128, 32, 8], F32)
    R["posA"] = p.sb("rposA", [128, 32], I32)
    R["posB"] = p.sb("rposB", [128, 32], I32)
    R["widx"] = p.sb("rwidx", [128, NIT, 14], I32)
    phase_kvq(kb, I)
    phase_attn(kb, I)
    ffn_phase(kb, [(I["ffn_wg"], I["ffn_wu"], I["ffn_wd"])], 2816, I["hn2T"], I["h1T"], I["h2T"])
    I["s5_fix"] = True
    phase_s5(kb, I, ["zero"])
    phase_glu_router_tm(kb, I)
    phase_route(kb, I)
    phase_moe_routed(kb, I)
    phase_combine(kb, I)
    p.finish()
    return nc


def host_weights_routed(inp):
    m = {}
    wg = inp["moe_w_gate"][0].reshape(8, 2, 4, 128, 7, 512)
    m["WG"] = np.ascontiguousarray(wg.transpose(0, 4, 1, 3, 2, 5).reshape(8 * 7 * 2 * 128, 2048))
    wu = inp["moe_w_up"][0].reshape(8, 2, 4, 128, 7, 512)
    m["WU"] = np.ascontiguousarray(wu.transpose(0, 4, 1, 3, 2, 5).reshape(8 * 7 * 2 * 128, 2048))
    wd = inp["moe_w_down"][0].reshape(8, 7, 2, 2, 128, 1024)
    m["WD"] = np.ascontiguousarray(wd.transpose(0, 1, 2, 4, 3, 5).reshape(8 * 7 * 2 * 128, 2048))
    m["gfin_bc"] = np.ascontiguousarray(np.broadcast_to(inp["final_norm"][None, :], (128, 1024)))
    return m


def kernel(**inp):
    inp = {k: np.asarray(v) for k, v in inp.items()}
    n = 8
    W = host_weights_l0(inp)
    W.update(host_weights_l1(inp, moe=False))
    W.update(host_weights_routed(inp))
    W.pop("sel", None)
    if "R" not in _CACHE:
        _CACHE["R"] = build_full_routed()
    maps = []
    for c_ in range(n):
        m = dict(W)
        m.update(host_inputs_l0(inp, c_))
        cst = W["cst"].copy()
        cst[:, 3] = float(c_ % 2)
        m["cst"] = cst
        maps.append(m)
    res = run_bass_kernel_spmd(_CACHE["R"], maps, core_ids=list(range(n)))
    out = np.zeros((4, 8192, D), np.float32)
    for c_ in range(n):
        b, hf = c_ // 2, c_ % 2
        out[b, hf * T:(hf + 1) * T, :] = np.asarray(res.results[c_]["out_tm"])
    return out
```

```python
import numpy as np
import concourse.bass as bass
import concourse.mybir as mybir
from contextlib import ExitStack, contextmanager

F32 = mybir.dt.float32
BF16 = mybir.dt.bfloat16
I32 = mybir.dt.int32
AF = mybir.ActivationFunctionType
ALU = mybir.AluOpType
AX = mybir.AxisListType

ENGS = ("pe", "dve", "act", "pool", "sp")
DMAQ = ("sp", "pool", "act")
SAME_ENGINE_FIFO = ("pe",)
NDMASEM = 12


def _key(k):
    if isinstance(k, (str, int)):
        return k
    if isinstance(k, tuple):
        return tuple(_key(x) for x in k)
    return k.name


class Prog:
    def __init__(self, nc):
        self.nc = nc
        self.ops = []
        self.emitted = 0
        self.last_w = {}
        self.readers = {}
        self.stack = ExitStack()
        self.semstack = ExitStack()
        self.sem_eng = {e: self.semstack.enter_context(nc.semaphore("s_" + e)) for e in ENGS}
        self.sem_dma = {e: [self.semstack.enter_context(nc.semaphore("d_%s_%d" % (e, i))) for i in range(NDMASEM)]
                        for e in DMAQ}
        self.cnt = {e: 0 for e in ENGS}
        self.dcnt = {e: 0 for e in ENGS}
        self.seen = {e: {} for e in ENGS}
        self.dma_last = {}

    def sb(self, name, shape, dt):
        self.uid = getattr(self, "uid", 0) + 1
        return self.stack.enter_context(self.nc.sbuf_tensor("sb%d_%s" % (self.uid, name), list(shape), dt))

    def ps(self, name, shape, dt=F32):
        self.uid = getattr(self, "uid", 0) + 1
        return self.stack.enter_context(self.nc.psum_tensor("ps%d_%s" % (self.uid, name), list(shape), dt))

    @contextmanager
    def phase(self):
        saved = self.stack
        st = ExitStack()
        self.stack = st
        try:
            yield
            self.flush()
        except BaseException:
            import traceback
            traceback.print_exc()
            raise
        finally:
            st.close()
            self.stack = saved

    def op(self, eng, fn, reads=(), writes=(), dma=False):
        oid = len(self.ops)
        reads = [_key(k) for k in reads]
        writes = [_key(k) for k in writes]
        deps = set()
        for k in reads:
            w = self.last_w.get(k)
            if w is not None:
                deps.add(w)
        for k in writes:
            w = self.last_w.get(k)
            if w is not None:
                deps.add(w)
            for r in self.readers.get(k, ()):
                deps.add(r)
        deps.discard(oid)
        rec = dict(id=oid, eng=eng, fn=fn, deps=deps, dma=dma, sig=False)
        self.ops.append(rec)
        for k in reads:
            self.readers.setdefault(k, []).append(oid)
        for k in writes:
            self.last_w[k] = oid
            self.readers[k] = []
        return oid

    def dma(self, eng, out, in_, reads=(), writes=(), **kw):
        def fn(e):
            return e.dma_start(out=out, in_=in_, **kw)
        return self.op(eng, fn, reads, writes, dma=True)

    def cc(self, kind, alu, groups, in_ap, out_ap, reads=(), writes=()):
        def fn(e):
            return e.collective_compute(kind, alu, replica_groups=groups, ins=[in_ap], outs=[out_ap])
        oid = self.op("pool", fn, reads, writes)
        self.ops[oid]["cc"] = True
        return oid

    def flush(self):
        nc = self.nc
        ops = self.ops
        batch = ops[self.emitted:]
        if not batch:
            return
        eng_ops = {e: [] for e in ENGS}
        for o in batch:
            eng_ops[o["eng"]].append(o)
            nd = set()
            for d in o["deps"]:
                po = ops[d]
                if po["eng"] == o["eng"] and po["eng"] in SAME_ENGINE_FIFO and not po["dma"] and not o["dma"] \
                        and not po.get("cc") and not o.get("cc"):
                    continue
                nd.add(d)
            o["deps"] = nd
            for d in nd:
                ops[d]["sig"] = True
        for e in ENGS:
            for o in reversed(eng_ops[e]):
                if not o["dma"] and not o.get("cc"):
                    o["sig"] = True
                    break
        for o in batch:
            e = o["eng"]
            if o["dma"]:
                k = self.dcnt[e]
                self.dcnt[e] += 1
                o["sem"] = self.sem_dma[e][k % NDMASEM]
                o["semkey"] = (e, k % NDMASEM)
                o["val"] = 16 * (k // NDMASEM + 1)
                o["prev_val"] = 16 * (k // NDMASEM)
                self.dma_last[o["semkey"]] = (o["sem"], o["val"])
            elif o.get("cc"):
                self.ncc = getattr(self, "ncc", 0) + 1
                o["sem"] = self.semstack.enter_context(nc.semaphore("cc_%d" % self.ncc))
                o["semkey"] = ("cc", self.ncc)
                o["val"] = 1
                self.dma_last[o["semkey"]] = (o["sem"], 1)
            elif o["sig"]:
                self.cnt[e] += 1
                o["sem"] = self.sem_eng[e]
                o["semkey"] = (e, -1)
                o["val"] = self.cnt[e]
        final_eng = {e: self.cnt[e] for e in ENGS}
        final_dma = dict(self.dma_last)

        def run_engine(ename, eobj):
            seen = self.seen[ename]
            for o in eng_ops[ename]:
                waits = {}
                for d in o["deps"]:
                    po = ops[d]
                    key = po["semkey"]
                    if seen.get(key, 0) >= po["val"]:
                        continue
                    if key not in waits or waits[key][1] < po["val"]:
                        waits[key] = (po["sem"], po["val"])
                if o["dma"] and o["prev_val"] > 0:
                    key = o["semkey"]
                    if seen.get(key, 0) < o["prev_val"]:
                        if key not in waits or waits[key][1] < o["prev_val"]:
                            waits[key] = (o["sem"], o["prev_val"])
                for key, (sem, val) in waits.items():
                    eobj.wait_ge(sem, val)
                    seen[key] = val
                ins = o["fn"](eobj)
                if o["dma"]:
                    ins.then_inc(o["sem"], 16)
                elif o.get("cc"):
                    ins.then_inc(o["sem"])
                elif o["sig"]:
                    ins.then_inc(o["sem"], 1)
            for e2 in ENGS:
                key = (e2, -1)
                if final_eng[e2] > seen.get(key, 0):
                    eobj.wait_ge(self.sem_eng[e2], final_eng[e2])
                    seen[key] = final_eng[e2]
            for key, (sem, val) in final_dma.items():
                if seen.get(key, 0) < val:
                    eobj.wait_ge(sem, val)
                    seen[key] = val

        with nc.Block() as block:
            @block.tensor
            def _(e):
                run_engine("pe", e)

            @block.vector
            def _(e):
                run_engine("dve", e)

            @block.scalar
            def _(e):
                run_engine("act", e)

            @block.gpsimd
            def _(e):
                run_engine("pool", e)

            @block.sync
            def _(e):
                run_engine("sp", e)
        self.emitted = len(ops)
        self.last_w = {}
        self.readers = {}
        for o in batch:
            o["fn"] = None

    def finish(self):
        self.flush()
        self.stack.close()
        self.semstack.close()

import math
from concourse.bass_utils import run_bass_kernel_spmd
from concourse.ap import AP

D = 1024
S = 8192
T = 4096
TT = 512
H = 16
EPS = 1e-6
NEGBIG = 30000.0
TWO_PI = 2.0 * math.pi


class Ring:
    def __init__(self, p, name, n, shape, dt, psum=False):
        self.tiles = [(p.ps if psum else p.sb)("%s%d" % (name, i), shape, dt) for i in range(n)]
        self.i = 0

    def next(self):
        t = self.tiles[self.i % len(self.tiles)]
        self.i += 1
        return t


class KB:
    def __init__(self, nc, dbg=None):
        self.nc = nc
        self.p = Prog(nc)
        self.dbg = dbg
        self.ins = {}
        self.outs = {}
        self.alt = 0
        self.wkeys = {}

    def din(self, name, shape, dt=F32):
        a = self.nc.dram_tensor(name, list(shape), dt, kind="ExternalInput").ap()
        self.ins[name] = a
        return a

    def dout(self, name, shape, dt=F32):
        a = self.nc.dram_tensor(name, list(shape), dt, kind="ExternalOutput").ap()
        self.outs[name] = a
        return a

    def dscr(self, name, shape, dt):
        return self.nc.dram_tensor(name, list(shape), dt).ap()

    def mm(self, out, lhsT, rhs, start, stop, r, w):
        self.p.op("pe", lambda e, a=out, b=lhsT, c=rhs, s=start, t=stop: e.matmul(a, b, c, start=s, stop=t),
                  reads=r, writes=w)

    def act(self, out, in_, func, r, w, bias=None, scale=None):
        kw = {}
        if bias is not None:
            kw["bias"] = bias
        if scale is not None:
            kw["scale"] = scale
        self.p.op("act", lambda e, a=out, b=in_, f=func, kw=kw: e.activation(a, b, f, **kw), reads=r, writes=w)

    def tt(self, eng, out, a, b, op, r, w):
        self.p.op(eng, lambda e, o=out, x=a, y=b, f=op: e.tensor_tensor(o, x, y, f), reads=r, writes=w)

    def ts(self, eng, out, a, s1, op0, r, w, s2=None, op1=None):
        if op1 is None:
            self.p.op(eng, lambda e, o=out, x=a, s=s1, f=op0: e.tensor_scalar(o, x, s, None, f), reads=r, writes=w)
        else:
            self.p.op(eng, lambda e, o=out, x=a, s=s1, f=op0, t=s2, g=op1: e.tensor_scalar(o, x, s, t, f, g),
                      reads=r, writes=w)

    def stt(self, eng, out, a, s, b, op0, op1, r, w):
        self.p.op(eng, lambda e, o=out, x=a, sc=s, y=b, f=op0, g=op1: e.scalar_tensor_tensor(o, x, sc, y, f, g),
                  reads=r, writes=w)

    def copy(self, eng, out, in_, r, w):
        if eng == "act":
            self.act(out, in_, AF.Copy, r, w)
        else:
            self.p.op(eng, lambda e, o=out, x=in_: e.tensor_copy(o, x), reads=r, writes=w)

    def memset(self, eng, ap, val, w):
        self.p.op(eng, lambda e, a=ap, v=val: e.memset(a, v), reads=(), writes=w)

    def recip(self, out, in_, r, w):
        self.p.op("dve", lambda e, o=out, x=in_: e.reciprocal(o, x), reads=r, writes=w)

    def dma(self, eng, out, in_, r=(), w=(), **kw):
        self.p.dma(eng, out, in_, reads=r, writes=w, **kw)

    def veng(self):
        self.alt += 1
        return "dve" if self.alt % 2 else "pool"

    def load_w(self, tile, dram, K, M, m0=0, mw=None, eng="pool"):
        kc = K // 128
        mw = M if mw is None else mw
        src = dram.rearrange("(k p) m -> p k m", p=128)
        step = 1024
        keys = []
        for a in range(0, mw, step):
            b = min(mw, a + step)
            self.dma(eng, tile[:, 0:kc, a:b], src[:, :, m0 + a:m0 + b], w=[(tile, "w", a)])
            keys.append((tile, "w", a))
        self.wkeys[tile.name] = keys
        return keys

    def wk(self, tile):
        return self.wkeys[tile.name]

    def rmsnorm(self, src_chunks, src_keys, nck, Dn, gains, gc, out_tile, out_key, W=TT):
        c = self.c
        sq = c["sq_ring"].next()
        for k in range(nck):
            self.act(sq[:, k, :W], src_chunks[k], AF.Square, r=[src_keys[k]], w=[(sq, k)])
        ps = c["ps_ring"].next()
        for k in range(nck):
            self.mm(ps[:, :W], c["ones_bf"][:], sq[:, k, :W], k == 0, k == nck - 1, r=[(sq, k), c["ones_bf"]], w=[ps])
        sd = c["sd_ring"].next()
        self.act(sd[:, :W], ps[:, :W], AF.Sqrt, r=[ps], w=[sd], bias=c["cst"][:, 2:3], scale=1.0 / Dn)
        rs = c["rs_ring"].next()
        self.recip(rs[:, :W], sd[:, :W], r=[sd], w=[rs])
        for k in range(nck):
            self.stt("dve", out_tile[:, k, :W], src_chunks[k], gains[:, gc + k:gc + k + 1], rs[:, :W],
                     ALU.mult, ALU.mult, r=[src_keys[k], rs], w=[(out_key, k)])
        return rs


G_MIX0, G_FFN0, G_MIX1, G_FFN1, G_FIN, G_QN, G_KVN = 0, 8, 16, 24, 32, 40, 43
RB = 2048


def norm_rings(kb, p, nps=None):
    c = kb.c
    c["sq_ring"] = Ring(p, "sq", 2, [128, 8, TT], BF16)
    c["sd_ring"] = Ring(p, "sd", 2, [128, TT], F32)
    c["rs_ring"] = Ring(p, "rs", 2, [128, TT], F32)


def phase_kvq(kb, I):
    p, c = kb.p, kb.c
    gains, cst = c["gains"], c["cst"]
    kT, qT, vS = I["kT"], I["qT"], I["vS"]
    with p.phase():
        ropeC = kb.dscr("ropeC", [128, S], F32)
        ropeS = kb.dscr("ropeS", [128, S], F32)
        with ExitStack() as tmp:
            p.stack, sv2 = tmp, p.stack
            posi = p.sb("posi", [128, RB], I32)
            ang = p.sb("ang", [128, RB], F32)
            t1 = p.sb("rt1", [128, RB], F32)
            t2 = p.sb("rt2", [128, RB], F32)
            ki = p.sb("rki", [128, RB], I32)
            tabt = p.sb("tabt", [128, RB], F32)
            for rb in range(S // RB):
                rc_ = slice(rb * RB, (rb + 1) * RB)
                kb.dma("sp", posi[:], I["pos128"][:, rc_], w=[posi])
                kb.copy("dve", ang[:], posi[:], r=[posi], w=[ang])
                kb.ts("dve", ang[:], ang[:], cst[:, 0:1], ALU.mult, r=[ang, cst], w=[ang])
                for (tab, shift, sgn) in ((ropeS, 0.0, True), (ropeC, math.pi / 2, False)):
                    kb.ts("dve", t1[:], ang[:], shift, ALU.add, r=[ang], w=[t1])
                    kb.ts("dve", t2[:], t1[:], 1.0 / TWO_PI, ALU.mult, r=[t1], w=[t2])
                    kb.copy("dve", ki[:], t2[:], r=[t2], w=[ki])
                    kb.copy("dve", t2[:], ki[:], r=[ki], w=[t2])
                    kb.stt("dve", t1[:], t2[:], -TWO_PI, t1[:], ALU.mult, ALU.add, r=[t1, t2], w=[t1])
                    kb.ts("dve", t2[:], t1[:], math.pi, ALU.is_gt, r=[t1], w=[t2])
                    kb.stt("dve", t1[:], t2[:], -TWO_PI, t1[:], ALU.mult, ALU.add, r=[t1, t2], w=[t1])
                    kb.ts("dve", t2[:], t1[:], -math.pi, ALU.is_lt, r=[t1], w=[t2])
                    kb.stt("dve", t1[:], t2[:], TWO_PI, t1[:], ALU.mult, ALU.add, r=[t1, t2], w=[t1])
                    kb.ts("dve", t1[:], t1[:], math.pi, ALU.min, r=[t1], w=[t1], s2=-math.pi, op1=ALU.max)
                    kb.act(tabt[:], t1[:], AF.Sin, r=[t1], w=[tabt])
                    if sgn:
                        kb.ts("dve", tabt[:], tabt[:], cst[:, 1:2], ALU.mult, r=[tabt, cst], w=[tabt])
                    kb.dma("sp", tab[:, rc_], tabt[:], r=[tabt], w=[("rope", rb)])
            kbf = p.sb("kbf", [128, S // 128], F32)
            kbb = p.sb("kbb", [128, S // 128], BF16)
            kb.dma("sp", kbf[:], I["kbias"], w=[kbf])
            kb.copy("dve", kbb[:], kbf[:], r=[kbf], w=[kbb])
            for h in range(H):
                kb.dma("sp", kT[h, 96:97, :].rearrange("o (p j) -> (o p) j", p=128), kbb[:], r=[kbb], w=[("kT", h, "b")])
            qm1 = p.sb("qm1", [128, T // 128], BF16)
            kb.memset("dve", qm1[:], -1.0, w=[qm1])
            for h in range(H):
                kb.dma("sp", qT[h, 96:97, :].rearrange("o (p j) -> (o p) j", p=128), qm1[:], r=[qm1], w=[("qT", h, "b")])
            p.flush()
            p.stack = sv2
        psr = c["ps_ring"] = Ring(p, "ps", 6, [128, 512], F32, psum=True)
        norm_rings(kb, p)
        W_in_q = p.sb("W_in_q", [128, 8, 384], BF16)
        W_in_kv = p.sb("W_in_kv", [128, 8, 256], BF16)
        W_in_pe = p.sb("W_in_pe", [128, 8, 32], BF16)
        W_in_pesw = p.sb("W_in_pesw", [128, 8, 32], BF16)
        W_uq_nope = p.sb("W_uq_nope", [128, 3, 1024], BF16)
        W_uq_pe = p.sb("W_uq_pe", [128, 3, 512], BF16)
        W_uq_pesw = p.sb("W_uq_pesw", [128, 3, 512], BF16)
        W_uk = p.sb("W_uk", [128, 2, 1024], BF16)
        W_uv = p.sb("W_uv", [128, 2, 1024], BF16)
        kb.load_w(W_in_kv, I["w_in_kv"], D, 256)
        kb.load_w(W_in_pe, I["w_in_pe"], D, 32)
        kb.load_w(W_in_pesw, I["w_in_pesw"], D, 32)
        kb.load_w(W_uk, I["w_uk"], 256, 1024)
        kb.load_w(W_uv, I["w_uv"], 256, 1024)
        kb.load_w(W_in_q, I["w_in_q"], D, 384)
        kb.load_w(W_uq_nope, I["w_uq_nope"], 384, 1024)
        kb.load_w(W_uq_pe, I["w_uq_pe"], 384, 512)
        kb.load_w(W_uq_pesw, I["w_uq_pesw"], 384, 512)
        ct_ring = Ring(p, "ct", 3, [128, TT], F32)
        st_ring = Ring(p, "st", 3, [128, TT], F32)
        x_ring = Ring(p, "xt", 2, [128, 8, TT], F32)
        hn_ring = Ring(p, "hn", 3, [128, 8, TT], BF16)
        lat_ring = Ring(p, "lat", 3, [128, 3, TT], F32)
        latn_ring = Ring(p, "latn", 3, [128, 3, TT], BF16)
        ev_ring = Ring(p, "ev", 4, [128, TT], BF16)
        rp_ring = Ring(p, "rp", 4, [128, TT], F32)
        vt_ring = Ring(p, "vt", 2, [128, 1024], BF16)
        xTv = I["xT"].rearrange("(k p) t -> p k t", p=128)
        n_tiles = S // TT
        st1 = {}

        def stage1(tt):
            cols = slice(tt * TT, (tt + 1) * TT)
            xt = x_ring.next()
            kb.dma("sp", xt[:], xTv[:, :, cols], w=[xt])
            Ct = ct_ring.next()
            St = st_ring.next()
            kb.dma("sp", Ct[:], ropeC[:, cols], w=[Ct])
            kb.dma("sp", St[:], ropeS[:, cols], w=[St])
            hn = hn_ring.next()
            kb.rmsnorm([xt[:, k, :] for k in range(8)], [xt] * 8, 8, D, gains, G_MIX0, hn, hn)
            st1[tt] = (hn, Ct, St)

        stage1(0)
        for tt in range(n_tiles):
            cols = slice(tt * TT, (tt + 1) * TT)
            own = tt >= n_tiles // 2
            ocols = slice((tt - n_tiles // 2) * TT, (tt - n_tiles // 2 + 1) * TT)
            if tt + 1 < n_tiles:
                stage1(tt + 1)
            hn, Ct, St = st1.pop(tt)
            hnk = [(hn, k) for k in range(8)]
            lat = lat_ring.next()
            for m in range(2):
                ps = psr.next()
                for k in range(8):
                    kb.mm(ps[:], W_in_kv[:, k, m * 128:(m + 1) * 128], hn[:, k, :], k == 0, k == 7,
                          r=[hnk[k]] + kb.wk(W_in_kv), w=[ps])
                kb.copy("act", lat[:, m, :], ps[:], r=[ps], w=[(lat, m)])
            if own:
                latq = lat_ring.next()
                for m in range(3):
                    ps = psr.next()
                    for k in range(8):
                        kb.mm(ps[:], W_in_q[:, k, m * 128:(m + 1) * 128], hn[:, k, :], k == 0, k == 7,
                              r=[hnk[k]] + kb.wk(W_in_q), w=[ps])
                    kb.copy("act", latq[:, m, :], ps[:], r=[ps], w=[(latq, m)])
            latn = latn_ring.next()
            kb.rmsnorm([lat[:, m, :] for m in range(2)], [(lat, m) for m in range(2)], 2, 256, gains, G_KVN, latn, latn)
            if own:
                latnq = latn_ring.next()
                kb.rmsnorm([latq[:, m, :] for m in range(3)], [(latq, m) for m in range(3)], 3, 384, gains, G_QN, latnq, latnq)
            psa = psr.next()
            psb = psr.next()
            for k in range(8):
                kb.mm(psa[0:32, :], W_in_pe[:, k, :], hn[:, k, :], k == 0, k == 7, r=[hnk[k]] + kb.wk(W_in_pe), w=[psa])
            for k in range(8):
                kb.mm(psb[0:32, :], W_in_pesw[:, k, :], hn[:, k, :], k == 0, k == 7, r=[hnk[k]] + kb.wk(W_in_pesw), w=[psb])
            r1 = rp_ring.next()
            r2 = rp_ring.next()
            kb.tt("dve", r1[0:32, :], psa[0:32, :], Ct[0:32, :], ALU.mult, r=[psa, Ct], w=[r1])
            kb.tt("dve", r2[0:32, :], psb[0:32, :], St[0:32, :], ALU.mult, r=[psb, St], w=[r2])
            kpe = ev_ring.next()
            kb.tt("pool", kpe[0:32, :], r1[0:32, :], r2[0:32, :], ALU.add, r=[r1, r2], w=[kpe])
            _src = kpe[0:32, :]
            _srcb = AP(_src.tensor, _src.offset, [list(_src.ap[0]), [0, H], list(_src.ap[1])])
            kb.dma("sp", kT[:, 64:96, cols].rearrange("h r t -> r h t"), _srcb, r=[kpe],
                   w=[("kT", h, tt, "pe") for h in range(H)])
            for j in range(8):
                ps = psr.next()
                for kc in range(2):
                    kb.mm(ps[:], W_uk[:, kc, j * 128:(j + 1) * 128], latn[:, kc, :], kc == 0, kc == 1,
                          r=[(latn, kc)] + kb.wk(W_uk), w=[ps])
                kn = ev_ring.next()
                kb.copy("act", kn[:], ps[:], r=[ps], w=[kn])
                kb.dma("sp", kT[2 * j, 0:64, cols], kn[0:64, :], r=[kn], w=[("kT", 2 * j, tt, "n")])
                kb.dma("sp", kT[2 * j + 1, 0:64, cols], kn[64:128, :], r=[kn], w=[("kT", 2 * j + 1, tt, "n")])
            for tb in range(4):
                vt = vt_ring.next()
                for nb in range(2):
                    ps = psr.next()
                    for kc in range(2):
                        kb.mm(ps[:], latn[:, kc, tb * 128:(tb + 1) * 128], W_uv[:, kc, nb * 512:(nb + 1) * 512],
                              kc == 0, kc == 1, r=[(latn, kc)] + kb.wk(W_uv), w=[ps])
                    kb.copy("dve", vt[:, nb * 512:(nb + 1) * 512], ps[:], r=[ps], w=[(vt, nb)])
                r0 = tt * TT + tb * 128
                kb.dma("sp", vS[r0:r0 + 128, :], vt[:], r=[(vt, 0), (vt, 1)], w=[("vS", tt, tb)])
            if own:
                latn = latnq
                for j in range(8):
                    ps = psr.next()
                    for kc in range(3):
                        kb.mm(ps[:], W_uq_nope[:, kc, j * 128:(j + 1) * 128], latn[:, kc, :], kc == 0, kc == 2,
                              r=[(latn, kc)] + kb.wk(W_uq_nope), w=[ps])
                    qn = ev_ring.next()
                    kb.copy("act", qn[:], ps[:], r=[ps], w=[qn])
                    kb.dma("sp", qT[2 * j, 0:64, ocols], qn[0:64, :], r=[qn], w=[("qT", 2 * j, tt, "n")])
                    kb.dma("sp", qT[2 * j + 1, 0:64, ocols], qn[64:128, :], r=[qn], w=[("qT", 2 * j + 1, tt, "n")])
                for j in range(4):
                    psa = psr.next()
                    psb = psr.next()
                    for kc in range(3):
                        kb.mm(psa[:], W_uq_pe[:, kc, j * 128:(j + 1) * 128], latn[:, kc, :], kc == 0, kc == 2,
                              r=[(latn, kc)] + kb.wk(W_uq_pe), w=[psa])
                    for kc in range(3):
                        kb.mm(psb[:], W_uq_pesw[:, kc, j * 128:(j + 1) * 128], latn[:, kc, :], kc == 0, kc == 2,
                              r=[(latn, kc)] + kb.wk(W_uq_pesw), w=[psb])
                    r1 = rp_ring.next()
                    r2 = rp_ring.next()
                    kb.tt("dve", r1[:], psa[:], Ct[:], ALU.mult, r=[psa, Ct], w=[r1])
                    kb.tt("dve", r2[:], psb[:], St[:], ALU.mult, r=[psb, St], w=[r2])
                    qpe = ev_ring.next()
                    kb.tt("pool", qpe[:], r1[:], r2[:], ALU.add, r=[r1, r2], w=[qpe])
                    for hh in range(4):
                        kb.dma("sp", qT[4 * j + hh, 64:96, ocols], qpe[32 * hh:32 * hh + 32, :], r=[qpe],
                               w=[("qT", 4 * j + hh, tt, "pe")])


def phase_attn(kb, I):
    p, c = kb.p, kb.c
    gains = c["gains"]
    kT, qT, vS = I["kT"], I["qT"], I["vS"]
    scale = 1.0 / math.sqrt(96.0)
    n_tiles = S // TT
    with p.phase():
        oT = p.sb("oT", [128, 8, T], BF16)
        pss_ring = Ring(p, "pss", 5, [128, TT], F32, psum=True)
        W_o = p.sb("W_o", [128, 8, D], BF16)
        kb.load_w(W_o, I["w_o"], D, D)
        with ExitStack() as tmp:
            p.stack, sv2 = tmp, p.stack
            masks = p.sb("masks", [128, 4, TT], BF16)
            kb.memset("pool", masks[:], 1.0, w=[masks])
            for d in range(4):
                p.op("pool", lambda e, d=d: e.affine_select(masks[:, d, :], masks[:, d, :], [[1, TT]], ALU.is_ge, 0.0,
                                                             base=-128 * d, channel_multiplier=-1),
                     reads=[masks], writes=[masks])
            K_ring = Ring(p, "Kh", 2, [97, S], BF16)
            Q_ring = Ring(p, "Qh", 2, [97, T], BF16)
            V_ring = Ring(p, "Vh", 2, [128, S // 128, 128], BF16)
            for vt in V_ring.tiles:
                kb.memset("pool", vt[:, :, 64:128], 1.0, w=[(vt, "ones")])
            pt_ring = Ring(p, "pt", 9, [128, TT], BF16)
            pso_ring = Ring(p, "pso", 2, [128, TT], F32, psum=True)
            rc_ring = Ring(p, "rc", 2, [64, TT], F32)
            LA = 4
            heads = {}

            def load_head(h):
                Kh = K_ring.next()
                Qh = Q_ring.next()
                Vh = V_ring.next()
                kb.dma("sp", Kh[:], kT[h], w=[Kh])
                kb.dma("sp", Qh[:], qT[h], w=[Qh])
                vsrc = vS[:, h * 64:(h + 1) * 64].rearrange("(kt p) d -> p kt d", p=128)
                for a in range(0, S // 128, 16):
                    kb.dma("sp", Vh[:, a:a + 16, 0:64], vsrc[:, a:a + 16, :], w=[(Vh, "v", a)])
                vkeys = [(Vh, "v", a) for a in range(0, S // 128, 16)] + [(Vh, "ones")]
                heads[h] = (Kh, Qh, Vh, vkeys)

            tiles = []
            for h in range(kb.nheads):
                for qt in range(T // TT):
                    nk = (T + TT * (qt + 1)) // 128
                    for kt in range(nk):
                        tiles.append((h, qt, kt, nk))
            load_head(0)
            pts = {}
            psos = {}
            for i in range(len(tiles) + LA):
                if i < len(tiles):
                    h, qt, kt, nk = tiles[i]
                    Kh, Qh, Vh, vkeys = heads[h]
                    pss = pss_ring.next()
                    kb.mm(pss[:], Kh[:, kt * 128:(kt + 1) * 128], Qh[:, qt * TT:(qt + 1) * TT], True, True,
                          r=[Kh, Qh], w=[pss])
                    pt = pt_ring.next()
                    kb.act(pt[:], pss[:], AF.Exp, r=[pss], w=[pt], scale=scale)
                    dd = kt - (nk - 4)
                    if dd >= 0:
                        kb.tt("pool", pt[:], pt[:], masks[:, dd, :], ALU.mult, r=[pt, masks], w=[pt])
                    pts[i] = pt
                if i >= LA:
                    j = i - LA
                    h, qt, kt, nk = tiles[j]
                    Kh, Qh, Vh, vkeys = heads[h]
                    if kt == 0:
                        psos[(h, qt)] = pso_ring.next()
                    pso = psos[(h, qt)]
                    pt = pts.pop(j)
                    kb.mm(pso[:], Vh[:, kt, :], pt[:], kt == 0, kt == nk - 1, r=vkeys + [pt], w=[pso])
                    if qt == 0 and kt == 0 and h + 1 < kb.nheads:
                        load_head(h + 1)
                    if kt == nk - 1:
                        rc = rc_ring.next()
                        kb.recip(rc[:], pso[64:128, :], r=[pso], w=[rc])
                        po = (h % 2) * 64
                        kb.tt("dve", oT[po:po + 64, h // 2, qt * TT:(qt + 1) * TT], pso[0:64, :], rc[:], ALU.mult,
                              r=[pso, rc], w=[("oT", h // 2, qt, h % 2)])
            p.flush()
            p.stack = sv2
        c["ps_ring"] = pss_ring
        norm_rings(kb, p)
        x_ring = Ring(p, "xt", 2, [128, 8, TT], F32)
        h1_ring = Ring(p, "h1", 2, [128, 8, TT], F32)
        hn_ring = Ring(p, "hn", 2, [128, 8, TT], BF16)
        xTv = I["xT"].rearrange("(k p) t -> p k t", p=128)
        h1Tv = I["h1T"].rearrange("(k p) t -> p k t", p=128)
        hn2Tv = I["hn2T"].rearrange("(k p) t -> p k t", p=128)
        for qt in range(T // TT):
            cols = slice(qt * TT, (qt + 1) * TT)
            xt = x_ring.next()
            kb.dma("sp", xt[:], xTv[:, :, T + qt * TT:T + (qt + 1) * TT], w=[xt])
            h1 = h1_ring.next()
            for m in range(8):
                ps = pss_ring.next()
                for k in range(8):
                    kb.mm(ps[:], W_o[:, k, m * 128:(m + 1) * 128], oT[:, k, cols], k == 0, k == 7,
                          r=[("oT", k, qt, 0), ("oT", k, qt, 1)] + kb.wk(W_o), w=[ps])
                kb.tt("dve", h1[:, m, :], ps[:], xt[:, m, :], ALU.add, r=[ps, xt], w=[(h1, m)])
            kb.dma("sp", h1Tv[:, :, cols], h1[:], r=[(h1, m) for m in range(8)], w=[("h1T", qt)])
            hn = hn_ring.next()
            kb.rmsnorm([h1[:, k, :] for k in range(8)], [(h1, k) for k in range(8)], 8, D, gains, G_FFN0, hn, hn)
            kb.dma("sp", hn2Tv[:, :, cols], hn[:], r=[(hn, k) for k in range(8)], w=[("hn2T", qt)])


def ffn_phase(kb, experts, FF, hnT, accT, outT, comb=None, final_norm=None):
    p, c = kb.p, kb.c
    nff = FF // 128
    BLK = 4
    blocks = [(b, min(BLK, nff - b)) for b in range(0, nff, BLK)]
    TS = 2048
    nq = TS // TT
    with p.phase():
        hn = p.sb("f_hn", [128, 8, TS], BF16)
        acc = p.sb("f_acc", [128, 8, TS], F32)
        psg_ring = Ring(p, "psg", 2, [128, TT], F32, psum=True)
        psu_ring = Ring(p, "psu", 2, [128, TT], F32, psum=True)
        psd_ring = Ring(p, "psd", 3, [128, TT], F32, psum=True)
        sg_ring = Ring(p, "sg", 3, [128, TT], F32)
        a_ring = Ring(p, "a", 2, [128, BLK, TT], BF16)
        wg_ring = Ring(p, "wg", 2, [128, 8, BLK * 128], BF16)
        wu_ring = Ring(p, "wu", 2, [128, 8, BLK * 128], BF16)
        wd_ring = Ring(p, "wd", 3, [128, BLK, D], BF16)
        hnv = hnT.rearrange("(k p) t -> p k t", p=128)
        accv = accT.rearrange("(k p) t -> p k t", p=128)
        outv = outT.rearrange("(k p) t -> p k t", p=128)
        def emit_down(a, Wd, nb, qt, l):
            for m in range(8):
                psd = psd_ring.next()
                for f in range(nb):
                    kb.mm(psd[:], Wd[:, f, m * 128:(m + 1) * 128], a[:, f, :], f == 0, f == nb - 1,
                          r=[(a, f)] + kb.wk(Wd), w=[psd])
                kb.tt("dve", acc[:, m, l], psd[:], acc[:, m, l], ALU.add, r=[psd, (acc, qt, m)],
                      w=[(acc, qt, m)])

        pend = None
        for sup in range(T // TS):
            for qt in range(nq):
                g = slice(sup * TS + qt * TT, sup * TS + (qt + 1) * TT)
                l = slice(qt * TT, (qt + 1) * TT)
                kb.dma("sp", hn[:, :, l], hnv[:, :, g], w=[(hn, qt)])
                kb.dma("sp", acc[:, :, l], accv[:, :, g], w=[(acc, qt, m) for m in range(8)])
            for ei, (wg_d, wu_d, wd_d) in enumerate(experts):
                for (b0, nb) in blocks:
                    Wg = wg_ring.next()
                    Wu = wu_ring.next()
                    Wd = wd_ring.next()
                    kb.load_w(Wg, wg_d, D, FF, m0=b0 * 128, mw=nb * 128)
                    kb.load_w(Wu, wu_d, D, FF, m0=b0 * 128, mw=nb * 128)
                    kb.load_w(Wd, wd_d[b0 * 128:(b0 + nb) * 128, :], nb * 128, D)
                    for qt in range(nq):
                        l = slice(qt * TT, (qt + 1) * TT)
                        a = a_ring.next()
                        for f in range(nb):
                            psg = psg_ring.next()
                            psu = psu_ring.next()
                            for k in range(8):
                                kb.mm(psg[:], Wg[:, k, f * 128:(f + 1) * 128], hn[:, k, l], k == 0, k == 7,
                                      r=[(hn, qt)] + kb.wk(Wg), w=[psg])
                            for k in range(8):
                                kb.mm(psu[:], Wu[:, k, f * 128:(f + 1) * 128], hn[:, k, l], k == 0, k == 7,
                                      r=[(hn, qt)] + kb.wk(Wu), w=[psu])
                            sg = sg_ring.next()
                            kb.act(sg[:], psg[:], AF.Silu, r=[psg], w=[sg])
                            if comb is None:
                                kb.tt("dve", a[:, f, :], psu[:], sg[:], ALU.mult, r=[psu, sg], w=[(a, f)])
                            else:
                                bc = comb(sup, qt, ei)
                                kb.tt("pool", sg[:], sg[:], bc[0], ALU.mult, r=[sg, bc[1]], w=[sg])
                                kb.tt("dve", a[:, f, :], psu[:], sg[:], ALU.mult, r=[psu, sg], w=[(a, f)])
                        emit_down(a, Wd, nb, qt, l)
            if pend is not None:
                emit_down(*pend)
                pend = None
            for qt in range(nq):
                g = slice(sup * TS + qt * TT, sup * TS + (qt + 1) * TT)
                l = slice(qt * TT, (qt + 1) * TT)
                if final_norm is None:
                    kb.dma("sp", outv[:, :, g], acc[:, :, l], r=[(acc, qt, m) for m in range(8)], w=[("fout", sup, qt)])
                else:
                    final_norm(sup, qt, acc, l, outv, g)


def make_inputs_l0(kb):
    I = {}
    I["xT"] = kb.din("xT", [D, S])
    I["pos128"] = kb.din("pos128", [128, S], I32)
    I["kbias"] = kb.din("kbias", [128, S // 128])
    I["gains_d"] = kb.din("gains", [128, 48])
    I["cst_d"] = kb.din("cst", [128, 8])
    for name, shp in (("w_in_q", [D, 384]), ("w_in_kv", [D, 256]), ("w_in_pe", [D, 32]), ("w_in_pesw", [D, 32]),
                      ("w_uq_nope", [384, 1024]), ("w_uq_pe", [384, 512]), ("w_uq_pesw", [384, 512]),
                      ("w_uk", [256, 1024]), ("w_uv", [256, 1024]), ("w_o", [D, D]),
                      ("ffn_wg", [D, 2816]), ("ffn_wu", [D, 2816]), ("ffn_wd", [2816, D])):
        I[name] = kb.din(name, shp)
    return I


def setup_consts(kb, I):
    p, c = kb.p, kb.c
    c["gains"] = p.sb("gains", [128, 48], F32)
    c["cst"] = p.sb("cst", [128, 8], F32)
    c["ones_bf"] = p.sb("ones_bf", [128, 128], BF16)
    kb.dma("sp", c["gains"][:], I["gains_d"], w=[c["gains"]])
    kb.dma("sp", c["cst"][:], I["cst_d"], w=[c["cst"]])
    kb.memset("pool", c["ones_bf"][:], 1.0, w=[c["ones_bf"]])


def build_l0(stop_after="all", nheads=H):
    nc = bass.Bass("TRN2", target_bir_lowering=False)
    kb = KB(nc)
    kb.c = {}
    kb.nheads = nheads
    p = kb.p
    I = make_inputs_l0(kb)
    dbg = stop_after != "all"
    mk = (lambda n, s, d: kb.dout(n, s, d)) if stop_after == "kv" else (lambda n, s, d: kb.dscr(n, s, d))
    I["kT"] = mk("kT", [H, 97, S], BF16)
    I["qT"] = mk("qT", [H, 97, T], BF16)
    I["vS"] = mk("vS", [S, 1024], BF16)
    mk = (lambda n, s, d: kb.dout(n, s, d)) if stop_after == "attn" else (lambda n, s, d: kb.dscr(n, s, d))
    I["h1T"] = mk("h1T", [D, T], F32)
    I["hn2T"] = mk("hn2T", [D, T], BF16)
    I["h2T"] = kb.dout("h2T", [D, T], F32)
    setup_consts(kb, I)
    phase_kvq(kb, I)
    if stop_after != "kv":
        phase_attn(kb, I)
        if stop_after != "attn":
            ffn_phase(kb, [(I["ffn_wg"], I["ffn_wu"], I["ffn_wd"])], 2816, I["hn2T"], I["h1T"], I["h2T"])
    p.finish()
    return nc, kb


def host_inputs_l0(inp, core):
    b, hf = core // 2, core % 2
    x = inp["x"][b]
    pos = inp["positions"][b]
    xT = np.zeros((D, S), np.float32)
    posl = np.zeros((S,), np.int32)
    kbias = np.zeros((S,), np.float32)
    if hf == 1:
        xT[:] = x.T
        posl[:] = pos
    else:
        xT[:, T:] = x[:T].T
        posl[T:] = pos[:T]
        kbias[:T] = NEGBIG
    m = {}
    m["xT"] = xT
    m["pos128"] = np.ascontiguousarray(np.broadcast_to(posl[None, :], (128, S)))
    m["kbias"] = np.ascontiguousarray(kbias.reshape(128, S // 128))
    return m


def host_weights_l0(inp):
    m = {}
    g = np.zeros((128, 48), np.float32)

    def col(v):
        return v.reshape(-1, 128).T
    g[:, 0:8] = col(inp["mix_norm"][0])
    g[:, 8:16] = col(inp["ffn_norm"][0])
    g[:, 16:24] = col(inp["mix_norm"][1])
    g[:, 24:32] = col(inp["ffn_norm"][1])
    g[:, 32:40] = col(inp["final_norm"])
    g[:, 40:43] = col(inp["mla_q_norm"][0])
    g[:, 43:45] = col(inp["mla_kv_norm"][0])
    m["gains"] = g
    cst = np.zeros((128, 8), np.float32)
    r = np.arange(128)
    cst[:, 0] = (10000.0 ** (-(np.arange(0, 32, 2, dtype=np.float32)) / 32.0)).astype(np.float32)[r % 16]
    cst[:, 1] = np.where((r % 32) < 16, -1.0, 1.0)
    cst[:, 2] = EPS
    m["cst"] = cst
    w_in = inp["mla_w_in"][0]
    m["w_in_q"] = np.ascontiguousarray(w_in[:, :384])
    m["w_in_kv"] = np.ascontiguousarray(w_in[:, 384:640])
    pe = w_in[:, 640:672]
    m["w_in_pe"] = np.ascontiguousarray(pe)
    m["w_in_pesw"] = np.ascontiguousarray(np.concatenate([pe[:, 16:], pe[:, :16]], 1))
    wq = inp["mla_w_uq"][0].reshape(384, 16, 96)
    m["w_uq_nope"] = np.ascontiguousarray(wq[:, :, :64].reshape(384, 1024))
    qpe = wq[:, :, 64:]
    m["w_uq_pe"] = np.ascontiguousarray(qpe.reshape(384, 512))
    m["w_uq_pesw"] = np.ascontiguousarray(np.concatenate([qpe[:, :, 16:], qpe[:, :, :16]], 2).reshape(384, 512))
    wkv = inp["mla_w_ukv"][0].reshape(256, 16, 128)
    m["w_uk"] = np.ascontiguousarray(wkv[:, :, :64].reshape(256, 1024))
    m["w_uv"] = np.ascontiguousarray(wkv[:, :, 64:].reshape(256, 1024))
    m["w_o"] = np.ascontiguousarray(inp["mla_w_o"][0])
    m["ffn_wg"] = np.ascontiguousarray(inp["ffn_w_gate"][0])
    m["ffn_wu"] = np.ascontiguousarray(inp["ffn_w_up"][0])
    m["ffn_wd"] = np.ascontiguousarray(inp["ffn_w_down"][0])
    return m

NCH = T // 8
XW = NCH + 2


def sincos(kb, p, ang, out_sin, out_cos, W, tag):
    t1 = p.sb(tag + "t1", [128, W], F32)
    t2 = p.sb(tag + "t2", [128, W], F32)
    ki = p.sb(tag + "ki", [128, W], I32)
    for (out, shift) in ((out_sin, 0.0), (out_cos, math.pi / 2)):
        kb.ts("dve", t1[:], ang[:], shift, ALU.add, r=[ang], w=[t1])
        kb.ts("dve", t2[:], t1[:], 1.0 / TWO_PI, ALU.mult, r=[t1], w=[t2])
        kb.copy("dve", ki[:], t2[:], r=[t2], w=[ki])
        kb.copy("dve", t2[:], ki[:], r=[ki], w=[t2])
        kb.stt("dve", t1[:], t2[:], -TWO_PI, t1[:], ALU.mult, ALU.add, r=[t1, t2], w=[t1])
        kb.ts("dve", t2[:], t1[:], math.pi, ALU.is_gt, r=[t1], w=[t2])
        kb.stt("dve", t1[:], t2[:], -TWO_PI, t1[:], ALU.mult, ALU.add, r=[t1, t2], w=[t1])
        kb.ts("dve", t2[:], t1[:], -math.pi, ALU.is_lt, r=[t1], w=[t2])
        kb.stt("dve", t1[:], t2[:], TWO_PI, t1[:], ALU.mult, ALU.add, r=[t1, t2], w=[t1])
        kb.ts("dve", t1[:], t1[:], math.pi, ALU.min, r=[t1], w=[t1], s2=-math.pi, op1=ALU.max)
        kb.act(out[:], t1[:], AF.Sin, r=[t1], w=[out])


def cmul(kb, p, outr, outi, ar, ai, br, bi, tmp, keys_r, keys_w):
    kb.tt("dve", outr, ar, br, ALU.mult, r=keys_r, w=keys_w[0:1])
    kb.tt("dve", tmp, ai, bi, ALU.mult, r=keys_r, w=[("tmp", _key(keys_w[0]))])
    kb.tt("dve", outr, outr, tmp, ALU.subtract, r=keys_w[0:1] + [("tmp", _key(keys_w[0]))], w=keys_w[0:1])
    kb.tt("dve", outi, ar, bi, ALU.mult, r=keys_r, w=keys_w[1:2])
    kb.tt("dve", tmp, ai, br, ALU.mult, r=keys_r + keys_w[0:1], w=[("tmp", _key(keys_w[0]))])
    kb.tt("dve", outi, outi, tmp, ALU.add, r=keys_w[1:2] + [("tmp", _key(keys_w[0]))], w=keys_w[1:2])


def s5_coefs(kb, p, lr_d, li_d, ldt_d, W, tag):
    lr = p.sb(tag + "lr", [128, W], F32)
    li = p.sb(tag + "li", [128, W], F32)
    dt = p.sb(tag + "dt", [128, W], F32)
    kb.dma("sp", lr[:], lr_d, w=[lr])
    kb.dma("sp", li[:], li_d, w=[li])
    kb.dma("sp", dt[:], ldt_d, w=[dt])
    kb.act(dt[:], dt[:], AF.Exp, r=[dt], w=[dt])
    mag = p.sb(tag + "mag", [128, W], F32)
    kb.tt("dve", mag[:], lr[:], dt[:], ALU.mult, r=[lr, dt], w=[mag])
    kb.act(mag[:], mag[:], AF.Exp, r=[mag], w=[mag])
    th = p.sb(tag + "th", [128, W], F32)
    kb.tt("dve", th[:], li[:], dt[:], ALU.mult, r=[li, dt], w=[th])
    sn = p.sb(tag + "sn", [128, W], F32)
    cs = p.sb(tag + "cs", [128, W], F32)
    sincos(kb, p, th, sn, cs, W, tag)
    apr = p.sb(tag + "apr", [128, 9, W], F32)
    api = p.sb(tag + "api", [128, 9, W], F32)
    kb.memset("dve", apr[:, 0, :], 1.0, w=[(apr, 0)])
    kb.memset("dve", api[:, 0, :], 0.0, w=[(api, 0)])
    kb.tt("dve", apr[:, 1, :], mag[:], cs[:], ALU.mult, r=[mag, cs], w=[(apr, 1)])
    kb.tt("dve", api[:, 1, :], mag[:], sn[:], ALU.mult, r=[mag, sn], w=[(api, 1)])
    tmp = p.sb(tag + "tmp", [128, W], F32)
    for n in range(2, 9):
        cmul(kb, p, apr[:, n, :], api[:, n, :], apr[:, n - 1, :], api[:, n - 1, :], apr[:, 1, :], api[:, 1, :], tmp[:],
             [(apr, n - 1), (api, n - 1), (apr, 1), (api, 1)], [(apr, n), (api, n)])
    den = p.sb(tag + "den", [128, W], F32)
    t2 = p.sb(tag + "t2", [128, W], F32)
    kb.tt("dve", den[:], lr[:], lr[:], ALU.mult, r=[lr], w=[den])
    kb.tt("dve", t2[:], li[:], li[:], ALU.mult, r=[li], w=[t2])
    kb.tt("dve", den[:], den[:], t2[:], ALU.add, r=[den, t2], w=[den])
    kb.recip(den[:], den[:], r=[den], w=[den])
    am1 = p.sb(tag + "am1", [128, W], F32)
    kb.ts("dve", am1[:], apr[:, 1, :], -1.0, ALU.add, r=[(apr, 1)], w=[am1])
    gr = p.sb(tag + "gr", [128, W], F32)
    gi = p.sb(tag + "gi", [128, W], F32)
    kb.tt("dve", gr[:], am1[:], lr[:], ALU.mult, r=[am1, lr], w=[gr])
    kb.tt("dve", t2[:], api[:, 1, :], li[:], ALU.mult, r=[(api, 1), li], w=[t2])
    kb.tt("dve", gr[:], gr[:], t2[:], ALU.add, r=[gr, t2], w=[gr])
    kb.tt("dve", gr[:], gr[:], den[:], ALU.mult, r=[gr, den], w=[gr])
    kb.tt("dve", gi[:], api[:, 1, :], lr[:], ALU.mult, r=[(api, 1), lr], w=[gi])
    kb.tt("dve", t2[:], am1[:], li[:], ALU.mult, r=[am1, li], w=[t2])
    kb.tt("dve", gi[:], gi[:], t2[:], ALU.subtract, r=[gi, t2], w=[gi])
    kb.tt("dve", gi[:], gi[:], den[:], ALU.mult, r=[gi, den], w=[gi])
    wgr = p.sb(tag + "wgr", [128, 8, W], F32)
    wgi = p.sb(tag + "wgi", [128, 8, W], F32)
    for n in range(8):
        cmul(kb, p, wgr[:, n, :], wgi[:, n, :], apr[:, n, :], api[:, n, :], gr[:], gi[:], tmp[:],
             [(apr, n), (api, n), gr, gi], [(wgr, n), (wgi, n)])
    return apr, api, wgr, wgi


def bc_last(ap2, n):
    return AP(ap2.tensor, ap2.offset, [list(ap2.ap[0]), list(ap2.ap[1]), [0, n]])


def bc_mid(ap2, n):
    return AP(ap2.tensor, ap2.offset, [list(ap2.ap[0]), [0, n], list(ap2.ap[1])])


def make_inputs_l1(kb, first, skip=(), moe=True):
    I = {}
    if "gains" not in skip:
        I["gains_d"] = kb.din("gains", [128, 48])
        I["cst_d"] = kb.din("cst", [128, 8])
    for name, shp in (("s5_w_in", [D, D]), ("s5_w_glu", [D, 2 * D]),
                      ("lam_re_pm", [128, 32]), ("lam_im_pm", [128, 32]), ("ldt_pm", [128, 32]),
                      ("lam_re_fm", [128, 1024]), ("lam_im_fm", [128, 1024]), ("ldt_fm", [128, 1024]),
                      ("Bp_re", [128, 1024]), ("Bp_im", [128, 1024]), ("Ct_re", [128, 1024]), ("Ct_im", [128, 1024]),
                      ("Bt_re", [128, 1024]), ("Bt_im", [128, 1024]), ("d_col", [128, 8]),
                      ("x0", [128, 64]), ("sel", [8, 1024]), ("w_router", [128, 64])):
        if name not in skip:
            I[name] = kb.din(name, shp)
    for e in range(8 if moe else 0):
        I["moe_wg%d" % e] = kb.din("moe_wg%d" % e, [D, 3584])
        I["moe_wu%d" % e] = kb.din("moe_wu%d" % e, [D, 3584])
        I["moe_wd%d" % e] = kb.din("moe_wd%d" % e, [3584, D])
    return I


def phase_s5(kb, I, passes):
    p, c = kb.p, kb.c
    gains = c["gains"]
    uT, ygT = I["uT"], I["ygT"]
    with p.phase():
        psr = c["ps_ring"] = Ring(p, "ps", 4, [128, TT], F32, psum=True)
        norm_rings(kb, p)
        W_s = p.sb("W_s5in", [128, 8, D], BF16)
        kb.load_w(W_s, I["s5_w_in"], D, D)
        x_ring = Ring(p, "xt", 2, [128, 8, TT], F32)
        hn_ring = Ring(p, "hn", 2, [128, 8, TT], BF16)
        u_ring = Ring(p, "ut", 2, [128, 8, TT], BF16)
        h2v = I["h2T"].rearrange("(k p) t -> p k t", p=128)
        uTv = uT.rearrange("(k p) t -> p k t", p=128)
        for qt in range(T // TT):
            cols = slice(qt * TT, (qt + 1) * TT)
            xt = x_ring.next()
            kb.dma("sp", xt[:], h2v[:, :, cols], w=[xt])
            hn = hn_ring.next()
            kb.rmsnorm([xt[:, k, :] for k in range(8)], [xt] * 8, 8, D, gains, G_MIX1, hn, hn)
            ut = u_ring.next()
            for m in range(8):
                ps = psr.next()
                for k in range(8):
                    kb.mm(ps[:], W_s[:, k, m * 128:(m + 1) * 128], hn[:, k, :], k == 0, k == 7,
                          r=[(hn, k)] + kb.wk(W_s), w=[ps])
                kb.copy("act", ut[:, m, :], ps[:], r=[ps], w=[(ut, m)])
            kb.dma("sp", uTv[:, :, cols], ut[:], r=[(ut, m) for m in range(8)], w=[("uT", qt)])
    with p.phase():
        Win = p.sb("Win", [128, 8, 8, 2, 128], BF16)
        Kt = p.sb("Kt", [128, 8, 8, 128], BF16)
        Cf = p.sb("Cf", [128, 8, 32, 2, 32], BF16)
        PQ = p.sb("PQ", [128, 2, 2, 32], F32)
        ident = p.sb("ident", [128, 128], F32)
        kb.memset("pool", ident[:], 0.0, w=[ident])
        p.op("pool", lambda e: e.affine_select(ident[:], ident[:], [[1, 128]], ALU.not_equal, 1.0,
                                               base=0, channel_multiplier=-1), reads=[ident], writes=[ident])
        for blk in range(4):
          with ExitStack() as tmp:
            p.stack, sv2 = tmp, p.stack
            cs_ = slice(blk * 256, (blk + 1) * 256)
            apr, api, wgr, wgi = s5_coefs(kb, p, I["lam_re_fm"][:, cs_], I["lam_im_fm"][:, cs_], I["ldt_fm"][:, cs_], 256, "fm%d" % blk)
            Btr = p.sb("Btr", [128, 256], F32)
            Bti = p.sb("Bti", [128, 256], F32)
            kb.dma("sp", Btr[:], I["Bt_re"][:, cs_], w=[Btr])
            kb.dma("sp", Bti[:], I["Bt_im"][:, cs_], w=[Bti])
            w1 = p.sb("w1", [128, 256], F32)
            w2 = p.sb("w2", [128, 256], F32)
            for j in range(8):
                n = 7 - j
                kb.tt("dve", w1[:], wgr[:, n, :], Btr[:], ALU.mult, r=[(wgr, n), Btr], w=[w1])
                kb.tt("dve", w2[:], wgi[:, n, :], Bti[:], ALU.mult, r=[(wgi, n), Bti], w=[w2])
                kb.tt("dve", Win[:, j, 2 * blk:2 * blk + 2, 0, :], w1[:].rearrange("p (a b) -> p a b", a=2),
                      w2[:].rearrange("p (a b) -> p a b", a=2), ALU.subtract, r=[w1, w2], w=[(Win, j, 0)])
                kb.tt("dve", w1[:], wgr[:, n, :], Bti[:], ALU.mult, r=[(wgr, n), Bti], w=[w1])
                kb.tt("dve", w2[:], wgi[:, n, :], Btr[:], ALU.mult, r=[(wgi, n), Btr], w=[w2])
                kb.tt("dve", Win[:, j, 2 * blk:2 * blk + 2, 1, :], w1[:].rearrange("p (a b) -> p a b", a=2),
                      w2[:].rearrange("p (a b) -> p a b", a=2), ALU.add, r=[w1, w2], w=[(Win, j, 1)])
            p.flush()
            p.stack = sv2
        with ExitStack() as tmp:
            p.stack, sv2 = tmp, p.stack
            apr, api, wgr, wgi = s5_coefs(kb, p, I["lam_re_pm"], I["lam_im_pm"], I["ldt_pm"], 32, "pm")
            Bpr = p.sb("Bpr", [128, 32, 32], F32)
            Bpi = p.sb("Bpi", [128, 32, 32], F32)
            Ctr = p.sb("Ctr", [128, 32, 32], F32)
            Cti = p.sb("Cti", [128, 32, 32], F32)
            for (t_, n_) in ((Bpr, "Bp_re"), (Bpi, "Bp_im"), (Ctr, "Ct_re"), (Cti, "Ct_im")):
                kb.dma("sp", t_[:], I[n_].rearrange("p (a b) -> p a b", a=32), w=[t_])
            dcol = p.sb("dcol", [128, 8], F32)
            kb.dma("sp", dcol[:], I["d_col"], w=[dcol])
            kb.copy("dve", PQ[:, 0, 0, :], apr[:, 8, :], r=[(apr, 8)], w=[(PQ, 0)])
            kb.copy("dve", PQ[:, 0, 1, :], apr[:, 8, :], r=[(apr, 8)], w=[(PQ, 1)])
            kb.ts("dve", PQ[:, 1, 0, :], api[:, 8, :], -1.0, ALU.mult, r=[(api, 8)], w=[(PQ, 2)])
            kb.copy("dve", PQ[:, 1, 1, :], api[:, 8, :], r=[(api, 8)], w=[(PQ, 3)])
            c1 = p.sb("c1", [128, 32, 32], F32)
            c2 = p.sb("c2", [128, 32, 32], F32)
            for s in range(8):
                arb = bc_last(apr[:, s + 1, :], 32)
                aib = bc_last(api[:, s + 1, :], 32)
                kb.tt("dve", c1[:], Ctr[:], arb, ALU.mult, r=[Ctr, (apr, s + 1)], w=[c1])
                kb.tt("dve", c2[:], Cti[:], aib, ALU.mult, r=[Cti, (api, s + 1)], w=[c2])
                kb.tt("dve", Cf[:, s, :, 0, :], c1[:], c2[:], ALU.subtract, r=[c1, c2], w=[(Cf, s, 0)])
                kb.tt("dve", c1[:], Ctr[:], aib, ALU.mult, r=[Ctr, (api, s + 1)], w=[c1])
                kb.tt("dve", c2[:], Cti[:], arb, ALU.mult, r=[Cti, (apr, s + 1)], w=[c2])
                kb.stt("dve", Cf[:, s, :, 1, :], c1[:], -1.0, c2[:], ALU.mult, ALU.subtract, r=[c1, c2], w=[(Cf, s, 1)])
            WBr = p.sb("WBr", [128, 32, 32], BF16)
            WBi = p.sb("WBi", [128, 32, 32], BF16)
            Ctrb = p.sb("Ctrb", [128, 32, 32], BF16)
            Ctib = p.sb("Ctib", [128, 32, 32], BF16)
            kb.copy("dve", Ctrb[:], Ctr[:], r=[Ctr], w=[Ctrb])
            kb.copy("dve", Ctib[:], Cti[:], r=[Cti], w=[Ctib])
            kst = p.sb("kst", [128, 128], F32)
            psk_ring = Ring(p, "psk", 2, [128, 32], F32, psum=True)
            for tau in range(8):
                wrb = bc_last(wgr[:, tau, :], 32)
                wib = bc_last(wgi[:, tau, :], 32)
                kb.tt("dve", c1[:], Bpr[:], wrb, ALU.mult, r=[Bpr, (wgr, tau)], w=[c1])
                kb.tt("dve", c2[:], Bpi[:], wib, ALU.mult, r=[Bpi, (wgi, tau)], w=[c2])
                kb.tt("dve", WBr[:], c1[:], c2[:], ALU.subtract, r=[c1, c2], w=[WBr])
                kb.tt("dve", c1[:], Bpi[:], wrb, ALU.mult, r=[Bpi, (wgr, tau)], w=[c1])
                kb.tt("dve", c2[:], Bpr[:], wib, ALU.mult, r=[Bpr, (wgi, tau)], w=[c2])
                kb.stt("dve", WBi[:], c1[:], -1.0, c2[:], ALU.mult, ALU.subtract, r=[c1, c2], w=[WBi])
                for kk in range(8):
                    psk = psk_ring.next()
                    for q in range(4):
                        k = 4 * kk + q
                        p.op("pe", lambda e, o=psk[32 * q:32 * q + 32, :], l=WBr[:, k, :], rr=Ctrb[:, k, :], q=q:
                             e.matmul(o, l, rr, start=True, stop=False, tile_position=(0, 32 * q)),
                             reads=[WBr, Ctrb], writes=[psk])
                        p.op("pe", lambda e, o=psk[32 * q:32 * q + 32, :], l=WBi[:, k, :], rr=Ctib[:, k, :], q=q:
                             e.matmul(o, l, rr, start=False, stop=True, tile_position=(0, 32 * q)),
                             reads=[WBi, Ctib], writes=[psk])
                    if tau == 0:
                        kb.ts("dve", kst[:], ident[:], dcol[:, kk:kk + 1], ALU.mult, r=[ident, dcol], w=[kst])
                    else:
                        kb.memset("dve", kst[:], 0.0, w=[kst])
                    for q in range(4):
                        sl = slice(32 * q, 32 * q + 32)
                        kb.tt("dve", kst[sl, sl], kst[sl, sl], psk[sl, :], ALU.add, r=[kst, psk], w=[kst])
                    kb.copy("act", Kt[:, tau, kk, :], kst[:], r=[kst], w=[(Kt, tau, kk)])
            p.flush()
            p.stack = sv2
        X = p.sb("X", [128, 2, 32, XW], BF16)
        pse_ring = Ring(p, "pse", 3, [128, NCH], F32, psum=True)
        u_ring = Ring(p, "uc", 2, [128, T], BF16)
        uTv = uT.rearrange("(k p) t -> p k t", p=128)
        for kk in range(8):
            uc = u_ring.next()
            kb.dma("sp", uc[:], uTv[:, kk, :], w=[uc])
            ucv = uc[:].rearrange("p (c j) -> p j c", j=8)
            for q in range(4):
                for ri in range(2):
                    pse = pse_ring.next()
                    for j in range(8):
                        p.op("pe", lambda e, o=pse[:], l=Win[32 * q:32 * q + 32, j, kk, ri, :], rr=ucv[32 * q:32 * q + 32, j, :],
                             j=j, q=q: e.matmul(o, l, rr, start=(j == 0), stop=(j == 7), tile_position=(32 * q, 0)),
                             reads=[uc, (Win, j, ri)], writes=[pse])
                    kb.copy("act" if ri == 0 else "dve", X[:, ri, 4 * kk + q, 1:1 + NCH], pse[:], r=[pse], w=[("X", ri, 4 * kk + q)])
        p.flush()
        s_ring = Ring(p, "st", 3, [128, 2, 32], F32)
        t_ring = Ring(p, "tt", 3, [128, 2, 32], F32)
        t2_ring = Ring(p, "t2", 3, [128, 2, 32], F32)
        x0 = p.sb("x0", [128, 2, 32], F32)
        for pi, mode in enumerate(passes):
            Sc = s_ring.next()
            if mode == "zero":
                kb.memset("dve", Sc[:], 0.0, w=[Sc])
            elif mode == "cc":
                kb.dma("sp", x0[:], I["xg"][0:128, :].rearrange("p (a b) -> p a b", a=2), r=["xg"], w=[x0])
                kb.ts("dve", Sc[:], x0[:], c["cst"][:, 3:4], ALU.mult, r=[x0, c["cst"]], w=[Sc])
            else:
                kb.dma("sp", x0[:], I["x0"].rearrange("p (a b) -> p a b", a=2), w=[x0])
                kb.copy("dve", Sc[:], x0[:], r=[x0], w=[Sc])
            final = (pi == len(passes) - 1)
            if final:
                kb.copy("act", X[:, :, :, 0], Sc[:], r=[Sc], w=[("Xc", 0)])
            for cc in range(NCH):
                Sa = Sc[:]
                Ssw = AP(Sa.tensor, Sa.offset + 32, [list(Sa.ap[0]), [-32, 2], [1, 32]])
                t1 = t_ring.next()
                t2 = t2_ring.next()
                Sn = s_ring.next()
                kb.tt("dve", t1[:], PQ[:, 0, :, :], Sc[:], ALU.mult, r=[Sc], w=[t1])
                kb.tt("pool", t2[:], PQ[:, 1, :, :], Ssw, ALU.mult, r=[Sc], w=[t2])
                kb.tt("dve", t1[:], t1[:], X[:, :, :, 1 + cc], ALU.add, r=[t1, ("Xc", 1 + cc)], w=[t1])
                kb.tt("dve", Sn[:], t1[:], t2[:], ALU.add, r=[t1, t2], w=[Sn])
                if final:
                    kb.copy("act", X[:, :, :, 1 + cc], Sn[:], r=[Sn], w=[("Xc", 1 + cc)])
                Sc = Sn
            if I.get("xfin") is not None and len(passes) == 1:
                kb.dma("sp", I["xfin"].rearrange("p (a b) -> p a b", a=2), Sc[:], r=[Sc], w=["xfin"])
            if not final and passes[pi + 1] == "cc":
                kb.dma("sp", I["xi"].rearrange("p (a b) -> p a b", a=2), Sc[:], r=[Sc], w=["xi"])
                p.cc("AllGather", ALU.bypass, [[0, 1], [2, 3], [4, 5], [6, 7]], I["xi"], I["xg"], reads=["xi"], writes=["xg"])
        p.flush()
        if passes == ["zero", "fix"][:1] and I.get("s5_fix"):
            kb.dma("sp", I["xi"].rearrange("p (a b) -> p a b", a=2), Sc[:], r=[], w=["xi"])
            p.cc("AllGather", ALU.bypass, [[0, 1], [2, 3], [4, 5], [6, 7]], I["xi"], I["xg"], reads=["xi"], writes=["xg"])
            kb.dma("sp", x0[:], I["xg"][0:128, :].rearrange("p (a b) -> p a b", a=2), r=["xg"], w=[x0])
            kb.ts("dve", x0[:], x0[:], c["cst"][:, 3:4], ALU.mult, r=[x0, c["cst"]], w=[x0])
            with ExitStack() as tmp:
                p.stack, sv2 = tmp, p.stack
                NL = 10
                PW = p.sb("PW", [128, NL, 2, 2, 32], F32)
                kb.copy("dve", PW[:, 0].rearrange("p a b c -> p (a b c)"), PQ[:].rearrange("p a b c -> p (a b c)"), r=[], w=[("PW", 0)])
                ta = p.sb("pwa", [128, 32], F32)
                tb_ = p.sb("pwb", [128, 32], F32)
                for l in range(NL - 1):
                    ar = PW[:, l, 0, 0, :]
                    ai = PW[:, l, 1, 1, :]
                    kb.tt("dve", ta[:], ar, ar, ALU.mult, r=[("PW", l)], w=[ta])
                    kb.tt("dve", tb_[:], ai, ai, ALU.mult, r=[("PW", l)], w=[tb_])
                    kb.tt("dve", PW[:, l + 1, 0, 0, :], ta[:], tb_[:], ALU.subtract, r=[ta, tb_], w=[("PW", l + 1)])
                    kb.copy("dve", PW[:, l + 1, 0, 1, :], PW[:, l + 1, 0, 0, :], r=[("PW", l + 1)], w=[("PW", l + 1)])
                    kb.tt("dve", ta[:], ar, ai, ALU.mult, r=[("PW", l)], w=[ta])
                    kb.ts("dve", PW[:, l + 1, 1, 1, :], ta[:], 2.0, ALU.mult, r=[ta], w=[("PW", l + 1)])
                    kb.ts("dve", PW[:, l + 1, 1, 0, :], ta[:], -2.0, ALU.mult, r=[ta], w=[("PW", l + 1)])
                ZW = 514
                KQ = 4
                Z = p.sb("Zq", [128, 2, KQ, ZW], F32)
                z1 = p.sb("zt1", [128, 2, KQ, 256], F32)
                z2 = p.sb("zt2", [128, 2, KQ, 256], F32)
                Za = Z[:]
                pstr = list(Za.ap[0])
                for kq in range(32 // KQ):
                    ks = slice(kq * KQ, (kq + 1) * KQ)
                    kb.copy("dve", Z[:, :, :, 0], x0[:, :, ks], r=[x0], w=[Z])
                    n = 1
                    for l in range(NL):
                        m = min(n, 513 - n)
                        Pa = PW[:, l, 0, :, ks]
                        Qa = PW[:, l, 1, :, ks]
                        Pb = AP(Pa.tensor, Pa.offset, [list(Pa.ap[0]), list(Pa.ap[1]), list(Pa.ap[2]), [0, m]])
                        Qb = AP(Qa.tensor, Qa.offset, [list(Qa.ap[0]), list(Qa.ap[1]), list(Qa.ap[2]), [0, m]])
                        Zsw = AP(Za.tensor, Za.offset + KQ * ZW, [pstr, [-KQ * ZW, 2], [ZW, KQ], [1, m]])
                        kb.tt("dve", z1[:, :, :, 0:m], Z[:, :, :, 0:m], Pb, ALU.mult, r=[Z, ("PW", l)], w=[z1])
                        kb.tt("dve", z2[:, :, :, 0:m], Qb, Zsw, ALU.mult, r=[Z, ("PW", l)], w=[z2])
                        kb.tt("dve", Z[:, :, :, n:n + m], z1[:, :, :, 0:m], z2[:, :, :, 0:m], ALU.add, r=[z1, z2], w=[Z])
                        n *= 2
                    kb.tt("dve", X[:, :, ks, 0:513], X[:, :, ks, 0:513], Z[:, :, :, 0:513], ALU.add, r=[Z], w=[("Xq", kq)])
                p.flush()
                p.stack = sv2
        if I.get("ygT") is None:
            return
        psy_ring = Ring(p, "psy", 3, [128, NCH], F32, psum=True)
        yg_ring = Ring(p, "yg", 2, [128, T], BF16)
        ygv = ygT.rearrange("(k p) t -> p k t", p=128)
        for kk in range(8):
            uc = u_ring.next()
            kb.dma("sp", uc[:], uTv[:, kk, :], w=[uc])
            ucv = uc[:].rearrange("p (c j) -> p j c", j=8)
            yg = yg_ring.next()
            ygs = yg[:].rearrange("p (c j) -> p j c", j=8)
            for s in range(8):
                psy = psy_ring.next()
                for j in range(s + 1):
                    kb.mm(psy[:], Kt[:, s - j, kk, :], ucv[:, j, :], j == 0, False, r=[uc], w=[psy])
                for q in range(4):
                    k = 4 * kk + q
                    for ri in range(2):
                        p.op("pe", lambda e, o=psy[32 * q:32 * q + 32, :], l=Cf[:, s, k, ri, :], rr=X[:, ri, k, 0:NCH],
                             q=q, last=(q == 3 and ri == 1):
                             e.matmul(o, l, rr, start=False, stop=last, tile_position=(0, 32 * q), skip_group_check=True),
                             reads=[], writes=[psy])
                kb.act(ygs[:, s, :], psy[:], AF.Gelu_apprx_tanh, r=[psy], w=[(yg, s)])
            kb.dma("sp", ygv[:, kk, :], yg[:], r=[(yg, s) for s in range(8)], w=[("ygT", kk)])


def phase_glu_router(kb, I):
    p, c = kb.p, kb.c
    gains = c["gains"]
    with p.phase():
        psr = c["ps_ring"] = Ring(p, "ps", 6, [128, TT], F32, psum=True)
        norm_rings(kb, p)
        Wglu = p.sb("Wglu", [128, 8, 2 * D], BF16)
        kb.load_w(Wglu, I["s5_w_glu"], D, 2 * D)
        Wr = p.sb("Wr", [128, 8, 8], F32)
        kb.dma("sp", Wr[:], I["w_router"].rearrange("p (a b) -> p a b", a=8), w=[Wr])
        ident = p.sb("ident", [128, 128], F32)
        kb.memset("pool", ident[:], 0.0, w=[ident])
        p.op("pool", lambda e: e.affine_select(ident[:], ident[:], [[1, 128]], ALU.not_equal, 1.0,
                                               base=0, channel_multiplier=-1), reads=[ident], writes=[ident])
        yg_ring = Ring(p, "ygt", 2, [128, 8, TT], BF16)
        x_ring = Ring(p, "xt", 2, [128, 8, TT], F32)
        h3_ring = Ring(p, "h3", 2, [128, 8, TT], F32)
        hn_ring = Ring(p, "hn", 2, [128, 8, TT], BF16)
        hf_ring = Ring(p, "hf", 1, [128, 8, TT], F32)
        sg_ring = Ring(p, "sg", 3, [128, TT], F32)
        lg_ring = Ring(p, "lg", 2, [128, 8], F32)
        sm_ring = Ring(p, "sm", 2, [128, 16], F32)
        cb_ring = Ring(p, "cb", 2, [128, 8], F32)
        ct_ring = Ring(p, "cbt", 2, [8, TT], F32)
        ygv = I["ygT"].rearrange("(k p) t -> p k t", p=128)
        h2v = I["h2T"].rearrange("(k p) t -> p k t", p=128)
        h3v = I["h3T"].rearrange("(k p) t -> p k t", p=128)
        hn4v = I["hn4T"].rearrange("(k p) t -> p k t", p=128)
        for qt in range(T // TT):
            cols = slice(qt * TT, (qt + 1) * TT)
            yg = yg_ring.next()
            kb.dma("sp", yg[:], ygv[:, :, cols], w=[yg])
            xt = x_ring.next()
            kb.dma("sp", xt[:], h2v[:, :, cols], w=[xt])
            h3 = h3_ring.next()
            for m in range(8):
                psa = psr.next()
                psb = psr.next()
                for k in range(8):
                    kb.mm(psa[:], Wglu[:, k, m * 128:(m + 1) * 128], yg[:, k, :], k == 0, k == 7, r=[yg] + kb.wk(Wglu), w=[psa])
                for k in range(8):
                    kb.mm(psb[:], Wglu[:, k, D + m * 128:D + (m + 1) * 128], yg[:, k, :], k == 0, k == 7, r=[yg] + kb.wk(Wglu), w=[psb])
                sg = sg_ring.next()
                kb.act(sg[:], psb[:], AF.Sigmoid, r=[psb], w=[sg])
                kb.tt("dve", sg[:], psa[:], sg[:], ALU.mult, r=[psa, sg], w=[sg])
                kb.tt("pool", h3[:, m, :], sg[:], xt[:, m, :], ALU.add, r=[sg, xt], w=[(h3, m)])
            kb.dma("sp", h3v[:, :, cols], h3[:], r=[(h3, m) for m in range(8)], w=[("h3T", qt)])
            hn = hn_ring.next()
            rs = kb.rmsnorm([h3[:, k, :] for k in range(8)], [(h3, k) for k in range(8)], 8, D, gains, G_FFN1, hn, hn)
            kb.dma("sp", hn4v[:, :, cols], hn[:], r=[(hn, k) for k in range(8)], w=[("hn4T", qt)])
            hf = hf_ring.next()
            for k in range(8):
                kb.stt("dve", hf[:, k, :], h3[:, k, :], gains[:, G_FFN1 + k:G_FFN1 + k + 1], rs[:], ALU.mult, ALU.mult,
                       r=[(h3, k), rs], w=[(hf, k)])
            cbt = ct_ring.next()
            for tb in range(4):
                psl = psr.next()
                for k in range(8):
                    kb.mm(psl[:, 0:8], hf[:, k, tb * 128:(tb + 1) * 128], Wr[:, k, :], k == 0, k == 7, r=[(hf, k), Wr], w=[psl])
                lg = lg_ring.next()
                kb.copy("dve", lg[:], psl[:, 0:8], r=[psl], w=[lg])
                sm = sm_ring.next()
                p.op("dve", lambda e, o=sm[:, 0:8], i=lg[:]: e.max(o, i), reads=[lg], writes=[sm])
                kb.tt("dve", sm[:, 8:9], sm[:, 1:2], sm[:, 0:1], ALU.subtract, r=[sm], w=[sm])
                kb.act(sm[:, 9:10], sm[:, 8:9], AF.Exp, r=[sm], w=[sm])
                kb.ts("dve", sm[:, 10:11], sm[:, 9:10], 1.0, ALU.add, r=[sm], w=[sm])
                kb.recip(sm[:, 10:11], sm[:, 10:11], r=[sm], w=[sm])
                kb.tt("dve", sm[:, 11:12], sm[:, 9:10], sm[:, 10:11], ALU.mult, r=[sm], w=[sm])
                cb = cb_ring.next()
                cb2 = cb_ring.next()
                kb.ts("dve", cb[:], lg[:], sm[:, 0:1], ALU.is_equal, r=[lg, sm], w=[cb], s2=sm[:, 10:11], op1=ALU.mult)
                kb.ts("dve", cb2[:], lg[:], sm[:, 1:2], ALU.is_equal, r=[lg, sm], w=[cb2], s2=sm[:, 11:12], op1=ALU.mult)
                kb.tt("dve", cb[:], cb[:], cb2[:], ALU.add, r=[cb, cb2], w=[cb])
                pst = psr.next()
                p.op("pe", lambda e, o=pst[0:8, 0:128], i=cb[:]: e.transpose(o, i, ident[:]), reads=[cb, ident], writes=[pst])
                kb.copy("dve", cbt[:, tb * 128:(tb + 1) * 128], pst[0:8, 0:128], r=[pst], w=[(cbt, tb)])
            kb.dma("sp", I["combT"][:, cols], cbt[:], r=[(cbt, tb) for tb in range(4)], w=[("combT", qt)])


def phase_moe(kb, I):
    p, c = kb.p, kb.c
    gains = c["gains"]
    experts = [(I["moe_wg%d" % e], I["moe_wu%d" % e], I["moe_wd%d" % e]) for e in range(8)]
    st = {}

    def comb(sup, qt, ei):
        key = (sup, ei)
        if st.get("cur") != key:
            st["cur"] = key
            if "sel" not in st:
                st["sel"] = p.sb("sel", [8, 8, 128], F32)
                kb.dma("sp", st["sel"][:], I["sel"].rearrange("p (a b) -> p a b", a=8), w=[st["sel"]])
                st["cT"] = p.sb("cT", [8, 2048], F32)
                st["bc_ring"] = Ring(p, "bc", 2, [128, 4, TT], F32)
                st["psb_ring"] = Ring(p, "psb", 1, [128, TT], F32, psum=True)
            if st.get("cTsup") != sup:
                st["cTsup"] = sup
                kb.dma("sp", st["cT"][:], I["combT"][:, sup * 2048:(sup + 1) * 2048], w=[st["cT"]])
            bc = st["bc"] = st["bc_ring"].next()
            for q2 in range(4):
                psb = st["psb_ring"].next()
                g0 = q2 * TT
                kb.mm(psb[:], st["sel"][:, ei, :], st["cT"][:, g0:g0 + TT], True, True, r=[st["sel"], st["cT"]], w=[psb])
                kb.copy("act", bc[:, q2, :], psb[:], r=[psb], w=[(bc, q2)])
        bc = st["bc"]
        return (bc[:, qt, :], (bc, qt))

    ffn_phase(kb, experts, 3584, I["hn4T"], I["h3T"], I["h4T"], comb=comb)
    with p.phase():
        psr = c["ps_ring"] = Ring(p, "ps", 2, [128, TT], F32, psum=True)
        norm_rings(kb, p)
        x_ring = Ring(p, "xt", 2, [128, 8, TT], F32)
        o_ring = Ring(p, "fo", 2, [128, 8, TT], F32)
        h4v = I["h4T"].rearrange("(k p) t -> p k t", p=128)
        outv = I["outT"].rearrange("(k p) t -> p k t", p=128)
        for qt in range(T // TT):
            cols = slice(qt * TT, (qt + 1) * TT)
            xt = x_ring.next()
            kb.dma("sp", xt[:], h4v[:, :, cols], w=[xt])
            o = o_ring.next()
            kb.rmsnorm([xt[:, k, :] for k in range(8)], [xt] * 8, 8, D, gains, G_FIN, o, o)
            kb.dma("sp", outv[:, :, cols], o[:], r=[(o, k) for k in range(8)], w=[("fout", qt)])


def host_weights_l1(inp, moe=True):
    m = {}
    lam_re, lam_im, ldt = inp["s5_lambda_re"][0], inp["s5_lambda_im"][0], inp["s5_log_dt"][0]
    b_re, b_im, c_re, c_im, d = inp["s5_b_re"][0], inp["s5_b_im"][0], inp["s5_c_re"][0], inp["s5_c_im"][0], inp["s5_d"][0]
    r = np.arange(128)
    glp, pp = r // 64, r % 64
    k = np.arange(32)
    g_pm = 2 * k[None, :] + glp[:, None]
    m["lam_re_pm"] = np.ascontiguousarray(lam_re[g_pm, pp[:, None]])
    m["lam_im_pm"] = np.ascontiguousarray(lam_im[g_pm, pp[:, None]])
    m["ldt_pm"] = np.ascontiguousarray(ldt[g_pm])
    for nm, arr, tr in (("Bp_re", b_re, False), ("Bp_im", b_im, False), ("Ct_re", c_re, True), ("Ct_im", c_im, True)):
        out = np.zeros((128, 32, 2, 16), np.float32)
        for gl in range(2):
            rows = np.where(glp == gl)[0]
            gg = g_pm[rows]
            if tr:
                vals = arr[gg, :, pp[rows][:, None]]
            else:
                vals = arr[gg, pp[rows][:, None], :]
            out[rows, :, gl, :] = vals
        m[nm] = np.ascontiguousarray(out.reshape(128, 1024))
    q_, gl_, h_ = r // 32, (r % 32) // 16, r % 16
    col = np.arange(1024)
    kk_, glc, pc = col // 128, (col % 128) // 64, col % 64
    g_fm = 2 * (4 * kk_[None, :] + q_[:, None]) + glc[None, :]
    m["lam_re_fm"] = np.ascontiguousarray(lam_re[g_fm, pc[None, :]])
    m["lam_im_fm"] = np.ascontiguousarray(lam_im[g_fm, pc[None, :]])
    m["ldt_fm"] = np.ascontiguousarray(ldt[g_fm])
    mask = (glc[None, :] == gl_[:, None])
    for nm, arr in (("Bt_re", b_re), ("Bt_im", b_im)):
        vals = arr[g_fm, pc[None, :], h_[:, None]]
        m[nm] = np.ascontiguousarray(np.where(mask, vals, np.float32(0)).astype(np.float32))
    m["d_col"] = np.ascontiguousarray(d.reshape(8, 128).T)
    sel = np.zeros((8, 8, 128), np.float32)
    for e in range(8):
        sel[e, e, :] = 1.0
    m["sel"] = sel.reshape(8, 1024)
    m["w_router"] = np.ascontiguousarray(inp["moe_w_router"][0].reshape(8, 128, 8).transpose(1, 0, 2).reshape(128, 64))
    m["s5_w_in"] = np.ascontiguousarray(inp["s5_w_in"][0])
    m["s5_w_glu"] = np.ascontiguousarray(inp["s5_w_glu"][0])
    for e in range(8 if moe else 0):
        m["moe_wg%d" % e] = inp["moe_w_gate"][0, e]
        m["moe_wu%d" % e] = inp["moe_w_up"][0, e]
        m["moe_wd%d" % e] = inp["moe_w_down"][0, e]
    return m


def build_A():
    nc = bass.Bass("TRN2", target_bir_lowering=False)
    kb = KB(nc)
    kb.c = {}
    kb.nheads = H
    p = kb.p
    I = make_inputs_l0(kb)
    for name, shp in (("s5_w_in", [D, D]), ("lam_re_pm", [128, 32]), ("lam_im_pm", [128, 32]), ("ldt_pm", [128, 32]),
                      ("lam_re_fm", [128, 1024]), ("lam_im_fm", [128, 1024]), ("ldt_fm", [128, 1024]),
                      ("Bp_re", [128, 1024]), ("Bp_im", [128, 1024]), ("Ct_re", [128, 1024]), ("Ct_im", [128, 1024]),
                      ("Bt_re", [128, 1024]), ("Bt_im", [128, 1024]), ("d_col", [128, 8])):
        I[name] = kb.din(name, shp)
    I["kT"] = kb.dscr("kT", [H, 97, S], BF16)
    I["qT"] = kb.dscr("qT", [H, 97, T], BF16)
    I["vS"] = kb.dscr("vS", [S, 1024], BF16)
    I["h1T"] = kb.dscr("h1T", [D, T], F32)
    I["hn2T"] = kb.dscr("hn2T", [D, T], BF16)
    I["h2T"] = kb.dout("h2T", [D, T], F32)
    I["uT"] = kb.dscr("uT", [D, T], BF16)
    I["ygT"] = None
    I["xfin"] = kb.dout("xfin", [128, 64], F32)
    setup_consts(kb, I)
    phase_kvq(kb, I)
    phase_attn(kb, I)
    ffn_phase(kb, [(I["ffn_wg"], I["ffn_wu"], I["ffn_wd"])], 2816, I["hn2T"], I["h1T"], I["h2T"])
    phase_s5(kb, I, ["zero"])
    p.finish()
    return nc


def build_B():
    nc = bass.Bass("TRN2", target_bir_lowering=False)
    kb = KB(nc)
    kb.c = {}
    p = kb.p
    I = make_inputs_l1(kb, False)
    I["h2T"] = kb.din("h2T", [D, T])
    I["uT"] = kb.dscr("uT", [D, T], BF16)
    I["ygT"] = kb.dscr("ygT", [D, T], BF16)
    I["h3T"] = kb.dscr("h3T", [D, T], F32)
    I["hn4T"] = kb.dscr("hn4T", [D, T], BF16)
    I["h4T"] = kb.dscr("h4T", [D, T], F32)
    I["combT"] = kb.dscr("combT", [8, T], F32)
    I["outT"] = kb.dout("outT", [D, T], F32)
    I["xfin"] = None
    setup_consts(kb, I)
    phase_s5(kb, I, ["x0"])
    phase_glu_router(kb, I)
    phase_moe(kb, I)
    p.finish()
    return nc


def build_full():
    nc = bass.Bass("TRN2", target_bir_lowering=False)
    kb = KB(nc)
    kb.c = {}
    kb.nheads = H
    p = kb.p
    I = make_inputs_l0(kb)
    I1 = make_inputs_l1(kb, False, skip=("gains", "cst", "x0"))
    I.update(I1)
    I["kT"] = kb.dscr("kT", [H, 97, S], BF16)
    I["qT"] = kb.dscr("qT", [H, 97, T], BF16)
    I["vS"] = kb.dscr("vS", [S, 1024], BF16)
    I["h1T"] = kb.dscr("h1T", [D, T], F32)
    I["hn2T"] = kb.dscr("hn2T", [D, T], BF16)
    I["h2T"] = kb.dscr("h2T", [D, T], F32)
    I["uT"] = kb.dscr("uT", [D, T], BF16)
    I["ygT"] = kb.dscr("ygT", [D, T], BF16)
    I["h3T"] = kb.dscr("h3T", [D, T], F32)
    I["hn4T"] = kb.dscr("hn4T", [D, T], BF16)
    I["h4T"] = kb.dscr("h4T", [D, T], F32)
    I["combT"] = kb.dscr("combT", [8, T], F32)
    I["xi"] = kb.dscr("xi", [128, 64], F32)
    I["xg"] = kb.dscr("xg", [256, 64], F32)
    I["xfin"] = None
    I["outT"] = kb.dout("outT", [D, T], F32)
    setup_consts(kb, I)
    phase_kvq(kb, I)
    phase_attn(kb, I)
    ffn_phase(kb, [(I["ffn_wg"], I["ffn_wu"], I["ffn_wd"])], 2816, I["hn2T"], I["h1T"], I["h2T"])
    phase_s5(kb, I, ["zero", "cc"])
    phase_glu_router(kb, I)
    phase_moe(kb, I)
    p.finish()
    return nc


_CACHE = {}


def kernel(**inp):
    inp = {k: np.asarray(v) for k, v in inp.items()}
    n = 8
    W = host_weights_l0(inp)
    W.update(host_weights_l1(inp))
    if "F" not in _CACHE:
        _CACHE["F"] = build_full()
    maps = []
    for c_ in range(n):
        m = dict(W)
        m.update(host_inputs_l0(inp, c_))
        cst = W["cst"].copy()
        cst[:, 3] = float(c_ % 2)
        m["cst"] = cst
        maps.append(m)
    res = run_bass_kernel_spmd(_CACHE["F"], maps, core_ids=list(range(n)))
    out = np.zeros((4, 8192, D), np.float32)
    for c_ in range(n):
        b, hf = c_ // 2, c_ % 2
        out[b, hf * T:(hf + 1) * T, :] = np.asarray(res.results[c_]["outT"]).T
    return out

NIT = 24
NSLOT = NIT * 512
IU32 = mybir.dt.uint32


def phase_glu_router_tm(kb, I):
    p, c = kb.p, kb.c
    gains = c["gains"]
    R = c["route"]
    with p.phase():
        psr = c["ps_ring"] = Ring(p, "ps", 4, [128, TT], F32, psum=True)
        pstf_ring = Ring(p, "pstf", 2, [128, TT], F32, psum=True)
        pstb_ring = Ring(p, "pstb", 1, [128, 1024], BF16, psum=True)
        norm_rings(kb, p)
        Wglu = p.sb("Wglu", [128, 8, 2 * D], BF16)
        kb.load_w(Wglu, I["s5_w_glu"], D, 2 * D)
        Wr = p.sb("Wr", [128, 8, 8], F32)
        kb.dma("sp", Wr[:], I["w_router"].rearrange("p (a b) -> p a b", a=8), w=[Wr])
        ident = p.sb("ident", [128, 128], F32)
        kb.memset("pool", ident[:], 0.0, w=[ident])
        p.op("pool", lambda e: e.affine_select(ident[:], ident[:], [[1, 128]], ALU.not_equal, 1.0,
                                               base=0, channel_multiplier=-1), reads=[ident], writes=[ident])
        identb = p.sb("identb", [128, 128], BF16)
        kb.copy("dve", identb[:], ident[:], r=[ident], w=[identb])
        yg_ring = Ring(p, "ygt", 2, [128, 8, TT], BF16)
        x_ring = Ring(p, "xt", 2, [128, 8, TT], F32)
        h3_ring = Ring(p, "h3", 2, [128, 8, TT], F32)
        hn_ring = Ring(p, "hn", 2, [128, 8, TT], BF16)
        hf_ring = Ring(p, "hf", 1, [128, 8, TT], F32)
        sg_ring = Ring(p, "sg", 3, [128, TT], F32)
        tmf_ring = Ring(p, "tmf", 2, [128, 1024], F32)
        tmb_ring = Ring(p, "tmb", 2, [128, 1024], BF16)
        ygv = I["ygT"].rearrange("(k p) t -> p k t", p=128)
        h2v = I["h2T"].rearrange("(k p) t -> p k t", p=128)
        for qt in range(T // TT):
            cols = slice(qt * TT, (qt + 1) * TT)
            yg = yg_ring.next()
            kb.dma("sp", yg[:], ygv[:, :, cols], w=[yg])
            xt = x_ring.next()
            kb.dma("sp", xt[:], h2v[:, :, cols], w=[xt])
            h3 = h3_ring.next()
            for m in range(8):
                psa = psr.next()
                psb = psr.next()
                for k in range(8):
                    kb.mm(psa[:], Wglu[:, k, m * 128:(m + 1) * 128], yg[:, k, :], k == 0, k == 7, r=[yg] + kb.wk(Wglu), w=[psa])
                for k in range(8):
                    kb.mm(psb[:], Wglu[:, k, D + m * 128:D + (m + 1) * 128], yg[:, k, :], k == 0, k == 7, r=[yg] + kb.wk(Wglu), w=[psb])
                sg = sg_ring.next()
                kb.act(sg[:], psb[:], AF.Sigmoid, r=[psb], w=[sg])
                kb.tt("dve", sg[:], psa[:], sg[:], ALU.mult, r=[psa, sg], w=[sg])
                kb.tt("pool", h3[:, m, :], sg[:], xt[:, m, :], ALU.add, r=[sg, xt], w=[(h3, m)])
            hn = hn_ring.next()
            rs = kb.rmsnorm([h3[:, k, :] for k in range(8)], [(h3, k) for k in range(8)], 8, D, gains, G_FFN1, hn, hn)
            hf = hf_ring.next()
            for k in range(8):
                kb.stt("dve", hf[:, k, :], h3[:, k, :], gains[:, G_FFN1 + k:G_FFN1 + k + 1], rs[:], ALU.mult, ALU.mult,
                       r=[(h3, k), rs], w=[(hf, k)])
            for tb in range(4):
                blk = qt * 4 + tb
                tsl = slice(tb * 128, (tb + 1) * 128)
                tmf = tmf_ring.next()
                for hh in range(2):
                    pst = pstf_ring.next()
                    for kq in range(4):
                        k = hh * 4 + kq
                        p.op("pe", lambda e, o=pst[:, kq * 128:(kq + 1) * 128], i=h3[:, k, tsl]: e.transpose(o, i, ident[:]),
                             reads=[(h3, k), ident], writes=[pst])
                    kb.copy("act", tmf[:, hh * 512:(hh + 1) * 512], pst[:], r=[pst], w=[(tmf, hh)])
                kb.dma("sp", I["h3TM"][blk * 128:(blk + 1) * 128, :], tmf[:], r=[(tmf, 0), (tmf, 1)], w=[("h3TM", blk)])
                tmb = tmb_ring.next()
                pstb = pstb_ring.next()
                for k in range(8):
                    p.op("pe", lambda e, o=pstb[:, k * 128:(k + 1) * 128], i=hn[:, k, tsl]: e.transpose(o, i, identb[:]),
                         reads=[(hn, k), identb], writes=[pstb])
                kb.copy("act", tmb[:], pstb[:], r=[pstb], w=[tmb])
                kb.dma("sp", I["hn4TM"][blk * 128:(blk + 1) * 128, :], tmb[:], r=[tmb], w=[("hn4TM", blk)])
                psl = psr.next()
                for k in range(8):
                    kb.mm(psl[:, 0:8], hf[:, k, tsl], Wr[:, k, :], k == 0, k == 7, r=[(hf, k), Wr], w=[psl])
                kb.copy("dve", R["LG"][:, blk, :], psl[:, 0:8], r=[psl], w=[("LG", blk)])


def phase_route(kb, I):
    p, c = kb.p, kb.c
    R = c["route"]
    NB = 32
    with p.phase():
        LG = R["LG"]
        v1 = p.sb("rv1", [128, NB], F32)
        v2 = p.sb("rv2", [128, NB], F32)
        lg2 = p.sb("rlg2", [128, NB, 8], F32)
        p.op("dve", lambda e: e.tensor_reduce(v1[:], LG[:], AX.X, ALU.max), reads=[], writes=[v1])
        kb.tt("dve", R["M1"][:], LG[:], bc_last(v1[:], 8), ALU.is_equal, r=[v1], w=[R["M1"]])
        kb.stt("dve", lg2[:], R["M1"][:], -1.0e30, LG[:], ALU.mult, ALU.add, r=[R["M1"]], w=[lg2])
        p.op("dve", lambda e: e.tensor_reduce(v2[:], lg2[:], AX.X, ALU.max), reads=[lg2], writes=[v2])
        kb.tt("dve", R["M2"][:], lg2[:], bc_last(v2[:], 8), ALU.is_equal, r=[v2, lg2], w=[R["M2"]])
        dlt = p.sb("rdlt", [128, NB], F32)
        kb.tt("dve", dlt[:], v2[:], v1[:], ALU.subtract, r=[v1, v2], w=[dlt])
        kb.act(dlt[:], dlt[:], AF.Exp, r=[dlt], w=[dlt])
        den_ = p.sb("rden", [128, NB], F32)
        kb.ts("dve", den_[:], dlt[:], 1.0, ALU.add, r=[dlt], w=[den_])
        kb.recip(den_[:], den_[:], r=[den_], w=[den_])
        kb.copy("dve", R["G"][:, :, 0], den_[:], r=[den_], w=[R["G"]])
        kb.tt("dve", R["G"][:, :, 1], dlt[:], den_[:], ALU.mult, r=[dlt, den_, R["G"]], w=[R["G"]])
        M = p.sb("rM", [128, NB, 8], F32)
        Mb = p.sb("rMb", [128, NB, 8], BF16)
        kb.tt("dve", M[:], R["M1"][:], R["M2"][:], ALU.add, r=[R["M1"], R["M2"]], w=[M])
        kb.copy("dve", Mb[:], M[:], r=[M], w=[Mb])
        U = p.sb("rU", [128, 128], BF16)
        kb.memset("pool", U[:], 1.0, w=[U])
        p.op("pool", lambda e: e.affine_select(U[:], U[:], [[1, 128]], ALU.is_gt, 0.0, base=0, channel_multiplier=-1),
             reads=[U], writes=[U])
        pst = p.ps("rpst", [128, NB * 8], F32)
        psp = p.ps("rpsp", [128, NB * 8], F32)
        Mbf = Mb[:].rearrange("p a b -> p (a b)")
        kb.mm(pst[:], c["ones_bf"][:], Mbf, True, True, r=[Mb, c["ones_bf"]], w=[pst])
        kb.mm(psp[:], U[:], Mbf, True, True, r=[Mb, U], w=[psp])
        tot = p.sb("rtot", [128, NB, 8], F32)
        pre = p.sb("rpre", [128, NB, 8], F32)
        kb.copy("dve", tot[:].rearrange("p a b -> p (a b)"), pst[:], r=[pst], w=[tot])
        kb.copy("dve", pre[:].rearrange("p a b -> p (a b)"), psp[:], r=[psp], w=[pre])
        boff = p.sb("rboff", [128, NB + 1, 8], F32)
        kb.memset("dve", boff[:, 0, :], 0.0, w=[boff])
        for b in range(NB):
            kb.tt("dve", boff[:, b + 1, :], boff[:, b, :], tot[:, b, :], ALU.add, r=[boff, tot], w=[boff])
        q = p.sb("rq", [128, 8], F32)
        qi = p.sb("rqi", [128, 8], I32)
        qf = p.sb("rqf", [128, 8], F32)
        fx = p.sb("rfx", [128, 8], F32)
        kb.ts("dve", q[:], boff[:, NB, :], 511.0, ALU.add, r=[boff], w=[q], s2=1.0 / 512.0, op1=ALU.mult)
        kb.copy("dve", qi[:], q[:], r=[q], w=[qi])
        kb.copy("dve", qf[:], qi[:], r=[qi], w=[qf])
        kb.tt("dve", fx[:], qf[:], q[:], ALU.is_gt, r=[qf, q], w=[fx])
        kb.tt("dve", qf[:], qf[:], fx[:], ALU.subtract, r=[qf, fx], w=[qf])
        start = p.sb("rstart", [128, 9], F32)
        kb.memset("dve", start[:, 0:1], 0.0, w=[start])
        for e in range(8):
            kb.stt("dve", start[:, e + 1:e + 2], qf[:, e:e + 1], 512.0, start[:, e:e + 1], ALU.mult, ALU.add,
                   r=[qf, start], w=[start])
        pos = p.sb("rpos", [128, NB, 8], F32)
        kb.tt("dve", pos[:], pre[:], boff[:, 0:NB, :], ALU.add, r=[pre, boff], w=[pos])
        kb.tt("dve", pos[:], pos[:], bc_mid(start[:, 0:8], NB), ALU.add, r=[pos, start], w=[pos])
        tmp = p.sb("rtmp", [128, NB, 8], F32)
        pf = p.sb("rpf", [128, NB], F32)
        for (Mx, dst) in ((R["M1"], R["posA"]), (R["M2"], R["posB"])):
            kb.tt("dve", tmp[:], pos[:], Mx[:], ALU.mult, r=[pos], w=[tmp])
            p.op("dve", lambda e, o=pf[:], i=tmp[:]: e.tensor_reduce(o, i, AX.X, ALU.add), reads=[tmp], writes=[pf])
            kb.copy("dve", dst[:], pf[:], r=[pf], w=[dst])
        tid = p.sb("rtid", [128, NB, 16], I32)
        p.op("pool", lambda e: e.iota(tid[:], [[128, NB], [0, 16]], base=0, channel_multiplier=1), writes=[tid])
        z = p.sb("rz", [128, NSLOT // 128 * 16], I32)
        kb.memset("pool", z[:], 0, w=[z])
        kb.dma("sp", I["tokT"].rearrange("(p j) c -> p (j c)", p=128), z[:], r=[z], w=["tokT0"])
        for b in range(NB):
            for di, dst in enumerate((R["posA"], R["posB"])):
                p.op("pool", lambda e, dst=dst, b=b: e.indirect_dma_start(
                    out=I["tokT"], out_offset=bass.IndirectOffsetOnAxis(ap=dst[:, b:b + 1], axis=0),
                    in_=tid[:, b, :], in_offset=None), reads=[dst, tid, "tokT0"], writes=[("tokT", b, di)], dma=True)
        thr = p.sb("rthr", [128, NIT], F32)
        p.op("pool", lambda e: e.iota(thr[:], [[512, NIT]], base=0, channel_multiplier=0, allow_small_or_imprecise_dtypes=True),
             writes=[thr])
        ei = p.sb("rei", [128, NIT], F32)
        kb.memset("dve", ei[:], 0.0, w=[ei])
        for e in range(1, 8):
            kb.stt("dve", ei[:], thr[:], start[:, e:e + 1], ei[:], ALU.is_ge, ALU.add, r=[thr, start, ei], w=[ei])
        pj = p.sb("rpj", [128, NIT, 14], F32)
        p.op("pool", lambda e: e.iota(pj[:], [[0, NIT], [128, 14]], base=0, channel_multiplier=1,
                                      allow_small_or_imprecise_dtypes=True), writes=[pj])
        e1792 = p.sb("re1792", [128, NIT], F32)
        kb.ts("dve", e1792[:], ei[:], 1792.0, ALU.mult, r=[ei], w=[e1792])
        kb.tt("dve", pj[:], pj[:], bc_last(e1792[:], 14), ALU.add, r=[pj, e1792], w=[pj])
        kb.copy("dve", R["widx"][:], pj[:], r=[pj], w=[R["widx"]])


def phase_moe_routed(kb, I):
    p, c = kb.p, kb.c
    R = c["route"]
    with p.phase():
        identb = p.sb("identb", [128, 128], BF16)
        idf = p.sb("idf", [128, 128], F32)
        kb.memset("pool", idf[:], 0.0, w=[idf])
        p.op("pool", lambda e: e.affine_select(idf[:], idf[:], [[1, 128]], ALU.not_equal, 1.0,
                                               base=0, channel_multiplier=-1), reads=[idf], writes=[idf])
        kb.copy("dve", identb[:], idf[:], r=[idf], w=[identb])
        idx_ring = Ring(p, "ix", 2, [128, 4, 16], I32)
        xg_ring = Ring(p, "xg", 2, [128, 4, 1024], BF16)
        xT_ring = Ring(p, "xT", 2, [128, 8, TT], BF16)
        wg_ring = Ring(p, "wg", 3, [128, 8, 512], BF16)
        wu_ring = Ring(p, "wu", 3, [128, 8, 512], BF16)
        wd_ring = Ring(p, "wd", 3, [128, 4, 1024], BF16)
        a_ring = Ring(p, "a", 2, [128, 4, TT], BF16)
        sg_ring = Ring(p, "sg", 3, [128, TT], F32)
        ya_ring = Ring(p, "ya", 2, [128, 4, 1024], F32)
        psg_ring = Ring(p, "psg", 2, [128, TT], F32, psum=True)
        psu_ring = Ring(p, "psu", 2, [128, TT], F32, psum=True)
        psd_ring = Ring(p, "psd", 2, [128, TT], F32, psum=True)
        pst_ring = Ring(p, "pstx", 2, [128, TT], BF16, psum=True)
        Yv = I["Y"].rearrange("(i j p) f -> i p j f", j=4, p=128)
        tokv = I["tokT"].rearrange("(i j p) c -> i p j c", j=4, p=128)
        def emit_down(a, Wd, ya, first):
            for j in range(4):
                for hc in range(2):
                    psd = psd_ring.next()
                    for f in range(4):
                        kb.mm(psd[:], a[:, f, j * 128:(j + 1) * 128], Wd[:, f, hc * 512:(hc + 1) * 512], f == 0, f == 3,
                              r=[(a, f), (Wd, 0), (Wd, 1)], w=[psd])
                    ysl = ya[:, j, hc * 512:(hc + 1) * 512]
                    if first:
                        kb.copy("act", ysl, psd[:], r=[psd], w=[(ya, j, hc)])
                    else:
                        kb.tt("dve", ysl, psd[:], ysl, ALU.add, r=[psd, (ya, j, hc)], w=[(ya, j, hc)])

        pend = None
        xgs = {}

        def gather_tokens(it):
            ix = idx_ring.next()
            kb.dma("sp", ix[:], tokv[it], w=[ix])
            xg = xg_ring.next()
            for j in range(4):
                p.op("pool", lambda e, o=xg[:, j, :], ia=ix[:, j, 0:1]: e.indirect_dma_start(
                    out=o, out_offset=None, in_=I["hn4TM"], in_offset=bass.IndirectOffsetOnAxis(ap=ia, axis=0)),
                    reads=[ix], writes=[(xg, j)], dma=True)
            xgs[it] = xg

        gather_tokens(0)
        for it in range(NIT):
            xg = xgs.pop(it)
            xT = xT_ring.next()
            for k in range(8):
                pst = pst_ring.next()
                for j in range(4):
                    p.op("pe", lambda e, o=pst[:, j * 128:(j + 1) * 128], i=xg[:, j, k * 128:(k + 1) * 128]:
                         e.transpose(o, i, identb[:]), reads=[(xg, j), identb], writes=[pst])
                kb.copy("act" if k % 2 else "dve", xT[:, k, :], pst[:], r=[pst], w=[(xT, k)])
            ya = ya_ring.next()
            for blk in range(7):
                Wg = wg_ring.next()
                Wu = wu_ring.next()
                Wd = wd_ring.next()
                for (Wt, tab, nm) in ((Wg, I["WG"], "g"), (Wu, I["WU"], "u"), (Wd, I["WD"], "d")):
                    for half in range(2):
                        if nm == "d":
                            o = Wt[:, 2 * half:2 * half + 2, :].rearrange("p a b -> p (a b)")
                        else:
                            o = Wt[:, 4 * half:4 * half + 4, :].rearrange("p a b -> p (a b)")
                        p.op("pool", lambda e, o=o, tab=tab, ia=R["widx"][:, it, 2 * blk + half:2 * blk + half + 1]:
                             e.indirect_dma_start(out=o, out_offset=None, in_=tab,
                                                  in_offset=bass.IndirectOffsetOnAxis(ap=ia, axis=0)),
                             reads=[R["widx"]], writes=[(Wt, half)], dma=True)
                if blk == 1 and it + 1 < NIT:
                    gather_tokens(it + 1)
                a = a_ring.next()
                for f in range(4):
                    psg = psg_ring.next()
                    psu = psu_ring.next()
                    for k in range(8):
                        kb.mm(psg[:], Wg[:, k, f * 128:(f + 1) * 128], xT[:, k, :], k == 0, k == 7,
                              r=[(xT, k), (Wg, 0), (Wg, 1)], w=[psg])
                    for k in range(8):
                        kb.mm(psu[:], Wu[:, k, f * 128:(f + 1) * 128], xT[:, k, :], k == 0, k == 7,
                              r=[(xT, k), (Wu, 0), (Wu, 1)], w=[psu])
                    sg = sg_ring.next()
                    kb.act(sg[:], psg[:], AF.Silu, r=[psg], w=[sg])
                    kb.tt("dve", a[:, f, :], psu[:], sg[:], ALU.mult, r=[psu, sg], w=[(a, f)])
                if pend is not None:
                    emit_down(*pend)
                pend = (a, Wd, ya, blk == 0)
            emit_down(*pend)
            pend = None
            kb.dma("sp", Yv[it], ya[:], r=[(ya, j, hc) for j in range(4) for hc in range(2)], w=[("Y", it)])


def phase_combine(kb, I):
    p, c = kb.p, kb.c
    R = c["route"]
    with p.phase():
        gf = p.sb("gfin", [128, 1024], F32)
        kb.dma("sp", gf[:], I["gfin_bc"], w=[gf])
        yA_ring = Ring(p, "yA", 2, [128, 1024], F32)
        yB_ring = Ring(p, "yB", 2, [128, 1024], F32)
        h_ring = Ring(p, "h3r", 2, [128, 1024], F32)
        j_ring = Ring(p, "junk", 2, [128, 1024], F32)
        ss_ring = Ring(p, "ss", 2, [128, 2], F32)
        o_ring = Ring(p, "oo", 2, [128, 1024], F32)
        for b in range(32):
            yA = yA_ring.next()
            yB = yB_ring.next()
            h = h_ring.next()
            for (yy, pos) in ((yA, R["posA"]), (yB, R["posB"])):
                p.op("pool", lambda e, o=yy[:], ia=pos[:, b:b + 1]: e.indirect_dma_start(
                    out=o, out_offset=None, in_=I["Y"], in_offset=bass.IndirectOffsetOnAxis(ap=ia, axis=0)),
                    reads=[pos], writes=[yy], dma=True)
            kb.dma("sp", h[:], I["h3TM"][b * 128:(b + 1) * 128, :], w=[h])
            kb.act(yA[:], yA[:], AF.Copy, r=[yA], w=[yA], scale=R["G"][:, b, 0:1])
            kb.act(yB[:], yB[:], AF.Copy, r=[yB], w=[yB], scale=R["G"][:, b, 1:2])
            kb.tt("pool", yA[:], yA[:], yB[:], ALU.add, r=[yA, yB], w=[yA])
            kb.tt("dve", h[:], yA[:], h[:], ALU.add, r=[yA, h], w=[h])
            jk = j_ring.next()
            ss = ss_ring.next()
            p.op("act", lambda e, o=jk[:], i=h[:], a=ss[:, 0:1]: e.activation(o, i, AF.Square, accum_out=a),
                 reads=[h], writes=[jk, ss])
            kb.act(ss[:, 1:2], ss[:, 0:1], AF.Sqrt, r=[ss], w=[ss], bias=c["cst"][:, 2:3], scale=1.0 / D)
            kb.recip(ss[:, 1:2], ss[:, 1:2], r=[ss], w=[ss])
            o = o_ring.next()
            kb.stt("dve", o[:], h[:], ss[:, 1:2], gf[:], ALU.mult, ALU.mult, r=[h, ss, gf], w=[o])
            kb.dma("sp", I["out_tm"][b * 128:(b + 1) * 128, :], o[:], r=[o], w=[("out", b)])


def build_full_routed():
    nc = bass.Bass("TRN2", target_bir_lowering=False)
    kb = KB(nc)
    kb.c = {}
    kb.nheads = H
    p = kb.p
    I = make_inputs_l0(kb)
    I1 = make_inputs_l1(kb, False, skip=("gains", "cst", "x0", "sel"), moe=False)
    I.update(I1)
    I["WG"] = kb.din("WG", [8 * 7 * 2 * 128, 2048])
    I["WU"] = kb.din("WU", [8 * 7 * 2 * 128, 2048])
    I["WD"] = kb.din("WD", [8 * 7 * 2 * 128, 2048])
    I["gfin_bc"] = kb.din("gfin_bc", [128, 1024])
    for nm, shp, dt in (("kT", [H, 97, S], BF16), ("qT", [H, 97, T], BF16), ("vS", [S, 1024], BF16), ("h1T", [D, T], F32),
                        ("hn2T", [D, T], BF16), ("h2T", [D, T], F32), ("uT", [D, T], BF16), ("ygT", [D, T], BF16),
                        ("h3TM", [T, D], F32), ("hn4TM", [T, D], BF16), ("Y", [NSLOT, D], F32), ("tokT", [NSLOT, 16], I32),
                        ("xi", [128, 64], F32), ("xg", [256, 64], F32)):
        I[nm] = kb.dscr(nm, shp, dt)
    I["xfin"] = None
    I["out_tm"] = kb.dout("out_tm", [T, D], F32)
    setup_consts(kb, I)
    R = kb.c["route"] = {}
    R["M1"] = p.sb("rM1", [128, 32, 8], F32)
    R["M2"] = p.sb("rM2", [128, 32, 8], F32)
    R["G"] = p.sb("rG", [128, 32, 2], F32)
    R["LG"] = p.sb("rLG", [128, 32, 8], F32)
    R["posA"] = p.sb("rposA", [128, 32], I32)
    R["posB"] = p.sb("rposB", [128, 32], I32)
    R["widx"] = p.sb("rwidx", [128, NIT, 14], I32)
    phase_kvq(kb, I)
    phase_attn(kb, I)
    ffn_phase(kb, [(I["ffn_wg"], I["ffn_wu"], I["ffn_wd"])], 2816, I["hn2T"], I["h1T"], I["h2T"])
    I["s5_fix"] = True
    phase_s5(kb, I, ["zero"])
    phase_glu_router_tm(kb, I)
    phase_route(kb, I)
    phase_moe_routed(kb, I)
    phase_combine(kb, I)
    p.finish()
    return nc


def host_weights_routed(inp):
    m = {}
    wg = inp["moe_w_gate"][0].reshape(8, 2, 4, 128, 7, 512)
    m["WG"] = np.ascontiguousarray(wg.transpose(0, 4, 1, 3, 2, 5).reshape(8 * 7 * 2 * 128, 2048))
    wu = inp["moe_w_up"][0].reshape(8, 2, 4, 128, 7, 512)
    m["WU"] = np.ascontiguousarray(wu.transpose(0, 4, 1, 3, 2, 5).reshape(8 * 7 * 2 * 128, 2048))
    wd = inp["moe_w_down"][0].reshape(8, 7, 2, 2, 128, 1024)
    m["WD"] = np.ascontiguousarray(wd.transpose(0, 1, 2, 4, 3, 5).reshape(8 * 7 * 2 * 128, 2048))
    m["gfin_bc"] = np.ascontiguousarray(np.broadcast_to(inp["final_norm"][None, :], (128, 1024)))
    return m


def kernel(**inp):
    inp = {k: np.asarray(v) for k, v in inp.items()}
    n = 8
    W = host_weights_l0(inp)
    W.update(host_weights_l1(inp, moe=False))
    W.update(host_weights_routed(inp))
    W.pop("sel", None)
    if "R" not in _CACHE:
        _CACHE["R"] = build_full_routed()
    maps = []
    for c_ in range(n):
        m = dict(W)
        m.update(host_inputs_l0(inp, c_))
        cst = W["cst"].copy()
        cst[:, 3] = float(c_ % 2)
        m["cst"] = cst
        maps.append(m)
    res = run_bass_kernel_spmd(_CACHE["R"], maps, core_ids=list(range(n)))
    out = np.zeros((4, 8192, D), np.float32)
    for c_ in range(n):
        b, hf = c_ // 2, c_ % 2
        out[b, hf * T:(hf + 1) * T, :] = np.asarray(res.results[c_]["out_tm"])
    return out
```
